# Optimizing a Trainium2 kernel written in Bass

```python
import jax, jax.numpy as jnp
from jax import lax
import numpy as np

D_MODEL = 2048
BATCH = 1
SEQ = 8192
DEPTH = 4

GRID_W = 64
CTX_LEN = 256
N_MIXERS = 2
LRU_WIDTH = D_MODEL
LRU_HEADS = 8
LRU_HEAD_DIM = LRU_WIDTH // LRU_HEADS
CONV_WIDTH = 4
LRU_C = 8.0
CHUNK = 128
SGU_WIDTH = D_MODEL
SGU_GROUPS = 8
SGU_GROUP_DIM = SGU_WIDTH // SGU_GROUPS
ROWS_PER_CHUNK = CHUNK // GRID_W
FFN_HIDDEN = ((8 * D_MODEL // 3 + 255) // 256) * 256
N_EXPERTS = 8
TOP_K = 2
MOE_BLOCK = 128
N_MOD = 6
EPS = 1e-6
N_EVEN = (DEPTH + 1) // 2
N_ODD = DEPTH // 2

kernel_name = "hybrid_rglru_chunkmlp_moe_dit"


def rmsnorm(x, g):
    xf = x.astype(jnp.float32)
    y = xf * lax.rsqrt(jnp.mean(xf * xf, axis=-1, keepdims=True) + EPS)
    return (y * g.astype(jnp.float32)).astype(x.dtype)


def layernorm(x, g, b):
    xf = x.astype(jnp.float32)
    mu = jnp.mean(xf, axis=-1, keepdims=True)
    xc = xf - mu
    y = xc * lax.rsqrt(jnp.mean(xc * xc, axis=-1, keepdims=True) + EPS)
    return (y * g.astype(jnp.float32) + b.astype(jnp.float32)).astype(x.dtype)


def modulate(h, shift, scale):
    return h * (1 + scale[:, None, :]) + shift[:, None, :]


def swiglu(t, w1, w3, w2):
    return (jax.nn.silu(t @ w1) * (t @ w3)) @ w2


def _dwconv_centred(x, w, b):
    n = x.shape[1]
    pad_l = CONV_WIDTH // 2
    pad_r = CONV_WIDTH - 1 - pad_l
    xp = jnp.pad(x, ((0, 0), (pad_l, pad_r), (0, 0)))
    y = b
    for k in range(CONV_WIDTH):
        y = y + w[k] * xp[:, k:k + n]
    return y


def _rglru_coeffs(xc, w_a, b_a, w_x, b_x, lam):
    bsz, n, _ = xc.shape
    xh = xc.reshape(bsz, n, LRU_HEADS, LRU_HEAD_DIM)
    gate_r = jax.nn.sigmoid((jnp.einsum("bnhi,hij->bnhj", xh, w_a) + b_a).astype(jnp.float32))
    gate_i = jax.nn.sigmoid((jnp.einsum("bnhi,hij->bnhj", xh, w_x) + b_x).astype(jnp.float32))
    log_a = -LRU_C * gate_r * jax.nn.softplus(-lam.astype(jnp.float32)).reshape(LRU_HEADS, LRU_HEAD_DIM)
    a = jnp.exp(log_a)
    b = jnp.sqrt(-jnp.expm1(2.0 * log_a)) * gate_i * xh.astype(jnp.float32)
    return a.reshape(bsz, n, LRU_WIDTH), b.reshape(bsz, n, LRU_WIDTH)


def _linear_scan(a, b, h0, reverse):
    edge = -1 if reverse else 0
    b = b.at[:, edge].add(a[:, edge] * h0)

    def combine(left, right):
        a_l, b_l = left
        a_r, b_r = right
        return a_l * a_r, a_r * b_l + b_r

    _, h = lax.associative_scan(combine, (a, b), axis=1, reverse=reverse)
    return h


def rglru_mixer(h_lat, h_ctx, w_in, conv_w, conv_b, w_a, b_a, w_x, b_x, lam, w_out, ctx_out):
    def branches(h):
        z = h @ w_in
        xb, gb = z[..., :LRU_WIDTH], z[..., LRU_WIDTH:]
        return _dwconv_centred(xb, conv_w, conv_b), gb

    xc_c, g_c = branches(h_ctx)
    xc_l, g_l = branches(h_lat)
    ys_l, ys_c = [], []
    for d, reverse in enumerate((False, True)):
        a_c, b_c = _rglru_coeffs(xc_c, w_a[d], b_a[d], w_x[d], b_x[d], lam[d])
        hc = _linear_scan(a_c, b_c, jnp.zeros_like(a_c[:, 0]), reverse)
        h_final = hc[:, 0] if reverse else hc[:, -1]
        a_l, b_l = _rglru_coeffs(xc_l, w_a[d], b_a[d], w_x[d], b_x[d], lam[d])
        ys_l.append(_linear_scan(a_l, b_l, h_final, reverse))
        if ctx_out:
            ys_c.append(hc)
    y_l = (ys_l[0] + ys_l[1]).astype(h_lat.dtype)
    out_l = (y_l * jax.nn.gelu(g_l)) @ w_out
    out_c = None
    if ctx_out:
        y_c = (ys_c[0] + ys_c[1]).astype(h_ctx.dtype)
        out_c = (y_c * jax.nn.gelu(g_c)) @ w_out
    return out_l, out_c


def chunk_mlp_mixer(h, n_chunks, w_in, ln_g, ln_b, w_s, b_s, w_out):
    bsz, n, _ = h.shape
    z = jax.nn.gelu(h @ w_in)
    u, v = z[..., :SGU_WIDTH], z[..., SGU_WIDTH:]
    v = layernorm(v, ln_g, ln_b)
    vg = v.reshape(bsz, n_chunks, CHUNK, SGU_GROUPS, SGU_GROUP_DIM)
    vm = jnp.einsum("gpq,bnqgc->bnpgc", w_s, vg) + b_s.T[None, None, :, :, None]
    return (u * vm.reshape(bsz, n, SGU_WIDTH)) @ w_out


def moe_swiglu(t, w_router, w1, w3, w2):
    n, d = t.shape
    nk = n * TOP_K
    logits = (t @ w_router).astype(jnp.float32)
    top_vals, top_idx = lax.top_k(logits, TOP_K)
    gates = jax.nn.softmax(top_vals, axis=-1)
    expert = top_idx.reshape(nk)
    token = jnp.repeat(jnp.arange(n), TOP_K)
    gate = gates.reshape(nk)
    order = jnp.argsort(expert)
    e_sorted, tok_sorted, gate_sorted = expert[order], token[order], gate[order]
    counts = jnp.bincount(expert, length=N_EXPERTS)
    starts = jnp.cumsum(counts) - counts
    padded = ((counts + MOE_BLOCK - 1) // MOE_BLOCK) * MOE_BLOCK
    pends = jnp.cumsum(padded)
    pstarts = pends - padded
    pos = pstarts[e_sorted] + (jnp.arange(nk) - starts[e_sorted])
    n_blocks = -(-(nk + N_EXPERTS * (MOE_BLOCK - 1)) // MOE_BLOCK)
    cap = n_blocks * MOE_BLOCK
    xbuf = jnp.zeros((cap, d), t.dtype).at[pos].set(t[tok_sorted])
    block_expert = jnp.clip(jnp.searchsorted(pends, jnp.arange(n_blocks) * MOE_BLOCK, side="right"), 0, N_EXPERTS - 1)

    def expert_block(args):
        xb, e = args
        return swiglu(xb, w1[e], w3[e], w2[e])

    ybuf = lax.map(expert_block, (xbuf.reshape(n_blocks, MOE_BLOCK, d), block_expert))
    y_sorted = ybuf.reshape(cap, d)[pos]
    return jnp.zeros_like(t).at[tok_sorted].add(y_sorted * gate_sorted[:, None].astype(t.dtype))


def setup_inputs(seed: int = 0) -> dict:
    key = jax.random.key(seed)
    ks = jax.random.split(key, 32)
    f32 = jnp.float32
    D, W, E, F = D_MODEL, LRU_WIDTH, SGU_WIDTH, FFN_HIDDEN

    def nrm(k, shape, scale):
        return jax.random.normal(k, shape, f32) * scale

    lam_u = jax.random.uniform(ks[12], (N_EVEN, 2, W), f32, minval=0.9, maxval=0.999)
    return {
        "x": nrm(ks[0], (BATCH, SEQ, D), 1.0),
        "c": nrm(ks[1], (BATCH, D), 1.0),
        "ctx": nrm(ks[2], (BATCH, CTX_LEN, D), 1.0),
        "c_ctx": nrm(ks[3], (D,), 1.0),
        "norm_g": 1.0 + nrm(ks[4], (DEPTH, 2, D), 0.1),
        "final_g": 1.0 + nrm(ks[5], (D,), 0.1),
        "w_mod": nrm(ks[6], (DEPTH, D, N_MOD * D), D ** -0.5),
        "b_mod": nrm(ks[7], (DEPTH, N_MOD * D), 0.01),
        "lru_w_in": nrm(ks[8], (N_EVEN, D, 2 * W), D ** -0.5),
        "lru_conv_w": nrm(ks[9], (N_EVEN, CONV_WIDTH, W), CONV_WIDTH ** -0.5),
        "lru_conv_b": nrm(ks[10], (N_EVEN, W), 0.01),
        "lru_w_a": nrm(ks[11], (N_EVEN, 2, LRU_HEADS, LRU_HEAD_DIM, LRU_HEAD_DIM), LRU_HEAD_DIM ** -0.5),
        "lru_b_a": nrm(ks[13], (N_EVEN, 2, LRU_HEADS, LRU_HEAD_DIM), 0.01),
        "lru_w_x": nrm(ks[14], (N_EVEN, 2, LRU_HEADS, LRU_HEAD_DIM, LRU_HEAD_DIM), LRU_HEAD_DIM ** -0.5),
        "lru_b_x": nrm(ks[15], (N_EVEN, 2, LRU_HEADS, LRU_HEAD_DIM), 0.01),
        "lru_lambda": jnp.log(lam_u) - jnp.log1p(-lam_u),
        "lru_w_out": nrm(ks[16], (N_EVEN, W, D), W ** -0.5),
        "sgu_w_in": nrm(ks[17], (N_ODD, D, 2 * E), D ** -0.5),
        "sgu_ln_g": 1.0 + nrm(ks[18], (N_ODD, E), 0.1),
        "sgu_ln_b": nrm(ks[19], (N_ODD, E), 0.01),
        "sgu_w_s": nrm(ks[20], (N_ODD, SGU_GROUPS, CHUNK, CHUNK), CHUNK ** -0.5),
        "sgu_b_s": 1.0 + nrm(ks[21], (N_ODD, SGU_GROUPS, CHUNK), 0.1),
        "sgu_w_out": nrm(ks[22], (N_ODD, E, D), E ** -0.5),
        "ffn_w1": nrm(ks[23], (N_EVEN, D, F), D ** -0.5),
        "ffn_w3": nrm(ks[24], (N_EVEN, D, F), D ** -0.5),
        "ffn_w2": nrm(ks[25], (N_EVEN, F, D), F ** -0.5),
        "moe_router": nrm(ks[26], (N_ODD, D, N_EXPERTS), D ** -0.5),
        "moe_w1": nrm(ks[27], (N_ODD, N_EXPERTS, D, F), D ** -0.5),
        "moe_w3": nrm(ks[28], (N_ODD, N_EXPERTS, D, F), D ** -0.5),
        "moe_w2": nrm(ks[29], (N_ODD, N_EXPERTS, F, D), F ** -0.5),
    }


def reference(x, c, ctx, c_ctx, norm_g, final_g, w_mod, b_mod,
              lru_w_in, lru_conv_w, lru_conv_b, lru_w_a, lru_b_a, lru_w_x, lru_b_x, lru_lambda, lru_w_out,
              sgu_w_in, sgu_ln_g, sgu_ln_b, sgu_w_s, sgu_b_s, sgu_w_out,
              ffn_w1, ffn_w3, ffn_w2,
              moe_router, moe_w1, moe_w3, moe_w2):
    bsz, n_lat, d = x.shape
    n_ctx = ctx.shape[1]
    rows = n_lat // GRID_W
    n_lat_chunks = rows // ROWS_PER_CHUNK
    n_ctx_chunks = n_ctx // CHUNK
    sc = jax.nn.silu(c)
    scc = jax.nn.silu(c_ctx)[None]

    for i in range(DEPTH):
        j = i // N_MIXERS
        is_a = (i % N_MIXERS) == 0
        ctx_next = any(k % N_MIXERS == 0 for k in range(i + 1, DEPTH))
        ctx_read = is_a or ctx_next
        mod_l = (sc @ w_mod[i] + b_mod[i]).reshape(bsz, N_MOD, d)
        mod_c = (scc @ w_mod[i] + b_mod[i]).reshape(1, N_MOD, d)

        hl = modulate(rmsnorm(x, norm_g[i, 0]), mod_l[:, 0], mod_l[:, 1])
        hc = modulate(rmsnorm(ctx, norm_g[i, 0]), mod_c[:, 0], mod_c[:, 1]) if ctx_read else None
        if is_a:
            ml, mc = rglru_mixer(hl, hc, lru_w_in[j], lru_conv_w[j], lru_conv_b[j],
                                 lru_w_a[j], lru_b_a[j], lru_w_x[j], lru_b_x[j], lru_lambda[j],
                                 lru_w_out[j], ctx_next)
        else:
            ml = chunk_mlp_mixer(hl, n_lat_chunks, sgu_w_in[j], sgu_ln_g[j], sgu_ln_b[j],
                                 sgu_w_s[j], sgu_b_s[j], sgu_w_out[j])
            mc = (chunk_mlp_mixer(hc, n_ctx_chunks, sgu_w_in[j], sgu_ln_g[j], sgu_ln_b[j],
                                  sgu_w_s[j], sgu_b_s[j], sgu_w_out[j]) if ctx_next else None)
        x = x + mod_l[:, 2][:, None, :] * ml
        if ctx_next:
            ctx = ctx + mod_c[:, 2][:, None, :] * mc

        fl = modulate(rmsnorm(x, norm_g[i, 1]), mod_l[:, 3], mod_l[:, 4])
        if ctx_next:
            fc = modulate(rmsnorm(ctx, norm_g[i, 1]), mod_c[:, 3], mod_c[:, 4])
            tokens = jnp.concatenate([fc, fl], axis=1)
        else:
            tokens = fl
        n_tok = tokens.shape[1]
        flat = tokens.reshape(bsz * n_tok, d)
        if i % 2 == 0:
            y = swiglu(flat, ffn_w1[j], ffn_w3[j], ffn_w2[j])
        else:
            y = moe_swiglu(flat, moe_router[j], moe_w1[j], moe_w3[j], moe_w2[j])
        y = y.reshape(bsz, n_tok, d)
        if ctx_next:
            ctx = ctx + mod_c[:, 5][:, None, :] * y[:, :n_ctx]
            x = x + mod_l[:, 5][:, None, :] * y[:, n_ctx:]
        else:
            x = x + mod_l[:, 5][:, None, :] * y

    return rmsnorm(x, final_g)
```

```python
import numpy as np
from contextlib import ExitStack
import concourse.bass as bass
import concourse.mybir as mybir
from concourse.bass_utils import run_bass_kernel_spmd

F32 = mybir.dt.float32
BF16 = mybir.dt.bfloat16
ALU = mybir.AluOpType
AF = mybir.ActivationFunctionType
AX = mybir.AxisListType
EPS = 1e-6
LRU_C = 8.0


class Cfg:
    def __init__(self, D=2048, NL=1024, NCX=256, F=5632, HEADS=8, SGROUPS=8, NEXP=8, NCORES=8):
        self.D, self.NL, self.NCX, self.F = D, NL, NCX, F
        self.HEADS, self.SGROUPS, self.NEXP, self.NCORES = HEADS, SGROUPS, NEXP, NCORES
        self.KC = D // 128
        self.FC = F // 128
        self.HC = (D // HEADS) // 128
        self.GCH = (D // SGROUPS) // 128
        self.T = NCX + NL


class Prog:
    ENGS = ("pe", "act", "dve", "pool", "sp")

    def __init__(self, nc, es, n_dma_sems=8):
        self.nc, self.es = nc, es
        self.streams = {e: [] for e in self.ENGS}
        self.psem = {e: es.enter_context(nc.semaphore("ps_" + e)) for e in ("pe", "act", "dve", "pool")}
        self.pcount = {e: 0 for e in self.psem}
        self.known = {e: {} for e in self.ENGS}
        self.semobj = {"ps_" + e: s for e, s in self.psem.items()}
        self.dsems, self.dcount, self.dnext = {}, {}, {}
        for q in ("sp", "pool"):
            names = ["ds_%s_%d" % (q, i) for i in range(n_dma_sems)]
            self.dsems[q] = names
            for n in names:
                self.semobj[n] = es.enter_context(nc.semaphore(n))
                self.dcount[n] = 0
            self.dnext[q] = 0
        self.last_write, self.readers = {}, {}
        self.ccsem = es.enter_context(nc.semaphore("cc_sem"))
        self.semobj["cc"] = self.ccsem
        self.ccount = 0

    def collective(self, fn, reads=(), writes=()):
        self._emit_waits("pool", self._deps(reads, writes))
        self.ccount += 1
        name = "cc%d" % self.ccount
        sem = self.es.enter_context(self.nc.semaphore(name))
        self.semobj[name] = sem
        tok = (name, 1)
        self.streams["pool"].append(("op", fn, sem, 1))
        self._commit(tok, reads, writes)
        return tok

    def _deps(self, reads, writes):
        toks = []
        for k in reads:
            toks += self.last_write.get(k, [])
        for k in writes:
            toks += self.last_write.get(k, [])
            toks += self.readers.get(k, [])
        return toks

    def _emit_waits(self, eng, toks):
        need = {}
        own = "ps_" + eng
        for (sn, v) in toks:
            if sn == own and eng == "pe":
                continue
            if self.known[eng].get(sn, 0) >= v:
                continue
            if need.get(sn, 0) < v:
                need[sn] = v
        for sn, v in need.items():
            self.known[eng][sn] = v
            self.streams[eng].append(("wait", self.semobj[sn], v))

    def _commit(self, tok, reads, writes):
        for k in reads:
            self.readers.setdefault(k, []).append(tok)
        for k in writes:
            self.last_write[k] = [tok]
            self.readers[k] = []

    def op(self, eng, fn, reads=(), writes=()):
        self._emit_waits(eng, self._deps(reads, writes))
        self.pcount[eng] += 1
        tok = ("ps_" + eng, self.pcount[eng])
        self.streams[eng].append(("op", fn, self.psem[eng], 1))
        self._commit(tok, reads, writes)
        return tok

    def dma(self, q, fn, reads=(), writes=()):
        toks = self._deps(reads, writes)
        names = self.dsems[q]
        sn = names[self.dnext[q] % len(names)]
        self.dnext[q] += 1
        if self.dcount[sn] > 0:
            toks = toks + [(sn, self.dcount[sn])]
        self._emit_waits(q, toks)
        self.dcount[sn] += 16
        tok = (sn, self.dcount[sn])
        self.streams[q].append(("op", fn, self.semobj[sn], 16))
        self._commit(tok, reads, writes)
        return tok

    def wait_all(self, eng, keys):
        toks = []
        for k in keys:
            toks += self.last_write.get(k, [])
        self._emit_waits(eng, toks)

    def replay(self, block):
        def run(stream):
            def body(e):
                for item in stream:
                    if item[0] == "wait":
                        e.wait_ge(item[1], item[2])
                    else:
                        item[1](e).then_inc(item[2], item[3])
            return body
        block.tensor(run(self.streams["pe"]))
        block.scalar(run(self.streams["act"]))
        block.vector(run(self.streams["dve"]))
        block.gpsimd(run(self.streams["pool"]))
        block.sync(run(self.streams["sp"]))


def col_tiles(c0, c1, maxw=512):
    out = []
    while c0 < c1:
        w = min(maxw, c1 - c0)
        out.append((c0, c0 + w))
        c0 += w
    return out


class Builder:
    def __init__(self, cfg, stage):
        self.cfg, self.stage = cfg, stage
        self.nc = bass.Bass("TRN2", target_bir_lowering=False)
        self.es = ExitStack()
        self.P = Prog(self.nc, self.es)
        self.uid = 0
        self.ins, self.outs = {}, {}
        self.out_keys = []

    def din(self, name, shape):
        t = self.nc.dram_tensor(name, list(shape), F32, kind="ExternalInput").ap()
        self.ins[name] = t
        return t

    def dout(self, name, shape):
        t = self.nc.dram_tensor(name, list(shape), F32, kind="ExternalOutput").ap()
        self.outs[name] = t
        return t

    def sb(self, name, shape, dt, stack=None):
        return (stack or self.es).enter_context(self.nc.sbuf_tensor(name, list(shape), dt))

    def pst(self, name, stack=None):
        return (stack or self.es).enter_context(self.nc.psum_tensor(name, [128, 512], F32))

    def key(self, base):
        self.uid += 1
        return "%s#%d" % (base, self.uid)

    def setup_common(self):
        cfg, P, nc = self.cfg, self.P, self.nc
        KC = cfg.KC
        self.ident_d = self.din("ident", [128, 128])
        self.ident = self.sb("ident_s", [128, 128], F32)
        P.dma("sp", lambda e: e.dma_start(out=self.ident[:], in_=self.ident_d), writes=["ident"])
        self.ones32 = self.sb("ones32", [128, 128], F32)
        self.ones16 = self.sb("ones16", [128, 128], BF16)
        P.op("dve", lambda e: e.memset(self.ones32[:], 1.0), writes=["ones32"])
        P.op("dve", lambda e: e.memset(self.ones16[:], 1.0), writes=["ones16"])
        self.X = self.sb("X", [128, KC, cfg.T], F32)
        self.H = self.sb("H", [128, KC, cfg.T], BF16)
        self.RSTD = self.sb("RSTD", [128, cfg.T], F32)
        self.psA = [self.pst("psA%d" % i) for i in range(3)]
        self.psB = [self.pst("psB%d" % i) for i in range(3)]
        self.psC = [self.pst("psC%d" % i) for i in range(2)]
        self.segs = [(0, cfg.NCX, 1), (cfg.NCX, cfg.T, 0)]

    def load_fm_vec(self, dst_ap, src_1d, q="sp", writes=()):
        with self.nc.allow_non_contiguous_dma(reason="tiny per-feature vectors"):
            pass
        def f(e):
            return e.dma_start(out=dst_ap, in_=src_1d.rearrange("(k p) -> p k", p=128), allow_slow_non_contiguous=True)
        return self.P.dma(q, f, writes=list(writes))

    def compute_mods(self, li, w_mod, b_mod, c_in, cctx_in, norm_g):
        cfg, P = self.cfg, self.P
        KC = cfg.KC
        MOD = self.sb("MOD%d" % li, [128, 6 * KC, 2], F32)
        d = {}
        for nm in ("GS_A", "GS_B"):
            d[nm] = self.sb("%s%d" % (nm, li), [128, KC, 2], F32)
        es = ExitStack()
        S = self.sb("modS%d" % li, [128, KC, 2], F32, es)
        bm = self.sb("modB%d" % li, [128, 6 * KC], F32, es)
        kS = self.key("S")
        with self.nc.allow_non_contiguous_dma(reason="tiny"):
            P.dma("sp", lambda e: e.dma_start(out=S[:, :, 0], in_=c_in.rearrange("(k p) -> p k", p=128), allow_slow_non_contiguous=True), writes=[kS + "a"])
            P.dma("sp", lambda e: e.dma_start(out=S[:, :, 1], in_=cctx_in.rearrange("(k p) -> p k", p=128), allow_slow_non_contiguous=True), writes=[kS + "b"])
            P.dma("sp", lambda e: e.dma_start(out=bm[:], in_=b_mod.rearrange("(k p) -> p k", p=128), allow_slow_non_contiguous=True), writes=[kS + "bm"])
        P.op("act", lambda e: e.activation(out=S[:], in_=S[:], func=AF.Silu), reads=[kS + "a", kS + "b"], writes=[kS])
        CB = 512 if 6 * cfg.D >= 512 else 6 * cfg.D
        nblk = (6 * cfg.D) // CB
        wbuf = [self.sb("modW%d_%d" % (li, i), [128, KC, CB], F32, es) for i in range(2)]
        for b in range(nblk):
            wb = wbuf[b % 2]
            kw = "modw%d" % (b % 2)
            P.dma("sp", lambda e, wb=wb, b=b: e.dma_start(out=wb[:], in_=w_mod[:, b * CB:(b + 1) * CB].rearrange("(k p) n -> p k n", p=128)), writes=[kw])
            for oc in range(CB // 128):
                o = b * (CB // 128) + oc
                ps = self.psC[o % 2]
                kp = "psC%d" % (o % 2)
                def mm(e, wb=wb, oc=oc, ps=ps):
                    r = None
                    for k in range(KC):
                        r = e.matmul(ps[:, 0:2], lhsT=wb[:, k, oc * 128:(oc + 1) * 128], rhs=S[:, k, :], start=(k == 0), stop=(k == KC - 1))
                    return r
                P.op("pe", mm, reads=[kw, kS], writes=[kp])
                P.op("dve", lambda e, o=o, ps=ps: e.tensor_scalar(out=MOD[:, o, :], in0=ps[:, 0:2], scalar1=bm[:, o:o + 1], scalar2=None, op0=ALU.add),
                     reads=[kp, kS + "bm"], writes=["MOD%d" % li])
        G = self.sb("modG%d" % li, [128, 2, KC], F32, es)
        gk = self.fm_rows(lambda i: G[:, i, :], norm_g, 2, kS + "g")
        self.P.op("dve", lambda e: e.tensor_copy(out=G[:], in_=G[:]), reads=gk, writes=[kS + "g"])
        for s in range(2):
            P.op("dve", lambda e, s=s: e.scalar_tensor_tensor(out=d["GS_A"][:, :, s], in0=MOD[:, KC:2 * KC, s], scalar=1.0, in1=G[:, 0, :], op0=ALU.add, op1=ALU.mult),
                 reads=["MOD%d" % li, kS + "g"], writes=["GS_A%d" % li])
            P.op("dve", lambda e, s=s: e.scalar_tensor_tensor(out=d["GS_B"][:, :, s], in0=MOD[:, 4 * KC:5 * KC, s], scalar=1.0, in1=G[:, 1, :], op0=ALU.add, op1=ALU.mult),
                 reads=["MOD%d" % li, kS + "g"], writes=["GS_B%d" % li])
        d["SH_A"] = MOD[:, 0:KC, :]
        d["GT_A"] = MOD[:, 2 * KC:3 * KC, :]
        d["SH_B"] = MOD[:, 3 * KC:4 * KC, :]
        d["GT_B"] = MOD[:, 5 * KC:6 * KC, :]
        d["keys"] = ["MOD%d" % li, "GS_A%d" % li, "GS_B%d" % li]
        P.wait_all("dve", ["GS_A%d" % li, "GS_B%d" % li, "MOD%d" % li])
        self._barrier(["GS_A%d" % li, "GS_B%d" % li, "MOD%d" % li, "modw0", "modw1", kS])
        es.close()
        return d

    def fm_rows(self, dst_fn, src2d, n, wkey, q="sp"):
        for i in range(n):
            self.P.dma(q, lambda e, i=i: e.dma_start(out=dst_fn(i), in_=src2d[i].rearrange("(k p) -> p k", p=128), allow_slow_non_contiguous=True), writes=["%s_r%d" % (wkey, i)])
        return ["%s_r%d" % (wkey, i) for i in range(n)]

    def compute_mods_sharded(self, w_mod, b_mod, c_in, cctx_in, norm_g, nlayers=4):
        cfg, P = self.cfg, self.P
        KC = cfg.KC
        NS = (6 * cfg.D) // 8
        NCH = NS // 128
        mods = []
        for li in range(nlayers):
            MOD = self.sb("MOD%d" % li, [128, 6 * KC, 2], F32)
            d = {"MOD": MOD}
            for nm in ("GS_A", "GS_B"):
                d[nm] = self.sb("%s%d" % (nm, li), [128, KC, 2], F32)
            mods.append(d)
        PART = self.sb("MODPART", [128, nlayers, NCH, 2], F32)
        es = ExitStack()
        S = self.sb("modS", [128, KC, 2], F32, es)
        bm = self.sb("modB", [128, nlayers, NCH], F32, es)
        G = self.sb("modG", [128, nlayers, 2, KC], F32, es)
        CB = 512 if NS % 512 == 0 else NS
        wbuf = [self.sb("modW%d" % i, [128, KC, CB], F32, es) for i in range(2)]
        wcnt = 0
        P.dma("sp", lambda e: e.dma_start(out=S[:, :, 0], in_=c_in.rearrange("(k p) -> p k", p=128), allow_slow_non_contiguous=True), writes=["mSa"])
        P.dma("sp", lambda e: e.dma_start(out=S[:, :, 1], in_=cctx_in.rearrange("(k p) -> p k", p=128), allow_slow_non_contiguous=True), writes=["mSb"])
        P.op("act", lambda e: e.activation(out=S[:], in_=S[:], func=AF.Silu), reads=["mSa", "mSb"], writes=["mS"])
        for li in range(nlayers):
            P.dma("sp", lambda e, li=li: e.dma_start(out=bm[:, li, :], in_=b_mod[li].rearrange("(k p) -> p k", p=128), allow_slow_non_contiguous=True), writes=["mbm%d" % li])
            gk = self.fm_rows(lambda i, li=li: G[:, li, i, :], norm_g[li], 2, "mg%d" % li)
            P.op("dve", lambda e, li=li: e.tensor_copy(out=G[:, li], in_=G[:, li]), reads=gk, writes=["mg%d" % li])
            for blk in range(NS // CB):
                wb = wbuf[wcnt % 2]
                kw = "modw%d" % (wcnt % 2)
                wcnt += 1
                P.dma("sp", lambda e, wb=wb, li=li, blk=blk: e.dma_start(out=wb[:], in_=w_mod[li][:, blk * CB:(blk + 1) * CB].rearrange("(k p) n -> p k n", p=128)), writes=[kw])
                for oi in range(CB // 128):
                    oc = blk * (CB // 128) + oi
                    ps = self.psC[oc % 2]
                    kp = "psC%d" % (oc % 2)
                    def mm(e, wb=wb, oi=oi, ps=ps):
                        r = None
                        for k in range(KC):
                            r = e.matmul(ps[:, 0:2], lhsT=wb[:, k, oi * 128:(oi + 1) * 128], rhs=S[:, k, :], start=(k == 0), stop=(k == KC - 1))
                        return r
                    P.op("pe", mm, reads=[kw, "mS"], writes=[kp])
                    P.op("dve", lambda e, li=li, oc=oc, ps=ps: e.tensor_scalar(out=PART[:, li, oc, :], in0=ps[:, 0:2], scalar1=bm[:, li, oc:oc + 1], scalar2=None, op0=ALU.add),
                         reads=[kp, "mbm%d" % li], writes=["MODPART"])
        n = nlayers * NCH * 2
        GX = exchange(self, PART[:].rearrange("p l c s -> p (l c s)"), n, "mods", "MODPART", es)
        GXv = GX[:].rearrange("p r (l c s) -> p r l c s", l=nlayers, c=NCH)
        for li in range(nlayers):
            d = mods[li]
            MOD = d["MOD"]
            P.op("dve", lambda e, li=li, MOD=MOD: e.tensor_copy(out=MOD[:].rearrange("p (r c) s -> p r c s", r=8), in_=GXv[:, :, li, :, :]), reads=["XG_mods"], writes=["MOD%d" % li])
            for s in range(2):
                P.op("dve", lambda e, s=s, d=d, MOD=MOD, li=li: e.scalar_tensor_tensor(out=d["GS_A"][:, :, s], in0=MOD[:, KC:2 * KC, s], scalar=1.0, in1=G[:, li, 0, :], op0=ALU.add, op1=ALU.mult),
                     reads=["MOD%d" % li, "mg%d" % li], writes=["GS_A%d" % li])
                P.op("dve", lambda e, s=s, d=d, MOD=MOD, li=li: e.scalar_tensor_tensor(out=d["GS_B"][:, :, s], in0=MOD[:, 4 * KC:5 * KC, s], scalar=1.0, in1=G[:, li, 1, :], op0=ALU.add, op1=ALU.mult),
                     reads=["MOD%d" % li, "mg%d" % li], writes=["GS_B%d" % li])
            d["SH_A"] = MOD[:, 0:KC, :]
            d["GT_A"] = MOD[:, 2 * KC:3 * KC, :]
            d["SH_B"] = MOD[:, 3 * KC:4 * KC, :]
            d["GT_B"] = MOD[:, 5 * KC:6 * KC, :]
            d["keys"] = ["MOD%d" % li, "GS_A%d" % li, "GS_B%d" % li]
        self.full_barrier()
        es.close()
        return mods

    def _barrier(self, keys):
        for eng in Prog.ENGS:
            toks = []
            for k in keys:
                toks += self.P.last_write.get(k, []) + self.P.readers.get(k, [])
            self.P._emit_waits(eng, toks)

    def full_barrier(self):
        toks = []
        for e, c in self.P.pcount.items():
            if c:
                toks.append(("ps_" + e, c))
        for sn, c in self.P.dcount.items():
            if c:
                toks.append((sn, c))
        for i in range(self.P.ccount):
            toks.append(("cc%d" % (i + 1), 1))
        for eng in Prog.ENGS:
            self.P._emit_waits(eng, toks)

    def norm_mod(self, GS, SH, modkeys, Hdst=None, extra_cols=None):
        cfg, P = self.cfg, self.P
        KC, T = cfg.KC, cfg.T
        H = self.H if Hdst is None else Hdst
        es = ExitStack()
        sq = [self.sb(self.key("sq"), [128, T], BF16, es) for _ in range(2)]
        tmp = [self.sb(self.key("nt"), [128, T], F32, es) for _ in range(2)]
        tiles = col_tiles(0, T)
        for k in range(KC):
            s = sq[k % 2]
            ks = "sq%d" % (k % 2)
            P.op("act", lambda e, k=k, s=s: e.activation(out=s[:], in_=self.X[:, k, :], func=AF.Square), reads=["X"], writes=[ks])
            def mm(e, k=k, s=s):
                r = None
                for i, (a, b) in enumerate(tiles):
                    r = e.matmul(self.psA[i][:, 0:b - a], lhsT=self.ones16[:], rhs=s[:, a:b], start=(k == 0), stop=(k == KC - 1))
                return r
            P.op("pe", mm, reads=[ks, "ones16"], writes=["psA0", "psA1", "psA2"])
        for i, (a, b) in enumerate(tiles):
            P.op("dve", lambda e, i=i, a=a, b=b: e.tensor_scalar(out=self.RSTD[:, a:b], in0=self.psA[i][:, 0:b - a], scalar1=1.0 / cfg.D, scalar2=EPS, op0=ALU.mult, op1=ALU.add),
                 reads=["psA%d" % i], writes=["RSTD"])
        P.op("act", lambda e: e.activation(out=self.RSTD[:], in_=self.RSTD[:], func=AF.Ln), reads=["RSTD"], writes=["RSTD"])
        P.op("act", lambda e: e.activation(out=self.RSTD[:], in_=self.RSTD[:], func=AF.Exp, scale=-0.5), reads=["RSTD"], writes=["RSTD"])
        for k in range(KC):
            t = tmp[k % 2]
            kt = "nt%d" % (k % 2)
            for (c0, c1, s) in self.segs:
                P.op("dve", lambda e, k=k, t=t, c0=c0, c1=c1, s=s: e.scalar_tensor_tensor(out=t[:, c0:c1], in0=self.X[:, k, c0:c1], scalar=GS[:, k, s:s + 1], in1=self.RSTD[:, c0:c1], op0=ALU.mult, op1=ALU.mult),
                     reads=["X", "RSTD"] + modkeys, writes=[kt + "_%d" % s])
                P.op("act", lambda e, k=k, t=t, c0=c0, c1=c1, s=s: e.activation(out=H[:, k, c0:c1], in_=t[:, c0:c1], func=AF.Identity, bias=SH[:, k, s:s + 1], scale=1.0),
                     reads=[kt + "_%d" % s] + modkeys, writes=["H"])
        self._barrier(["sq0", "sq1", "nt0_0", "nt0_1", "nt1_0", "nt1_1", "H", "RSTD"])
        es.close()

    def swiglu_into_x(self, w1, w3, w2, GT, modkeys, gbc=None, gbc_key=None, tag="ffn", segs=None):
        cfg, P = self.cfg, self.P
        KC, T, FC = cfg.KC, cfg.T, cfg.FC
        G = 2
        tiles = []
        for (c0, c1, s) in (segs or self.segs):
            tiles += [(a, b, s) for (a, b) in col_tiles(c0, c1)]
        es = ExitStack()
        W1 = [self.sb(self.key("W1"), [128, KC, G * 128], BF16, es) for _ in range(2)]
        W3 = [self.sb(self.key("W3"), [128, KC, G * 128], BF16, es) for _ in range(2)]
        W2 = [self.sb(self.key("W2"), [128, G, cfg.D], BF16, es) for _ in range(2)]
        ACT = [self.sb(self.key("ACTB"), [128, G, T], BF16, es) for _ in range(2)]
        S1 = [self.sb(self.key("S1"), [128, 512], F32, es) for _ in range(2)]
        TMPB = [self.sb(self.key("TMPB"), [128, 512], F32, es) for _ in range(2)]
        ng = FC // G
        cnt = 0
        ycnt = 0

        def issue_w(g):
            b = g % 2
            kw = "%s_w%d" % (tag, b)
            P.dma("pool", lambda e, g=g, b=b: e.dma_start(out=W1[b][:], in_=w1[:, g * G * 128:(g + 1) * G * 128].rearrange("(k p) n -> p k n", p=128)), writes=[kw + "1"])
            P.dma("pool", lambda e, g=g, b=b: e.dma_start(out=W3[b][:], in_=w3[:, g * G * 128:(g + 1) * G * 128].rearrange("(k p) n -> p k n", p=128)), writes=[kw + "3"])
            P.dma("pool", lambda e, g=g, b=b: e.dma_start(out=W2[b][:], in_=w2[g * G * 128:(g + 1) * G * 128, :].rearrange("(f p) n -> p f n", p=128)), writes=[kw + "2"])

        issue_w(0)
        for g in range(ng):
            b = g % 2
            kw = "%s_w%d" % (tag, b)
            if g + 1 < ng:
                issue_w(g + 1)
            ka = "%s_act%d" % (tag, b)
            for fi in range(G):
                for (a, bb, s) in tiles:
                    w = bb - a
                    pi = cnt % 2
                    cnt += 1
                    p1, p3 = self.psA[pi], self.psB[pi]
                    k1, k3 = "psA%d" % pi, "psB%d" % pi
                    def mm1(e, b=b, fi=fi, a=a, bb=bb, w=w, p1=p1):
                        r = None
                        for k in range(KC):
                            r = e.matmul(p1[:, 0:w], lhsT=W1[b][:, k, fi * 128:(fi + 1) * 128], rhs=self.H[:, k, a:bb], start=(k == 0), stop=(k == KC - 1))
                        return r
                    def mm3(e, b=b, fi=fi, a=a, bb=bb, w=w, p3=p3):
                        r = None
                        for k in range(KC):
                            r = e.matmul(p3[:, 0:w], lhsT=W3[b][:, k, fi * 128:(fi + 1) * 128], rhs=self.H[:, k, a:bb], start=(k == 0), stop=(k == KC - 1))
                        return r
                    P.op("pe", mm1, reads=[kw + "1", "H"], writes=[k1])
                    P.op("pe", mm3, reads=[kw + "3", "H"], writes=[k3])
                    s1 = S1[pi]
                    ks1 = "%s_s1_%d" % (tag, pi)
                    P.op("act", lambda e, s1=s1, p1=p1, w=w: e.activation(out=s1[:, 0:w], in_=p1[:, 0:w], func=AF.Silu), reads=[k1], writes=[ks1])
                    if gbc is None:
                        P.op("dve", lambda e, b=b, fi=fi, a=a, bb=bb, w=w, s1=s1, p3=p3: e.tensor_tensor(out=ACT[b][:, fi, a:bb], in0=s1[:, 0:w], in1=p3[:, 0:w], op=ALU.mult),
                             reads=[ks1, k3], writes=[ka])
                    else:
                        s2 = s1
                        ks2 = ks1
                        P.op("dve", lambda e, w=w, s1=s1, s2=s2, p3=p3: e.tensor_tensor(out=s2[:, 0:w], in0=s1[:, 0:w], in1=p3[:, 0:w], op=ALU.mult),
                             reads=[ks1, k3], writes=[ks2])
                        P.op("dve", lambda e, b=b, fi=fi, a=a, bb=bb, w=w, s2=s2: e.tensor_tensor(out=ACT[b][:, fi, a:bb], in0=s2[:, 0:w], in1=gbc[:, a:bb], op=ALU.mult),
                             reads=[ks2, gbc_key], writes=[ka])
            for d in range(KC):
                for (a, bb, s) in tiles:
                    w = bb - a
                    ybanks = [(self.psC[0], "psC0"), (self.psC[1], "psC1"), (self.psA[2], "psA2"), (self.psB[2], "psB2")]
                    py, ky = ybanks[ycnt % 4]
                    ycnt += 1
                    def mmy(e, b=b, d=d, a=a, bb=bb, w=w, py=py):
                        r = None
                        for fi in range(G):
                            r = e.matmul(py[:, 0:w], lhsT=W2[b][:, fi, d * 128:(d + 1) * 128], rhs=ACT[b][:, fi, a:bb], start=(fi == 0), stop=(fi == G - 1))
                        return r
                    P.op("pe", mmy, reads=[kw + "2", ka], writes=[ky])
                    xk = "X_%d_%d" % (d, a)
                    if ycnt % 3 == 0:
                        tb = TMPB[(ycnt // 3) % 2]
                        kt = "%s_tmpb%d" % (tag, (ycnt // 3) % 2)
                        P.op("act", lambda e, d=d, w=w, s=s, py=py, tb=tb: e.mul(out=tb[:, 0:w], in_=py[:, 0:w], mul=GT[:, d, s:s + 1]), reads=[ky] + modkeys, writes=[kt])
                        P.op("pool", lambda e, d=d, a=a, bb=bb, w=w, tb=tb: e.tensor_tensor(out=self.X[:, d, a:bb], in0=self.X[:, d, a:bb], in1=tb[:, 0:w], op=ALU.add), reads=[kt, xk], writes=[xk])
                    else:
                        P.op("dve", lambda e, d=d, a=a, bb=bb, w=w, s=s, py=py: e.scalar_tensor_tensor(out=self.X[:, d, a:bb], in0=py[:, 0:w], scalar=GT[:, d, s:s + 1], in1=self.X[:, d, a:bb], op0=ALU.mult, op1=ALU.add),
                             reads=[ky, xk] + modkeys, writes=[xk])
        self.full_barrier()
        es.close()

    def outproj_into_x(self, Wt, nk, rhs_fn, rhs_key, wkey, GT, modkeys, c0, c1, s):
        cfg, P = self.cfg, self.P
        cnt = 0
        for d in range(cfg.KC):
            for (a, bb) in col_tiles(c0, c1):
                w = bb - a
                pi = cnt % 2
                cnt += 1
                py = self.psC[pi]
                ky = "psC%d" % pi
                def mmy(e, d=d, a=a, bb=bb, w=w, py=py):
                    r = None
                    for ic in range(nk):
                        r = e.matmul(py[:, 0:w], lhsT=Wt[:, ic, d * 128:(d + 1) * 128], rhs=rhs_fn(ic, a - c0, bb - c0), start=(ic == 0), stop=(ic == nk - 1))
                    return r
                P.op("pe", mmy, reads=[wkey, rhs_key], writes=[ky])
                P.op("dve", lambda e, d=d, a=a, bb=bb, w=w, py=py: e.scalar_tensor_tensor(out=self.X[:, d, a:bb], in0=py[:, 0:w], scalar=GT[:, d, s:s + 1], in1=self.X[:, d, a:bb], op0=ALU.mult, op1=ALU.add),
                     reads=[ky, "X_%d_%d" % (d, a)] + modkeys, writes=["X_%d_%d" % (d, a)])

    def lru_params(self, j, prm):
        cfg, P = self.cfg, self.P
        KC = cfg.KC
        d = {}
        d["CW"] = self.sb("lruCW%d" % j, [128, 4, KC], F32)
        d["CB"] = self.sb("lruCB%d" % j, [128, KC], F32)
        d["BA"] = self.sb("lruBA%d" % j, [128, 2, KC], F32)
        d["BX"] = self.sb("lruBX%d" % j, [128, 2, KC], F32)
        d["C1"] = self.sb("lruC1%d" % j, [128, 2, KC], F32)
        d["C2"] = self.sb("lruC2%d" % j, [128, 2, KC], F32)
        with self.nc.allow_non_contiguous_dma(reason="tiny"):
            k_ = self.fm_rows(lambda i: d["CW"][:, i, :], prm["lru_conv_w"], 4, "lruprm%d_cw" % j)
            P.op("dve", lambda e: e.tensor_copy(out=d["CW"][:], in_=d["CW"][:]), reads=k_, writes=["lruprm%d_" % j + "cw"])
            P.dma("sp", lambda e: e.dma_start(out=d["CB"][:], in_=prm["lru_conv_b"].rearrange("(k p) -> p k", p=128), allow_slow_non_contiguous=True), writes=["lruprm%d_" % j + "cb"])
            k_ = self.fm_rows(lambda i: d["BA"][:, i, :], prm["lru_b_a"], 2, "lruprm%d_" % j + "ba")
            P.op("dve", lambda e: e.tensor_copy(out=d["BA"][:], in_=d["BA"][:]), reads=k_, writes=["lruprm%d_" % j + "ba"])
            k_ = self.fm_rows(lambda i: d["BX"][:, i, :], prm["lru_b_x"], 2, "lruprm%d_" % j + "bx")
            P.op("dve", lambda e: e.tensor_copy(out=d["BX"][:], in_=d["BX"][:]), reads=k_, writes=["lruprm%d_" % j + "bx"])
            k_ = self.fm_rows(lambda i: d["C1"][:, i, :], prm["lru_lambda"], 2, "lruprm%d_" % j + "c1")
            P.op("dve", lambda e: e.tensor_copy(out=d["C1"][:], in_=d["C1"][:]), reads=k_, writes=["lruprm%d_" % j + "c1"])
        P.op("act", lambda e: e.activation(out=d["C1"][:], in_=d["C1"][:], func=AF.Exp, scale=-1.0), reads=["lruprm%d_" % j + "c1"], writes=["lruprm%d_" % j + "c1"])
        P.op("act", lambda e: e.activation(out=d["C1"][:], in_=d["C1"][:], func=AF.Ln, bias=1.0, scale=1.0), reads=["lruprm%d_" % j + "c1"], writes=["lruprm%d_" % j + "c1"])
        P.op("dve", lambda e: e.tensor_scalar(out=d["C2"][:], in0=d["C1"][:], scalar1=-2.0 * LRU_C, scalar2=None, op0=ALU.mult), reads=["lruprm%d_" % j + "c1"], writes=["lruprm%d_" % j + "c2"])
        P.op("dve", lambda e: e.tensor_scalar(out=d["C1"][:], in0=d["C1"][:], scalar1=-LRU_C, scalar2=None, op0=ALU.mult), reads=["lruprm%d_" % j + "c1", "lruprm%d_" % j + "c2"], writes=["lruprm%d_" % j + "c1"])
        d["keys"] = ["lruprm%d_" % j + "cw", "lruprm%d_" % j + "cb", "lruprm%d_" % j + "ba", "lruprm%d_" % j + "bx", "lruprm%d_" % j + "c1", "lruprm%d_" % j + "c2"]
        return d

    def lru_pass(self, prm, lp, mods, c0, c1, is_ctx, phase, HH=None, EF=None, CARRY=None, CFIN=None, PSOUT=None, do_out=True, SPILL=None):
        cfg, P = self.cfg, self.P
        KC, HC = cfg.KC, cfg.HC
        W = c1 - c0
        s = 1 if is_ctx else 0
        es = ExitStack()
        WX = self.sb(self.key("WX"), [128, KC, HC * 128], BF16, es)
        WGt = self.sb(self.key("WG"), [128, 2, 2, HC, HC * 128], BF16, es)
        XB = self.sb(self.key("XB"), [128, HC, W + 3], F32, es)
        XC = self.sb(self.key("XC"), [128, HC, W], F32, es)
        XCb = self.sb(self.key("XCb"), [128, HC, W], BF16, es)
        R = self.sb(self.key("R"), [128, W], F32, es)
        A = self.sb(self.key("A"), [128, W], F32, es)
        A2 = self.sb(self.key("A2"), [128, W], F32, es)
        I = R
        HS = self.sb(self.key("HS"), [128, 2, W], F32, es)
        SR = self.sb(self.key("SR"), [128, 1], F32, es)
        need_out = do_out and phase == 2
        use_spill_load = (phase == 2 and not is_ctx and SPILL is not None)
        if need_out:
            WO = self.sb(self.key("WO"), [128, HC, cfg.D], BF16, es)
            GG = self.sb(self.key("GG"), [128, HC, W], BF16, es)
            YG = self.sb(self.key("YG"), [128, HC, W], BF16, es)
        tiles = col_tiles(0, W)
        w_in, w_a, w_x, w_out = prm["lru_w_in"], prm["lru_w_a"], prm["lru_w_x"], prm["lru_w_out"]
        lk = lp["keys"]
        for hd in range(cfg.HEADS):
            hcol = hd * HC * 128
            if not use_spill_load:
                P.dma("pool", lambda e, hcol=hcol: e.dma_start(out=WX[:], in_=w_in[:, hcol:hcol + HC * 128].rearrange("(k p) n -> p k n", p=128)), writes=["WX"])
                for dr in range(2):
                    P.dma("pool", lambda e, dr=dr, hd=hd: e.dma_start(out=WGt[:, dr, 0], in_=w_a[dr, hd].rearrange("(i p) n -> p i n", p=128)), writes=["WG%d0" % dr])
                    P.dma("pool", lambda e, dr=dr, hd=hd: e.dma_start(out=WGt[:, dr, 1], in_=w_x[dr, hd].rearrange("(i p) n -> p i n", p=128)), writes=["WG%d1" % dr])
            for oc in (range(HC) if not use_spill_load else ()):
                ch = hd * HC + oc
                for ti, (a, b) in enumerate(tiles):
                    ps = self.psA[ti % 3]
                    kp = "psA%d" % (ti % 3)
                    def mm(e, oc=oc, a=a, b=b, ps=ps):
                        r = None
                        for k in range(KC):
                            r = e.matmul(ps[:, 0:b - a], lhsT=WX[:, k, oc * 128:(oc + 1) * 128], rhs=self.H[:, k, c0 + a:c0 + b], start=(k == 0), stop=(k == KC - 1))
                        return r
                    P.op("pe", mm, reads=["WX", "H"], writes=[kp])
                    P.op("act", lambda e, oc=oc, a=a, b=b, ps=ps: e.copy(out=XB[:, oc, 2 + a:2 + b], in_=ps[:, 0:b - a]), reads=[kp], writes=["XB"])
                if is_ctx:
                    P.op("dve", lambda e, oc=oc: e.memset(XB[:, oc, 0:2], 0.0), writes=["XB"])
                    P.op("dve", lambda e, oc=oc: e.memset(XB[:, oc, W + 2:W + 3], 0.0), writes=["XB"])
                else:
                    ps = self.psB[0]
                    def mmh(e, oc=oc, ps=ps):
                        r = None
                        for k in range(KC):
                            r = e.matmul(ps[:, 0:3], lhsT=WX[:, k, oc * 128:(oc + 1) * 128], rhs=HH[:, k, :], start=(k == 0), stop=(k == KC - 1))
                        return r
                    P.op("pe", mmh, reads=["WX", "HH"], writes=["psB0"])
                    P.op("dve", lambda e, oc=oc, ps=ps: e.tensor_scalar(out=XB[:, oc, 0:2], in0=ps[:, 0:2], scalar1=EF[:, 0:1], scalar2=None, op0=ALU.mult), reads=["psB0", "EF"], writes=["XB"])
                    P.op("dve", lambda e, oc=oc, ps=ps: e.tensor_scalar(out=XB[:, oc, W + 2:W + 3], in0=ps[:, 2:3], scalar1=EF[:, 1:2], scalar2=None, op0=ALU.mult), reads=["psB0", "EF"], writes=["XB"])
                P.op("dve", lambda e, oc=oc, ch=ch: e.tensor_scalar(out=XC[:, oc, :], in0=XB[:, oc, 0:W], scalar1=lp["CW"][:, 0, ch:ch + 1], scalar2=lp["CB"][:, ch:ch + 1], op0=ALU.mult, op1=ALU.add),
                     reads=["XB"] + lk, writes=["XC"])
                for kk in range(1, 4):
                    P.op("dve", lambda e, oc=oc, ch=ch, kk=kk: e.scalar_tensor_tensor(out=XC[:, oc, :], in0=XB[:, oc, kk:kk + W], scalar=lp["CW"][:, kk, ch:ch + 1], in1=XC[:, oc, :], op0=ALU.mult, op1=ALU.add),
                         reads=["XB", "XC"] + lk, writes=["XC"])
                P.op("act", lambda e, oc=oc: e.copy(out=XCb[:, oc, :], in_=XC[:, oc, :]), reads=["XC"], writes=["XCb"])
            if need_out:
                P.dma("pool", lambda e, hcol=hcol: e.dma_start(out=WX[:], in_=w_in[:, cfg.D + hcol:cfg.D + hcol + HC * 128].rearrange("(k p) n -> p k n", p=128)), writes=["WX"])
                P.dma("pool", lambda e, hcol=hcol: e.dma_start(out=WO[:], in_=w_out[hcol:hcol + HC * 128, :].rearrange("(i p) n -> p i n", p=128)), writes=["WO"])
                for oc in range(HC):
                    for ti, (a, b) in enumerate(tiles):
                        ps = self.psA[ti % 3]
                        kp = "psA%d" % (ti % 3)
                        def mmg(e, oc=oc, a=a, b=b, ps=ps):
                            r = None
                            for k in range(KC):
                                r = e.matmul(ps[:, 0:b - a], lhsT=WX[:, k, oc * 128:(oc + 1) * 128], rhs=self.H[:, k, c0 + a:c0 + b], start=(k == 0), stop=(k == KC - 1))
                            return r
                        P.op("pe", mmg, reads=["WX", "H"], writes=[kp])
                        P.op("act", lambda e, oc=oc, a=a, b=b, ps=ps: e.activation(out=GG[:, oc, a:b], in_=ps[:, 0:b - a], func=AF.Gelu_apprx_tanh), reads=[kp], writes=["GG"])
            for oc in range(HC):
                ch = hd * HC + oc
                for dr in range(2):
                    si = (ch * 2 + dr) * 2
                    if use_spill_load:
                        P.dma("sp", lambda e, si=si: e.dma_start(out=A[:], in_=SPILL[si]), reads=["spill%d" % si], writes=["A"])
                        P.dma("sp", lambda e, si=si: e.dma_start(out=A2[:], in_=SPILL[si + 1]), reads=["spill%d" % (si + 1)], writes=["A2"])
                    else:
                        def gate_mm(gi, oc=oc, dr=dr, ch=ch):
                            for ti, (a, b) in enumerate(tiles):
                                ps = self.psB[ti % 3]
                                kp = "psB%d" % (ti % 3)
                                def mmz(e, oc=oc, dr=dr, gi=gi, a=a, b=b, ps=ps):
                                    r = None
                                    for ic in range(HC):
                                        r = e.matmul(ps[:, 0:b - a], lhsT=WGt[:, dr, gi, ic, oc * 128:(oc + 1) * 128], rhs=XCb[:, ic, a:b], start=(ic == 0), stop=(ic == HC - 1))
                                    return r
                                P.op("pe", mmz, reads=["WG%d%d" % (dr, gi), "XCb"], writes=[kp])
                                bias = lp["BA"] if gi == 0 else lp["BX"]
                                P.op("act", lambda e, ch=ch, dr=dr, a=a, b=b, ps=ps, bias=bias: e.activation(out=R[:, a:b], in_=ps[:, 0:b - a], func=AF.Sigmoid, bias=bias[:, dr, ch:ch + 1], scale=1.0), reads=[kp] + lk, writes=["R"])
                        gate_mm(0)
                        P.op("act", lambda e, ch=ch, dr=dr: e.activation(out=A[:], in_=R[:], func=AF.Exp, scale=lp["C1"][:, dr, ch:ch + 1]), reads=["R"] + lk, writes=["A"])
                        P.op("act", lambda e, ch=ch, dr=dr: e.activation(out=A2[:], in_=R[:], func=AF.Exp, scale=lp["C2"][:, dr, ch:ch + 1]), reads=["R"] + lk, writes=["A2"])
                        if phase == 1:
                            P.op("dve", lambda e: e.reduce_sum(out=SR[:], in_=R[:], axis=AX.X), reads=["R"], writes=["SR"])
                            P.op("act", lambda e, dr=dr, ch=ch: e.activation(out=PSOUT[:, dr, 0, ch:ch + 1], in_=SR[:], func=AF.Exp, scale=lp["C1"][:, dr, ch:ch + 1]), reads=["SR"] + lk, writes=["PSOUT"])
                        gate_mm(1)
                        P.op("act", lambda e: e.activation(out=A2[:], in_=A2[:], func=AF.Ln, bias=1.0, scale=-1.0), reads=["A2"], writes=["A2"])
                        P.op("act", lambda e: e.activation(out=A2[:], in_=A2[:], func=AF.Exp, scale=0.5), reads=["A2"], writes=["A2"])
                        P.op("dve", lambda e: e.tensor_tensor(out=A2[:], in0=A2[:], in1=I[:], op=ALU.mult), reads=["A2", "R"], writes=["A2"])
                        P.op("dve", lambda e, oc=oc: e.tensor_tensor(out=A2[:], in0=A2[:], in1=XC[:, oc, :], op=ALU.mult), reads=["A2", "XC"], writes=["A2"])
                        if phase == 1 and SPILL is not None:
                            P.dma("sp", lambda e, si=si: e.dma_start(out=SPILL[si], in_=A[:]), reads=["A"], writes=["spill%d" % si])
                            P.dma("sp", lambda e, si=si: e.dma_start(out=SPILL[si + 1], in_=A2[:]), reads=["A2"], writes=["spill%d" % (si + 1)])
                    if phase == 1:
                        init = 0.0
                        rk = []
                    elif is_ctx:
                        init = 0.0
                        rk = []
                    else:
                        init = CARRY[:, dr, ch:ch + 1]
                        rk = ["CARRY"]
                    if dr == 0:
                        P.op("dve", lambda e, init=init: e.tensor_tensor_scan(out=HS[:, 0, :], data0=A[:], data1=A2[:], initial=init, op0=ALU.mult, op1=ALU.add), reads=["A", "A2"] + rk, writes=["HS0"])
                    else:
                        P.op("dve", lambda e, init=init: e.tensor_tensor_scan(out=HS[:, 1, ::-1], data0=A[:, ::-1], data1=A2[:, ::-1], initial=init, op0=ALU.mult, op1=ALU.add), reads=["A", "A2"] + rk, writes=["HS1"])
                    fin = (W - 1) if dr == 0 else 0
                    if phase == 1:
                        P.op("dve", lambda e, dr=dr, ch=ch, fin=fin: e.tensor_copy(out=PSOUT[:, dr, 1, ch:ch + 1], in_=HS[:, dr, fin:fin + 1]), reads=["HS%d" % dr], writes=["PSOUT"])
                    elif is_ctx:
                        P.op("dve", lambda e, dr=dr, ch=ch, fin=fin: e.tensor_copy(out=CFIN[:, dr, ch:ch + 1], in_=HS[:, dr, fin:fin + 1]), reads=["HS%d" % dr], writes=["CFIN"])
                if need_out:
                    P.op("dve", lambda e: e.tensor_tensor(out=HS[:, 0, :], in0=HS[:, 0, :], in1=HS[:, 1, :], op=ALU.add), reads=["HS0", "HS1"], writes=["HS0"])
                    P.op("dve", lambda e, oc=oc: e.tensor_tensor(out=YG[:, oc, :], in0=HS[:, 0, :], in1=GG[:, oc, :], op=ALU.mult), reads=["HS0", "GG"], writes=["YG"])
            if need_out:
                self.outproj_into_x(WO, HC, lambda ic, a, b: YG[:, ic, a:b], "YG", "WO", mods["GT_A"], mods["keys"], c0, c1, s)
        self.full_barrier()
        es.close()

    def sgu_params(self, prm, pstack):
        cfg, P = self.cfg, self.P
        KC, SG = cfg.KC, cfg.SGROUPS
        d = {}
        d["WST"] = self.sb(self.key("sguWST"), [128, SG, 128], BF16, pstack)
        d["LG"] = self.sb(self.key("sguLG"), [128, KC], F32, pstack)
        d["CC"] = self.sb(self.key("sguCC"), [128, KC, 128], F32, pstack)
        es = ExitStack()
        WSn = self.sb(self.key("sguWSn"), [128, SG, 128], F32, es)
        LB = self.sb(self.key("sguLB"), [128, KC], F32, es)
        BS = self.sb(self.key("sguBS"), [128, SG, 128], F32, es)
        P.dma("sp", lambda e: e.dma_start(out=WSn[:], in_=prm["sgu_w_s"].rearrange("g p q -> p g q")), writes=["sguWSn"])
        with self.nc.allow_non_contiguous_dma(reason="tiny"):
            P.dma("sp", lambda e: e.dma_start(out=d["LG"][:], in_=prm["sgu_ln_g"].rearrange("(k p) -> p k", p=128), allow_slow_non_contiguous=True), writes=["sguLG"])
            P.dma("sp", lambda e: e.dma_start(out=LB[:], in_=prm["sgu_ln_b"].rearrange("(k p) -> p k", p=128), allow_slow_non_contiguous=True), writes=["sguLB"])
        bsrc = prm["sgu_b_s"]
        bs_bc = bass.AP(bsrc.tensor, bsrc.offset, [[0, 128], [1, SG * 128]])
        P.dma("sp", lambda e: e.dma_start(out=BS[:].rearrange("p g q -> p (g q)"), in_=bs_bc), writes=["sguBS"])
        for g in range(SG):
            ps = self.psA[g % 2]
            kp = "psA%d" % (g % 2)
            P.op("pe", lambda e, g=g, ps=ps: e.transpose(ps[:, 0:128], WSn[:, g, :], self.ident[:]), reads=["sguWSn", "ident"], writes=[kp])
            P.op("act", lambda e, g=g, ps=ps: e.copy(out=d["WST"][:, g, :], in_=ps[:, 0:128]), reads=[kp], writes=["sguWST"])
            ps2 = self.psB[g % 2]
            kp2 = "psB%d" % (g % 2)
            P.op("pe", lambda e, g=g, ps2=ps2: e.matmul(ps2[:, 0:128], lhsT=self.ones16[:], rhs=d["WST"][:, g, :], start=True, stop=True), reads=["sguWST", "ones16"], writes=[kp2])
            for cc in range(cfg.GCH):
                ch = g * cfg.GCH + cc
                P.op("dve", lambda e, g=g, ch=ch, ps2=ps2: e.scalar_tensor_tensor(out=d["CC"][:, ch, :], in0=ps2[:, 0:128], scalar=LB[:, ch:ch + 1], in1=BS[:, g, :], op0=ALU.mult, op1=ALU.add),
                     reads=[kp2, "sguLB", "sguBS"], writes=["sguCC"])
        d["keys"] = ["sguWST", "sguLG", "sguCC"]
        self.full_barrier()
        es.close()
        return d

    def sgu_pass(self, prm, sp, mods, c0, c1, is_ctx):
        cfg, P = self.cfg, self.P
        KC, GCH, D = cfg.KC, cfg.GCH, cfg.D
        W = c1 - c0
        NT = W // 128
        s = 1 if is_ctx else 0
        w_in, w_out = prm["sgu_w_in"], prm["sgu_w_out"]
        es = ExitStack()
        VT = self.sb(self.key("VT"), [128, NT, D], BF16, es)
        VW = GCH * 128
        WV = [self.sb(self.key("WV"), [128, KC, VW], BF16, es) for _ in range(2)]
        SQ = self.sb(self.key("SQ"), [128, D], BF16, es)
        ST = self.sb(self.key("ST"), [128, NT, 4], F32, es)
        WU = WV[0]
        WO = WV[1][:].rearrange("p k n -> p (k n)").rearrange("p (i n) -> p i n", i=GCH)
        U = self.sb(self.key("U"), [128, GCH, W], BF16, es)
        VM = self.sb(self.key("VM"), [128, 512], F32, es)
        if GCH * W <= D:
            PR = SQ[:, 0:GCH * W].rearrange("p (c w) -> p c w", c=GCH)
        else:
            PR = self.sb(self.key("PR"), [128, GCH, W], BF16, es)
        for vt in range(D // VW):
            wb = WV[vt % 2]
            kw = "WV%d" % (vt % 2)
            P.dma("pool", lambda e, vt=vt, wb=wb: e.dma_start(out=wb[:], in_=w_in[:, D + vt * VW:D + (vt + 1) * VW].rearrange("(k p) n -> p k n", p=128)), writes=[kw])
            for n in range(NT):
                ps = self.psA[n % 3]
                kp = "psA%d" % (n % 3)
                def mmv(e, n=n, wb=wb, ps=ps):
                    r = None
                    for k in range(KC):
                        r = e.matmul(ps[:, 0:VW], lhsT=self.H[:, k, c0 + n * 128:c0 + (n + 1) * 128], rhs=wb[:, k, :], start=(k == 0), stop=(k == KC - 1))
                    return r
                P.op("pe", mmv, reads=[kw, "H"], writes=[kp])
                P.op("act", lambda e, n=n, vt=vt, ps=ps: e.activation(out=VT[:, n, vt * VW:(vt + 1) * VW], in_=ps[:, 0:VW], func=AF.Gelu_apprx_tanh), reads=[kp], writes=["VT"])
        for n in range(NT):
            P.op("dve", lambda e, n=n: e.reduce_sum(out=ST[:, n, 0:1], in_=VT[:, n, :], axis=AX.X), reads=["VT"], writes=["ST"])
            P.op("dve", lambda e, n=n: e.tensor_tensor(out=SQ[:], in0=VT[:, n, :], in1=VT[:, n, :], op=ALU.mult), reads=["VT"], writes=["SQ"])
            P.op("dve", lambda e, n=n: e.reduce_sum(out=ST[:, n, 1:2], in_=SQ[:], axis=AX.X), reads=["SQ"], writes=["ST"])
        P.op("dve", lambda e: e.tensor_scalar(out=ST[:, :, 2:3], in0=ST[:, :, 0:1], scalar1=1.0 / D, scalar2=None, op0=ALU.mult), reads=["ST"], writes=["ST"])
        P.op("dve", lambda e: e.tensor_tensor(out=ST[:, :, 3:4], in0=ST[:, :, 2:3], in1=ST[:, :, 2:3], op=ALU.mult), reads=["ST"], writes=["ST"])
        P.op("dve", lambda e: e.scalar_tensor_tensor(out=ST[:, :, 3:4], in0=ST[:, :, 1:2], scalar=1.0 / D, in1=ST[:, :, 3:4], op0=ALU.mult, op1=ALU.subtract), reads=["ST"], writes=["ST"])
        P.op("dve", lambda e: e.tensor_scalar(out=ST[:, :, 3:4], in0=ST[:, :, 3:4], scalar1=EPS, scalar2=None, op0=ALU.add), reads=["ST"], writes=["ST"])
        P.op("act", lambda e: e.activation(out=ST[:, :, 3:4], in_=ST[:, :, 3:4], func=AF.Ln), reads=["ST"], writes=["ST"])
        P.op("act", lambda e: e.activation(out=ST[:, :, 3:4], in_=ST[:, :, 3:4], func=AF.Exp, scale=-0.5), reads=["ST"], writes=["ST"])
        for n in range(NT):
            P.op("dve", lambda e, n=n: e.tensor_scalar(out=VT[:, n, :], in0=VT[:, n, :], scalar1=ST[:, n, 2:3], scalar2=ST[:, n, 3:4], op0=ALU.subtract, op1=ALU.mult), reads=["VT", "ST"], writes=["VT"])
        NB = 4
        for g in range(cfg.SGROUPS):
            gcol = g * GCH * 128
            P.dma("pool", lambda e, gcol=gcol: e.dma_start(out=WU[:], in_=w_in[:, gcol:gcol + GCH * 128].rearrange("(k p) n -> p k n", p=128)), writes=["WV0"])
            P.dma("pool", lambda e, gcol=gcol: e.dma_start(out=WO, in_=w_out[gcol:gcol + GCH * 128, :].rearrange("(i p) n -> p i n", p=128)), writes=["WV1"])
            for cc in range(GCH):
                ch = g * GCH + cc
                for ti, (a, b) in enumerate(col_tiles(0, W)):
                    ps = self.psA[ti % 3]
                    kp = "psA%d" % (ti % 3)
                    def mmu(e, cc=cc, a=a, b=b, ps=ps):
                        r = None
                        for k in range(KC):
                            r = e.matmul(ps[:, 0:b - a], lhsT=WU[:, k, cc * 128:(cc + 1) * 128], rhs=self.H[:, k, c0 + a:c0 + b], start=(k == 0), stop=(k == KC - 1))
                        return r
                    P.op("pe", mmu, reads=["WV0", "H"], writes=[kp])
                    P.op("act", lambda e, cc=cc, a=a, b=b, ps=ps: e.activation(out=U[:, cc, a:b], in_=ps[:, 0:b - a], func=AF.Gelu_apprx_tanh), reads=[kp], writes=["U"])
                for n0 in range(0, NT, NB):
                    nb = min(NB, NT - n0)
                    pi = (n0 // NB) % 3
                    ps = self.psB[pi]
                    kp = "psB%d" % pi
                    def mms(e, g=g, ch=ch, n0=n0, nb=nb, ps=ps):
                        r = None
                        for i in range(nb):
                            r = e.matmul(ps[:, i * 128:(i + 1) * 128], lhsT=VT[:, n0 + i, ch * 128:(ch + 1) * 128], rhs=sp["WST"][:, g, :], start=True, stop=True)
                        return r
                    P.op("pe", mms, reads=["VT"] + sp["keys"], writes=[kp])
                    for i in range(nb):
                        P.op("dve", lambda e, ch=ch, i=i, ps=ps: e.scalar_tensor_tensor(out=VM[:, i * 128:(i + 1) * 128], in0=ps[:, i * 128:(i + 1) * 128], scalar=sp["LG"][:, ch:ch + 1], in1=sp["CC"][:, ch, :], op0=ALU.mult, op1=ALU.add),
                             reads=[kp] + sp["keys"], writes=["VM"])
                    P.op("dve", lambda e, cc=cc, n0=n0, nb=nb: e.tensor_tensor(out=PR[:, cc, n0 * 128:(n0 + nb) * 128], in0=VM[:, 0:nb * 128], in1=U[:, cc, n0 * 128:(n0 + nb) * 128], op=ALU.mult),
                         reads=["VM", "U", "SQ"], writes=["PR", "SQ"])
            self.outproj_into_x(WO, GCH, lambda ic, a, b: PR[:, ic, a:b], "PR", "WV1", mods["GT_A"], mods["keys"], c0, c1, s)
        self.full_barrier()
        es.close()

    def moe_gates(self, router, mods, GATE=None):
        cfg, P = self.cfg, self.P
        KC, T, NE = cfg.KC, cfg.T, cfg.NEXP
        NTC = T // 128
        if GATE is None:
            GATE = self.sb("GATE", [128, NTC, NE], F32)
        es = ExitStack()
        Rt = self.sb(self.key("Rt"), [128, KC, NE], F32, es)
        RG = self.sb(self.key("RG"), [128, 2, KC, NE], F32, es)
        RS = self.sb(self.key("RS"), [128, 2, KC, NE], F32, es)
        CONST = self.sb(self.key("CONST"), [128, 2, NE], F32, es)
        RT = self.sb(self.key("RTK"), [128, NTC], F32, es)
        L = self.sb(self.key("L"), [128, NTC, NE], F32, es)
        L2 = self.sb(self.key("L2"), [128, NTC, NE], F32, es)
        E1 = self.sb(self.key("E1"), [128, NTC, NE], F32, es)
        E2 = self.sb(self.key("E2"), [128, NTC, NE], F32, es)
        M = self.sb(self.key("M"), [128, 4, NTC], F32, es)
        P.dma("sp", lambda e: e.dma_start(out=Rt[:], in_=router.rearrange("(k p) n -> p k n", p=128)), writes=["Rt"])
        for s in range(2):
            for k in range(KC):
                P.op("dve", lambda e, s=s, k=k: e.tensor_scalar(out=RG[:, s, k, :], in0=Rt[:, k, :], scalar1=mods["GS_B"][:, k, s:s + 1], scalar2=None, op0=ALU.mult), reads=["Rt"] + mods["keys"], writes=["RG"])
                P.op("dve", lambda e, s=s, k=k: e.tensor_scalar(out=RS[:, s, k, :], in0=Rt[:, k, :], scalar1=mods["SH_B"][:, k, s:s + 1], scalar2=None, op0=ALU.mult), reads=["Rt"] + mods["keys"], writes=["RS"])
            ps = self.psC[s]
            def mmc(e, s=s, ps=ps):
                r = None
                for k in range(KC):
                    r = e.matmul(ps[:, 0:NE], lhsT=self.ones32[:], rhs=RS[:, s, k, :], start=(k == 0), stop=(k == KC - 1))
                return r
            P.op("pe", mmc, reads=["RS", "ones32"], writes=["psC%d" % s])
            P.op("dve", lambda e, s=s, ps=ps: e.tensor_copy(out=CONST[:, s, :], in_=ps[:, 0:NE]), reads=["psC%d" % s], writes=["CONST"])
        for n in range(NTC):
            s = 1 if n * 128 < cfg.NCX else 0
            ps = self.psA[n % 3]
            kp = "psA%d" % (n % 3)
            def mml(e, n=n, s=s, ps=ps):
                r = None
                for k in range(KC):
                    r = e.matmul(ps[:, 0:NE], lhsT=self.X[:, k, n * 128:(n + 1) * 128], rhs=RG[:, s, k, :], start=(k == 0), stop=(k == KC - 1))
                r = e.matmul(ps[:, 16:17], lhsT=self.RSTD[0:1, n * 128:(n + 1) * 128], rhs=self.ones32[0:1, 0:1], start=True, stop=True)
                return r
            P.op("pe", mml, reads=["X", "RG", "RSTD", "ones32"], writes=[kp])
            P.op("dve", lambda e, n=n, ps=ps: e.tensor_copy(out=RT[:, n:n + 1], in_=ps[:, 16:17]), reads=[kp], writes=["RTK"])
            P.op("dve", lambda e, n=n, s=s, ps=ps: e.scalar_tensor_tensor(out=L[:, n, :], in0=ps[:, 0:NE], scalar=RT[:, n:n + 1], in1=CONST[:, s, :], op0=ALU.mult, op1=ALU.add), reads=[kp, "RTK", "CONST"], writes=["L"])
        P.op("dve", lambda e: e.reduce_max(out=M[:, 0, :], in_=L[:], axis=AX.X), reads=["L"], writes=["M0"])
        P.op("dve", lambda e: e.tensor_tensor(out=E1[:], in0=L[:], in1=M[:, 0, :].unsqueeze(2).to_broadcast([128, NTC, NE]), op=ALU.is_equal), reads=["L", "M0"], writes=["E1"])
        P.op("dve", lambda e: e.scalar_tensor_tensor(out=L2[:], in0=E1[:], scalar=-1e30, in1=L[:], op0=ALU.mult, op1=ALU.add), reads=["E1", "L"], writes=["L2"])
        P.op("dve", lambda e: e.reduce_max(out=M[:, 1, :], in_=L2[:], axis=AX.X), reads=["L2"], writes=["M1"])
        P.op("dve", lambda e: e.tensor_tensor(out=E2[:], in0=L2[:], in1=M[:, 1, :].unsqueeze(2).to_broadcast([128, NTC, NE]), op=ALU.is_equal), reads=["L2", "M1"], writes=["E2"])
        P.op("dve", lambda e: e.tensor_tensor(out=M[:, 2, :], in0=M[:, 1, :], in1=M[:, 0, :], op=ALU.subtract), reads=["M0", "M1"], writes=["M2"])
        P.op("act", lambda e: e.activation(out=M[:, 3, :], in_=M[:, 2, :], func=AF.Exp), reads=["M2"], writes=["M3"])
        P.op("dve", lambda e: e.tensor_scalar(out=M[:, 2, :], in0=M[:, 3, :], scalar1=1.0, scalar2=None, op0=ALU.add), reads=["M3"], writes=["M2"])
        P.op("dve", lambda e: e.reciprocal(out=M[:, 2, :], in_=M[:, 2, :]), reads=["M2"], writes=["M2"])
        P.op("dve", lambda e: e.tensor_tensor(out=M[:, 3, :], in0=M[:, 3, :], in1=M[:, 2, :], op=ALU.mult), reads=["M2", "M3"], writes=["M3"])
        P.op("dve", lambda e: e.tensor_tensor(out=E1[:], in0=E1[:], in1=M[:, 2, :].unsqueeze(2).to_broadcast([128, NTC, NE]), op=ALU.mult), reads=["E1", "M2"], writes=["E1"])
        P.op("dve", lambda e: e.tensor_tensor(out=E2[:], in0=E2[:], in1=M[:, 3, :].unsqueeze(2).to_broadcast([128, NTC, NE]), op=ALU.mult), reads=["E2", "M3"], writes=["E2"])
        P.op("dve", lambda e: e.tensor_tensor(out=GATE[:], in0=E1[:], in1=E2[:], op=ALU.add), reads=["E1", "E2"], writes=["GATE"])
        self.full_barrier()
        es.close()
        return GATE

    def gate_bcast(self, GATE, ex, GBC, DG):
        cfg, P = self.cfg, self.P
        NTC = cfg.T // 128
        for n0 in range(0, NTC, 4):
            nb = min(4, NTC - n0)
            pi = (n0 // 4) % 3
            ps = self.psB[pi]
            kp = "psB%d" % pi
            for i in range(nb):
                P.op("dve", lambda e, i=i, n0=n0: e.tensor_scalar(out=DG[:, i, :], in0=self.ident[:], scalar1=GATE[:, n0 + i, ex:ex + 1], scalar2=None, op0=ALU.mult), reads=["ident", "GATE"], writes=["DG%d" % i])
            def mmb(e, nb=nb, ps=ps):
                r = None
                for i in range(nb):
                    r = e.matmul(ps[:, i * 128:(i + 1) * 128], lhsT=self.ones32[:], rhs=DG[:, i, :], start=True, stop=True)
                return r
            P.op("pe", mmb, reads=["ones32"] + ["DG%d" % i for i in range(nb)], writes=[kp])
            P.op("act", lambda e, n0=n0, nb=nb, ps=ps: e.copy(out=GBC[:, n0 * 128:(n0 + nb) * 128], in_=ps[:, 0:nb * 128]), reads=[kp], writes=["GBC"])

    def finish(self):
        with self.nc.Block() as block:
            self.P.replay(block)
        self.es.close()
        return self.nc


LRU_KEYS = ["lru_w_in", "lru_conv_w", "lru_conv_b", "lru_w_a", "lru_b_a", "lru_w_x", "lru_b_x", "lru_lambda", "lru_w_out"]


def build_stage(cfg, stage):
    B = Builder(cfg, stage)
    nc, P = B.nc, B.P
    D, KC, T, NL, NCX, F = cfg.D, cfg.KC, cfg.T, cfg.NL, cfg.NCX, cfg.F
    HD = D // cfg.HEADS
    xin = B.din("xin", [NL, D])
    xhalo = B.din("xhalo", [3, D])
    cin = B.din("cin", [NCX, D])
    ef = B.din("ef", [128, 2])
    c_v = B.din("c", [D])
    cctx_v = B.din("c_ctx", [D])
    li0 = 0 if stage in (1, 2) else 2
    layers = [li0] if stage in (1, 3) else [li0, li0 + 1]
    w_mod = {li: B.din("w_mod%d" % li, [D, 6 * D]) for li in layers}
    b_mod = {li: B.din("b_mod%d" % li, [6 * D]) for li in layers}
    norm_g = {li: B.din("norm_g%d" % li, [2, D]) for li in layers}
    prm = {}
    shapes = {"lru_w_in": [D, 2 * D], "lru_conv_w": [4, D], "lru_conv_b": [D], "lru_w_a": [2, cfg.HEADS, HD, HD], "lru_b_a": [2, D],
              "lru_w_x": [2, cfg.HEADS, HD, HD], "lru_b_x": [2, D], "lru_lambda": [2, D], "lru_w_out": [D, D]}
    for k in LRU_KEYS:
        prm[k] = B.din(k, shapes[k])
    if stage in (2, 4):
        pscar = B.din("carry_ps", [2, 7, 2, D])
        prm["ffn_w1"] = B.din("ffn_w1", [D, F])
        prm["ffn_w3"] = B.din("ffn_w3", [D, F])
        prm["ffn_w2"] = B.din("ffn_w2", [F, D])
        prm["sgu_w_in"] = B.din("sgu_w_in", [D, 2 * D])
        prm["sgu_ln_g"] = B.din("sgu_ln_g", [D])
        prm["sgu_ln_b"] = B.din("sgu_ln_b", [D])
        prm["sgu_w_s"] = B.din("sgu_w_s", [cfg.SGROUPS, 128, 128])
        prm["sgu_b_s"] = B.din("sgu_b_s", [cfg.SGROUPS, 128])
        prm["sgu_w_out"] = B.din("sgu_w_out", [D, D])
        prm["moe_router"] = B.din("moe_router", [D, cfg.NEXP])
        prm["moe_w1"] = B.din("moe_w1", [cfg.NEXP, D, F])
        prm["moe_w3"] = B.din("moe_w3", [cfg.NEXP, D, F])
        prm["moe_w2"] = B.din("moe_w2", [cfg.NEXP, F, D])
    if stage == 4:
        final_g = B.din("final_g", [D])
    B.setup_common()
    X = B.X
    XH = B.sb("XH", [128, KC, 3], F32)
    HH = B.sb("HH", [128, KC, 3], BF16)
    EF = B.sb("EF", [128, 2], F32)
    es = ExitStack()
    TM = [B.sb("TM%d" % i, [128, D], F32, es) for i in range(2)]
    P.dma("sp", lambda e: e.dma_start(out=EF[:], in_=ef), writes=["EF"])
    NTC = T // 128
    for n in range(NTC):
        tm = TM[n % 2]
        kt = "TM%d" % (n % 2)
        src = cin[n * 128:(n + 1) * 128, :] if n * 128 < NCX else xin[n * 128 - NCX:(n + 1) * 128 - NCX, :]
        P.dma("sp", lambda e, tm=tm, src=src: e.dma_start(out=tm[:], in_=src), writes=[kt])
        for k0 in range(0, KC, 4):
            nk = min(4, KC - k0)
            pi = (k0 // 4) % 3
            ps = B.psA[pi]
            kp = "psA%d" % pi
            def tr(e, tm=tm, k0=k0, nk=nk, ps=ps):
                r = None
                for i in range(nk):
                    r = e.transpose(ps[:, i * 128:(i + 1) * 128], tm[:, (k0 + i) * 128:(k0 + i + 1) * 128], B.ident[:])
                return r
            P.op("pe", tr, reads=[kt, "ident"], writes=[kp])
            P.op("act", lambda e, n=n, k0=k0, nk=nk, ps=ps: e.copy(out=X[:, k0:k0 + nk, n * 128:(n + 1) * 128], in_=ps[:, 0:nk * 128].rearrange("p (k t) -> p k t", k=nk)), reads=[kp], writes=["X"])
    with nc.allow_non_contiguous_dma(reason="3 halo rows"):
        k_ = B.fm_rows(lambda i: XH[:, :, i], xhalo, 3, "XH")
        P.op("dve", lambda e: e.tensor_copy(out=XH[:], in_=XH[:]), reads=k_, writes=["XH"])
    B.full_barrier()
    es.close()

    mods = {li: B.compute_mods(li, w_mod[li], b_mod[li], c_v, cctx_v, norm_g[li]) for li in layers}
    m0 = mods[li0]
    B.norm_mod(m0["GS_A"], m0["SH_A"], m0["keys"])
    es = ExitStack()
    sqh = B.sb("sqh", [128, KC, 3], F32, es)
    rh = B.sb("rh", [128, 3], F32, es)
    th = B.sb("th", [128, KC, 3], F32, es)
    P.op("dve", lambda e: e.tensor_tensor(out=sqh[:], in0=XH[:], in1=XH[:], op=ALU.mult), reads=["XH"], writes=["sqh"])
    def mmh(e):
        r = None
        for k in range(KC):
            r = e.matmul(B.psC[0][:, 0:3], lhsT=B.ones32[:], rhs=sqh[:, k, :], start=(k == 0), stop=(k == KC - 1))
        return r
    P.op("pe", mmh, reads=["sqh", "ones32"], writes=["psC0"])
    P.op("dve", lambda e: e.tensor_scalar(out=rh[:], in0=B.psC[0][:, 0:3], scalar1=1.0 / D, scalar2=EPS, op0=ALU.mult, op1=ALU.add), reads=["psC0"], writes=["rh"])
    P.op("act", lambda e: e.activation(out=rh[:], in_=rh[:], func=AF.Ln), reads=["rh"], writes=["rh"])
    P.op("act", lambda e: e.activation(out=rh[:], in_=rh[:], func=AF.Exp, scale=-0.5), reads=["rh"], writes=["rh"])
    for k in range(KC):
        P.op("dve", lambda e, k=k: e.scalar_tensor_tensor(out=th[:, k, :], in0=XH[:, k, :], scalar=m0["GS_A"][:, k, 0:1], in1=rh[:], op0=ALU.mult, op1=ALU.mult), reads=["XH", "rh"] + m0["keys"], writes=["th"])
        P.op("act", lambda e, k=k: e.activation(out=HH[:, k, :], in_=th[:, k, :], func=AF.Identity, bias=m0["SH_A"][:, k, 0:1], scale=1.0), reads=["th"] + m0["keys"], writes=["HH"])
    B.full_barrier()
    es.close()

    lp = B.lru_params(0, prm)
    CFIN = B.sb("CFIN", [128, 2, KC], F32)
    ctx_out = (li0 == 0)
    if stage in (1, 3):
        PSO = B.sb("PSO", [128, 2, 2, KC], F32)
        B.lru_pass(prm, lp, m0, NCX, T, False, 1, HH=HH, EF=EF, PSOUT=PSO)
        pso = B.dout("ps_out", [2, 2, D])
        with nc.allow_non_contiguous_dma(reason="tiny"):
            ok_ = []
            for a_ in range(2):
                for b_ in range(2):
                    P.dma("sp", lambda e, a_=a_, b_=b_: e.dma_start(out=pso[a_, b_].rearrange("(k p) -> p k", p=128), in_=PSO[:, a_, b_, :], allow_slow_non_contiguous=True), reads=["PSOUT"], writes=["o_ps%d%d" % (a_, b_)])
                    ok_.append("o_ps%d%d" % (a_, b_))
        P.wait_all("sp", ok_)
        return B.finish(), B

    B.lru_pass(prm, lp, m0, 0, NCX, True, 2, CFIN=CFIN, do_out=ctx_out)
    CAR = B.sb("CAR", [128, 2, KC], F32)
    PSC = B.sb("PSC", [128, 2, 7, 2, KC], F32)
    with nc.allow_non_contiguous_dma(reason="tiny"):
        for dr in range(2):
            k_ = B.fm_rows(lambda i, dr=dr: PSC[:, dr, i // 2, i % 2, :], pscar[dr].rearrange("s b d -> (s b) d"), 14, "PSC%d" % dr)
            P.op("dve", lambda e, dr=dr: e.tensor_copy(out=PSC[:, dr], in_=PSC[:, dr]), reads=k_, writes=["PSC%d" % dr])
    P.op("dve", lambda e: e.tensor_copy(out=CAR[:], in_=CFIN[:]), reads=["CFIN"], writes=["CARRY"])
    for dr in range(2):
        for st in range(7):
            P.op("dve", lambda e, dr=dr, st=st: e.tensor_tensor(out=CAR[:, dr, :], in0=CAR[:, dr, :], in1=PSC[:, dr, st, 0, :], op=ALU.mult), reads=["CARRY", "PSC%d" % dr], writes=["CARRY"])
            P.op("dve", lambda e, dr=dr, st=st: e.tensor_tensor(out=CAR[:, dr, :], in0=CAR[:, dr, :], in1=PSC[:, dr, st, 1, :], op=ALU.add), reads=["CARRY", "PSC%d" % dr], writes=["CARRY"])
    B.lru_pass(prm, lp, m0, NCX, T, False, 2, HH=HH, EF=EF, CARRY=CAR, do_out=True)
    B.norm_mod(m0["GS_B"], m0["SH_B"], m0["keys"])
    B.swiglu_into_x(prm["ffn_w1"], prm["ffn_w3"], prm["ffn_w2"], m0["GT_B"], m0["keys"], tag="ffn")
    m1 = mods[li0 + 1]
    sp_es = ExitStack()
    sp = B.sgu_params(prm, sp_es)
    B.norm_mod(m1["GS_A"], m1["SH_A"], m1["keys"])
    if li0 == 0:
        B.sgu_pass(prm, sp, m1, 0, NCX, True)
    B.sgu_pass(prm, sp, m1, NCX, T, False)
    B.full_barrier()
    sp_es.close()
    B.norm_mod(m1["GS_B"], m1["SH_B"], m1["keys"])
    GATE = B.moe_gates(prm["moe_router"], m1)
    GBC = B.sb("GBC", [128, T], F32)
    DG = B.sb("DG", [128, 4, 128], F32)
    for ex in range(cfg.NEXP):
        B.gate_bcast(GATE, ex, GBC, DG)
        B.swiglu_into_x(prm["moe_w1"][ex], prm["moe_w3"][ex], prm["moe_w2"][ex], m1["GT_B"], m1["keys"], gbc=GBC, gbc_key="GBC", tag="moe")
    es = ExitStack()
    OT = [B.sb("OT%d" % i, [128, D], F32, es) for i in range(2)]
    if stage == 4:
        FG = B.sb("FG", [128, KC], F32, es)
        with nc.allow_non_contiguous_dma(reason="tiny"):
            P.dma("sp", lambda e: e.dma_start(out=FG[:], in_=final_g.rearrange("(k p) -> p k", p=128), allow_slow_non_contiguous=True), writes=["FG"])
        sq = [B.sb("fsq%d" % i, [128, T], BF16, es) for i in range(2)]
        tiles = col_tiles(0, T)
        for k in range(KC):
            s_ = sq[k % 2]
            ks = "fsq%d" % (k % 2)
            P.op("act", lambda e, k=k, s_=s_: e.activation(out=s_[:], in_=X[:, k, :], func=AF.Square), reads=["X"], writes=[ks])
            def mm(e, k=k, s_=s_):
                r = None
                for i, (a, b) in enumerate(tiles):
                    r = e.matmul(B.psA[i][:, 0:b - a], lhsT=B.ones16[:], rhs=s_[:, a:b], start=(k == 0), stop=(k == KC - 1))
                return r
            P.op("pe", mm, reads=[ks, "ones16"], writes=["psA0", "psA1", "psA2"])
        for i, (a, b) in enumerate(tiles):
            P.op("dve", lambda e, i=i, a=a, b=b: e.tensor_scalar(out=B.RSTD[:, a:b], in0=B.psA[i][:, 0:b - a], scalar1=1.0 / D, scalar2=EPS, op0=ALU.mult, op1=ALU.add), reads=["psA%d" % i], writes=["RSTD"])
        P.op("act", lambda e: e.activation(out=B.RSTD[:], in_=B.RSTD[:], func=AF.Ln), reads=["RSTD"], writes=["RSTD"])
        P.op("act", lambda e: e.activation(out=B.RSTD[:], in_=B.RSTD[:], func=AF.Exp, scale=-0.5), reads=["RSTD"], writes=["RSTD"])
        for k in range(KC):
            P.op("dve", lambda e, k=k: e.scalar_tensor_tensor(out=X[:, k, NCX:T], in0=X[:, k, NCX:T], scalar=FG[:, k:k + 1], in1=B.RSTD[:, NCX:T], op0=ALU.mult, op1=ALU.mult), reads=["X", "RSTD", "FG"], writes=["X"])
    xout = B.dout("xout", [NL, D])
    cout = B.dout("cout", [NCX, D])
    okeys = []
    for n in range(NTC):
        ot = OT[n % 2]
        ko = "OT%d" % (n % 2)
        for k0 in range(0, KC, 4):
            nk = min(4, KC - k0)
            pi = (k0 // 4) % 3
            ps = B.psA[pi]
            kp = "psA%d" % pi
            def tr(e, n=n, k0=k0, nk=nk, ps=ps):
                r = None
                for i in range(nk):
                    r = e.transpose(ps[:, i * 128:(i + 1) * 128], X[:, k0 + i, n * 128:(n + 1) * 128], B.ident[:])
                return r
            P.op("pe", tr, reads=["X", "ident"], writes=[kp])
            P.op("act", lambda e, ot=ot, k0=k0, nk=nk, ps=ps: e.copy(out=ot[:, k0 * 128:(k0 + nk) * 128], in_=ps[:, 0:nk * 128]), reads=[kp], writes=[ko])
        dst = cout[n * 128:(n + 1) * 128, :] if n * 128 < NCX else xout[n * 128 - NCX:(n + 1) * 128 - NCX, :]
        kk = "o_x%d" % n
        P.dma("sp", lambda e, ot=ot, dst=dst: e.dma_start(out=dst, in_=ot[:]), reads=[ko], writes=[kk])
        okeys.append(kk)
    P.wait_all("sp", okeys)
    es.close()
    return B.finish(), B


_PROG_CACHE = {}


def _get_prog(cfg, stage):
    key = (cfg.D, cfg.NL, cfg.NCX, cfg.F, cfg.HEADS, cfg.SGROUPS, cfg.NEXP, stage)
    if key not in _PROG_CACHE:
        _PROG_CACHE[key] = build_stage(cfg, stage)
    return _PROG_CACHE[key]


def run_module(cfg, inp):
    NCO, NL, D = cfg.NCORES, cfg.NL, cfg.D
    f = lambda a: np.ascontiguousarray(a, dtype=np.float32)
    x = f(inp["x"][0])
    ctx = f(inp["ctx"][0])
    ident = np.eye(128, dtype=np.float32)
    zero_row = np.zeros((1, D), np.float32)

    def halos(xfull, c):
        lo = xfull[c * NL - 2:c * NL] if c > 0 else np.concatenate([zero_row, zero_row], 0)
        hi = xfull[(c + 1) * NL:(c + 1) * NL + 1] if c < NCO - 1 else zero_row
        return f(np.concatenate([lo, hi], 0))

    def efl(c):
        e = np.ones((128, 2), np.float32)
        if c == 0:
            e[:, 0] = 0.0
        if c == NCO - 1:
            e[:, 1] = 0.0
        return e

    def lru_inputs(j):
        d = {}
        for k in LRU_KEYS:
            a = inp[k][j]
            if k in ("lru_b_a", "lru_b_x"):
                a = a.reshape(2, D)
            d[k] = f(a)
        return d

    def base(c, xfull, cfull, lis):
        d = {"xin": f(xfull[c * NL:(c + 1) * NL]), "xhalo": halos(xfull, c), "cin": cfull, "ef": efl(c), "ident": ident,
             "c": f(inp["c"][0]), "c_ctx": f(inp["c_ctx"])}
        for li in lis:
            d["w_mod%d" % li] = f(inp["w_mod"][li])
            d["b_mod%d" % li] = f(inp["b_mod"][li])
            d["norm_g%d" % li] = f(inp["norm_g"][li])
        return d

    def carries(ps_all, c):
        out = np.zeros((2, 7, 2, D), np.float32)
        out[:, :, 0, :] = 1.0
        seq_f = list(range(0, c))
        seq_r = list(range(NCO - 1, c, -1))
        for i, k in enumerate(seq_f):
            out[0, i] = ps_all[k][0]
        for i, k in enumerate(seq_r):
            out[1, i] = ps_all[k][1]
        return out

    cores = list(range(NCO))
    for half in range(2):
        li0, j = 2 * half, half
        lw = lru_inputs(j)
        nc1, _ = _get_prog(cfg, 1 if half == 0 else 3)
        maps = []
        for c in cores:
            d = base(c, x, ctx, [li0])
            d.update(lw)
            maps.append(d)
        res = run_bass_kernel_spmd(nc1, maps, core_ids=cores)
        ps_all = [res.results[c]["ps_out"] for c in cores]
        nc2, _ = _get_prog(cfg, 2 if half == 0 else 4)
        maps = []
        for c in cores:
            d = base(c, x, ctx, [li0, li0 + 1])
            d.update(lw)
            d["carry_ps"] = carries(ps_all, c)
            d["ffn_w1"], d["ffn_w3"], d["ffn_w2"] = f(inp["ffn_w1"][j]), f(inp["ffn_w3"][j]), f(inp["ffn_w2"][j])
            for k in ("sgu_w_in", "sgu_ln_g", "sgu_ln_b", "sgu_w_s", "sgu_b_s", "sgu_w_out", "moe_router", "moe_w1", "moe_w3", "moe_w2"):
                d[k] = f(inp[k][j])
            if half == 1:
                d["final_g"] = f(inp["final_g"])
            maps.append(d)
        res = run_bass_kernel_spmd(nc2, maps, core_ids=cores)
        x = np.concatenate([res.results[c]["xout"] for c in cores], 0)
        ctx = f(res.results[0]["cout"])
    return x[None].astype(np.float32)


QUADS = [[0, 1, 2, 3], [4, 5, 6, 7]]
XPAIRS = [[0, 4], [1, 5], [2, 6], [3, 7]]


def exchange(B, src_ap, n, tag, rkey, stack=None):
    nc, P = B.nc, B.P
    b0 = nc.dram_tensor("xb0_" + tag, [128, n], F32)
    b1 = nc.dram_tensor("xb1_" + tag, [4 * 128, n], F32)
    b2 = nc.dram_tensor("xb2_" + tag, [8 * 128, n], F32)
    G = B.sb("XG_" + tag, [128, 8, n], F32, stack)
    P.dma("sp", lambda e: e.dma_start(out=b0.ap(), in_=src_ap), reads=[rkey], writes=["xb0_" + tag])
    P.collective(lambda e: e.collective_compute("AllGather", ALU.bypass, replica_groups=QUADS, ins=[b0.ap().opt()], outs=[b1.ap().opt()]),
                 reads=["xb0_" + tag], writes=["xb1_" + tag])
    P.collective(lambda e: e.collective_compute("AllGather", ALU.bypass, replica_groups=XPAIRS, ins=[b1.ap().opt()], outs=[b2.ap().opt()]),
                 reads=["xb1_" + tag], writes=["xb2_" + tag])
    P.dma("sp", lambda e: e.dma_start(out=G[:], in_=b2.ap().rearrange("(r p) n -> p r n", p=128)), reads=["xb2_" + tag], writes=["XG_" + tag])
    return G


def build_fused(cfg):
    B = Builder(cfg, 0)
    nc, P = B.nc, B.P
    D, KC, T, NL, NCX, F = cfg.D, cfg.KC, cfg.T, cfg.NL, cfg.NCX, cfg.F
    HD = D // cfg.HEADS
    xin = B.din("xin", [NL, D])
    xhalo = B.din("xhalo", [3, D])
    cin = B.din("cin", [NCX, D])
    ef = B.din("ef", [128, 2])
    selm = B.din("selmask", [128, 16])
    carm = B.din("carmask", [128, 16])
    c_v = B.din("c", [D])
    cctx_v = B.din("c_ctx", [D])
    w_mod = {li: B.din("w_mod%d" % li, [D, (6 * D) // 8]) for li in range(4)}
    b_mod = {li: B.din("b_mod%d" % li, [(6 * D) // 8]) for li in range(4)}
    norm_g = {li: B.din("norm_g%d" % li, [2, D]) for li in range(4)}
    final_g = B.din("final_g", [D])
    shapes = {"lru_w_in": [D, 2 * D], "lru_conv_w": [4, D], "lru_conv_b": [D], "lru_w_a": [2, cfg.HEADS, HD, HD], "lru_b_a": [2, D],
              "lru_w_x": [2, cfg.HEADS, HD, HD], "lru_b_x": [2, D], "lru_lambda": [2, D], "lru_w_out": [D, D],
              "ffn_w1": [D, F], "ffn_w3": [D, F], "ffn_w2": [F, D], "sgu_w_in": [D, 2 * D], "sgu_ln_g": [D], "sgu_ln_b": [D],
              "sgu_w_s": [cfg.SGROUPS, 128, 128], "sgu_b_s": [cfg.SGROUPS, 128], "sgu_w_out": [D, D], "moe_router": [D, cfg.NEXP],
              "moe_w1": [cfg.NEXP, D, F], "moe_w3": [cfg.NEXP, D, F], "moe_w2": [cfg.NEXP, F, D]}
    prms = []
    for j in range(2):
        prms.append({k: B.din("%s_%d" % (k, j), shp) for k, shp in shapes.items()})
    B.setup_common()
    X = B.X
    XH = B.sb("XH", [128, KC, 3], F32)
    HH = B.sb("HH", [128, KC, 3], BF16)
    EF = B.sb("EF", [128, 2], F32)
    SEL = B.sb("SEL", [128, 16], F32)
    CMK = B.sb("CMK", [128, 16], F32)
    CFIN = B.sb("CFIN", [128, 2, KC], F32)
    PSO = B.sb("PSO", [128, 2, 2, KC], F32)
    CAR = B.sb("CAR", [128, 2, KC], F32)
    T1 = B.sb("T1c", [128, KC], F32)
    HSRC = B.sb("HSRC", [128, KC, 3], F32)
    P.dma("sp", lambda e: e.dma_start(out=EF[:], in_=ef), writes=["EF"])
    P.dma("sp", lambda e: e.dma_start(out=SEL[:], in_=selm), writes=["SEL"])
    P.dma("sp", lambda e: e.dma_start(out=CMK[:], in_=carm), writes=["CMK"])
    es = ExitStack()
    TM = [B.sb("TM%d" % i, [128, D], F32, es) for i in range(2)]
    NTC = T // 128
    for n in range(NTC):
        tm = TM[n % 2]
        kt = "TM%d" % (n % 2)
        src = cin[n * 128:(n + 1) * 128, :] if n * 128 < NCX else xin[n * 128 - NCX:(n + 1) * 128 - NCX, :]
        P.dma("sp", lambda e, tm=tm, src=src: e.dma_start(out=tm[:], in_=src), writes=[kt])
        for k0 in range(0, KC, 4):
            nk = min(4, KC - k0)
            pi = (k0 // 4) % 3
            ps = B.psA[pi]
            kp = "psA%d" % pi
            def tr(e, tm=tm, k0=k0, nk=nk, ps=ps):
                r = None
                for i in range(nk):
                    r = e.transpose(ps[:, i * 128:(i + 1) * 128], tm[:, (k0 + i) * 128:(k0 + i + 1) * 128], B.ident[:])
                return r
            P.op("pe", tr, reads=[kt, "ident"], writes=[kp])
            P.op("act", lambda e, n=n, k0=k0, nk=nk, ps=ps: e.copy(out=X[:, k0:k0 + nk, n * 128:(n + 1) * 128], in_=ps[:, 0:nk * 128].rearrange("p (k t) -> p k t", k=nk)), reads=[kp], writes=["X"])
    k_ = B.fm_rows(lambda i: XH[:, :, i], xhalo, 3, "XH")
    P.op("dve", lambda e: e.tensor_copy(out=XH[:], in_=XH[:]), reads=k_, writes=["XH"])
    B.full_barrier()
    es.close()

    mods = B.compute_mods_sharded(w_mod, b_mod, c_v, cctx_v, norm_g)

    def run_half(half):
        li0, j = 2 * half, half
        prm = prms[j]
        m0 = mods[li0]
        if half == 1:
            P.op("dve", lambda e: e.tensor_copy(out=HSRC[:, :, 0:1], in_=X[:, :, NCX:NCX + 1]), reads=["X"], writes=["HSRC"])
            P.op("dve", lambda e: e.tensor_copy(out=HSRC[:, :, 1:3], in_=X[:, :, T - 2:T]), reads=["X"], writes=["HSRC"])
            x_es = ExitStack()
            GH = exchange(B, HSRC[:].rearrange("p k t -> p (k t)"), KC * 3, "halo", "HSRC", x_es)
            GHv = GH[:].rearrange("p r (k t) -> p r k t", t=3)
            P.op("dve", lambda e: e.memset(XH[:], 0.0), reads=["XH"], writes=["XH"])
            for r in range(8):
                P.op("dve", lambda e, r=r: e.scalar_tensor_tensor(out=XH[:, :, 0:2], in0=GHv[:, r, :, 1:3], scalar=SEL[:, r:r + 1], in1=XH[:, :, 0:2], op0=ALU.mult, op1=ALU.add), reads=["XG_halo", "SEL", "XH"], writes=["XH"])
                P.op("dve", lambda e, r=r: e.scalar_tensor_tensor(out=XH[:, :, 2:3], in0=GHv[:, r, :, 0:1], scalar=SEL[:, 8 + r:9 + r], in1=XH[:, :, 2:3], op0=ALU.mult, op1=ALU.add), reads=["XG_halo", "SEL", "XH"], writes=["XH"])
            B.full_barrier()
            x_es.close()
        B.norm_mod(m0["GS_A"], m0["SH_A"], m0["keys"])
        es = ExitStack()
        sqh = B.sb(B.key("sqh"), [128, KC, 3], F32, es)
        rh = B.sb(B.key("rh"), [128, 3], F32, es)
        th = B.sb(B.key("th"), [128, KC, 3], F32, es)
        P.op("dve", lambda e: e.tensor_tensor(out=sqh[:], in0=XH[:], in1=XH[:], op=ALU.mult), reads=["XH"], writes=["sqh"])
        def mmh(e):
            r = None
            for k in range(KC):
                r = e.matmul(B.psC[0][:, 0:3], lhsT=B.ones32[:], rhs=sqh[:, k, :], start=(k == 0), stop=(k == KC - 1))
            return r
        P.op("pe", mmh, reads=["sqh", "ones32"], writes=["psC0"])
        P.op("dve", lambda e: e.tensor_scalar(out=rh[:], in0=B.psC[0][:, 0:3], scalar1=1.0 / D, scalar2=EPS, op0=ALU.mult, op1=ALU.add), reads=["psC0"], writes=["rh"])
        P.op("act", lambda e: e.activation(out=rh[:], in_=rh[:], func=AF.Ln), reads=["rh"], writes=["rh"])
        P.op("act", lambda e: e.activation(out=rh[:], in_=rh[:], func=AF.Exp, scale=-0.5), reads=["rh"], writes=["rh"])
        for k in range(KC):
            P.op("dve", lambda e, k=k: e.scalar_tensor_tensor(out=th[:, k, :], in0=XH[:, k, :], scalar=m0["GS_A"][:, k, 0:1], in1=rh[:], op0=ALU.mult, op1=ALU.mult), reads=["XH", "rh"] + m0["keys"], writes=["th"])
            P.op("act", lambda e, k=k: e.activation(out=HH[:, k, :], in_=th[:, k, :], func=AF.Identity, bias=m0["SH_A"][:, k, 0:1], scale=1.0), reads=["th"] + m0["keys"], writes=["HH"])
        B.full_barrier()
        es.close()

        lp = B.lru_params(j, prm)
        spill = nc.dram_tensor("lru_spill%d" % j, [KC * 4, 128, NL], F32).ap()
        B.lru_pass(prm, lp, m0, 0, NCX, True, 2, CFIN=CFIN, do_out=(half == 0))
        B.lru_pass(prm, lp, m0, NCX, T, False, 1, HH=HH, EF=EF, PSOUT=PSO, SPILL=spill)
        x_es2 = ExitStack()
        GPS = exchange(B, PSO[:].rearrange("p a b k -> p (a b k)"), 4 * KC, "ps%d" % half, "PSOUT", x_es2)
        GPv = GPS[:].rearrange("p r (a b k) -> p r a b k", a=2, b=2)
        gk = "XG_ps%d" % half
        P.op("dve", lambda e: e.tensor_copy(out=CAR[:], in_=CFIN[:]), reads=["CFIN", "CARRY"], writes=["CARRY"])
        for dr in range(2):
            order = list(range(8)) if dr == 0 else list(range(7, -1, -1))
            for r in order:
                P.op("dve", lambda e, dr=dr, r=r: e.tensor_tensor(out=T1[:], in0=CAR[:, dr, :], in1=GPv[:, r, dr, 0, :], op=ALU.mult), reads=["CARRY", gk], writes=["T1c"])
                P.op("dve", lambda e, dr=dr, r=r: e.tensor_tensor(out=T1[:], in0=T1[:], in1=GPv[:, r, dr, 1, :], op=ALU.add), reads=["T1c", gk], writes=["T1c"])
                P.op("dve", lambda e, dr=dr, r=r: e.tensor_tensor(out=T1[:], in0=T1[:], in1=CAR[:, dr, :], op=ALU.subtract), reads=["T1c", "CARRY"], writes=["T1c"])
                P.op("dve", lambda e, dr=dr, r=r: e.scalar_tensor_tensor(out=CAR[:, dr, :], in0=T1[:], scalar=CMK[:, dr * 8 + r:dr * 8 + r + 1], in1=CAR[:, dr, :], op0=ALU.mult, op1=ALU.add), reads=["T1c", "CARRY", "CMK"], writes=["CARRY"])
        B.full_barrier()
        x_es2.close()
        B.lru_pass(prm, lp, m0, NCX, T, False, 2, HH=HH, EF=EF, CARRY=CAR, do_out=True, SPILL=spill)
        B.norm_mod(m0["GS_B"], m0["SH_B"], m0["keys"])
        fsegs = None if half == 0 else [(NCX, T, 0)]
        B.swiglu_into_x(prm["ffn_w1"], prm["ffn_w3"], prm["ffn_w2"], m0["GT_B"], m0["keys"], tag="ffn", segs=fsegs)
        m1 = mods[li0 + 1]
        sp_es = ExitStack()
        sp = B.sgu_params(prm, sp_es)
        B.norm_mod(m1["GS_A"], m1["SH_A"], m1["keys"])
        if half == 0:
            B.sgu_pass(prm, sp, m1, 0, NCX, True)
        B.sgu_pass(prm, sp, m1, NCX, T, False)
        B.full_barrier()
        sp_es.close()
        B.norm_mod(m1["GS_B"], m1["SH_B"], m1["keys"])
        g_es = ExitStack()
        GATE = B.sb(B.key("GATE"), [128, T // 128, cfg.NEXP], F32, g_es)
        GBC = B.sb(B.key("GBC"), [128, T], F32, g_es)
        DG = B.sb(B.key("DG"), [128, 4, 128], F32, g_es)
        B.moe_gates(prm["moe_router"], m1, GATE=GATE)
        for ex in range(cfg.NEXP):
            B.gate_bcast(GATE, ex, GBC, DG)
            B.swiglu_into_x(prm["moe_w1"][ex], prm["moe_w3"][ex], prm["moe_w2"][ex], m1["GT_B"], m1["keys"], gbc=GBC, gbc_key="GBC", tag="moe", segs=fsegs)
        B.full_barrier()
        g_es.close()

    for half_ in range(2):
        run_half(half_)

    es = ExitStack()
    OT = [B.sb("OT%d" % i, [128, D], F32, es) for i in range(2)]
    FG = B.sb("FG", [128, KC], F32, es)
    P.dma("sp", lambda e: e.dma_start(out=FG[:], in_=final_g.rearrange("(k p) -> p k", p=128), allow_slow_non_contiguous=True), writes=["FG"])
    sq = [B.sb("fsq%d" % i, [128, T], BF16, es) for i in range(2)]
    tiles = col_tiles(0, T)
    for k in range(KC):
        s_ = sq[k % 2]
        ks = "fsq%d" % (k % 2)
        P.op("act", lambda e, k=k, s_=s_: e.activation(out=s_[:], in_=X[:, k, :], func=AF.Square), reads=["X"], writes=[ks])
        def mm(e, k=k, s_=s_):
            r = None
            for i, (a, b) in enumerate(tiles):
                r = e.matmul(B.psA[i][:, 0:b - a], lhsT=B.ones16[:], rhs=s_[:, a:b], start=(k == 0), stop=(k == KC - 1))
            return r
        P.op("pe", mm, reads=[ks, "ones16"], writes=["psA0", "psA1", "psA2"])
    for i, (a, b) in enumerate(tiles):
        P.op("dve", lambda e, i=i, a=a, b=b: e.tensor_scalar(out=B.RSTD[:, a:b], in0=B.psA[i][:, 0:b - a], scalar1=1.0 / D, scalar2=EPS, op0=ALU.mult, op1=ALU.add), reads=["psA%d" % i], writes=["RSTD"])
    P.op("act", lambda e: e.activation(out=B.RSTD[:], in_=B.RSTD[:], func=AF.Ln), reads=["RSTD"], writes=["RSTD"])
    P.op("act", lambda e: e.activation(out=B.RSTD[:], in_=B.RSTD[:], func=AF.Exp, scale=-0.5), reads=["RSTD"], writes=["RSTD"])
    for k in range(KC):
        P.op("dve", lambda e, k=k: e.scalar_tensor_tensor(out=X[:, k, NCX:T], in0=X[:, k, NCX:T], scalar=FG[:, k:k + 1], in1=B.RSTD[:, NCX:T], op0=ALU.mult, op1=ALU.mult), reads=["X", "RSTD", "FG"], writes=["X"])
    xout = B.dout("xout", [NL, D])
    okeys = []
    for n in range(NCX // 128, NTC):
        ot = OT[n % 2]
        ko = "OT%d" % (n % 2)
        for k0 in range(0, KC, 4):
            nk = min(4, KC - k0)
            pi = (k0 // 4) % 3
            ps = B.psA[pi]
            kp = "psA%d" % pi
            def tr(e, n=n, k0=k0, nk=nk, ps=ps):
                r = None
                for i in range(nk):
                    r = e.transpose(ps[:, i * 128:(i + 1) * 128], X[:, k0 + i, n * 128:(n + 1) * 128], B.ident[:])
                return r
            P.op("pe", tr, reads=["X", "ident"], writes=[kp])
            P.op("act", lambda e, ot=ot, k0=k0, nk=nk, ps=ps: e.copy(out=ot[:, k0 * 128:(k0 + nk) * 128], in_=ps[:, 0:nk * 128]), reads=[kp], writes=[ko])
        dst = xout[n * 128 - NCX:(n + 1) * 128 - NCX, :]
        kk = "o_x%d" % n
        P.dma("sp", lambda e, ot=ot, dst=dst: e.dma_start(out=dst, in_=ot[:]), reads=[ko], writes=[kk])
        okeys.append(kk)
    P.wait_all("sp", okeys)
    es.close()
    return B.finish(), B


def run_fused(cfg, inp):
    NCO, NL, D = cfg.NCORES, cfg.NL, cfg.D
    assert NCO == 8
    f = lambda a: np.ascontiguousarray(a, dtype=np.float32)
    x = f(inp["x"][0])
    ctx = f(inp["ctx"][0])
    ident = np.eye(128, dtype=np.float32)
    zero_row = np.zeros((1, D), np.float32)
    key = ("fused", cfg.D, cfg.NL, cfg.NCX, cfg.F, cfg.HEADS, cfg.SGROUPS, cfg.NEXP)
    if key not in _PROG_CACHE:
        _PROG_CACHE[key] = build_fused(cfg)
    nc, _ = _PROG_CACHE[key]
    shared = {"cin": ctx, "ident": ident, "c": f(inp["c"][0]), "c_ctx": f(inp["c_ctx"]), "final_g": f(inp["final_g"])}
    NS = (6 * D) // 8
    for li in range(4):
        shared["norm_g%d" % li] = f(inp["norm_g"][li])
    for j in range(2):
        for k in LRU_KEYS + ["ffn_w1", "ffn_w3", "ffn_w2", "sgu_w_in", "sgu_ln_g", "sgu_ln_b", "sgu_w_s", "sgu_b_s", "sgu_w_out", "moe_router", "moe_w1", "moe_w3", "moe_w2"]:
            a = inp[k][j]
            if k in ("lru_b_a", "lru_b_x"):
                a = a.reshape(2, D)
            shared["%s_%d" % (k, j)] = f(a)
    maps = []
    for c in range(NCO):
        d = dict(shared)
        d["xin"] = f(x[c * NL:(c + 1) * NL])
        for li in range(4):
            d["w_mod%d" % li] = f(inp["w_mod"][li][:, c * NS:(c + 1) * NS])
            d["b_mod%d" % li] = f(inp["b_mod"][li][c * NS:(c + 1) * NS])
        lo = x[c * NL - 2:c * NL] if c > 0 else np.concatenate([zero_row, zero_row], 0)
        hi = x[(c + 1) * NL:(c + 1) * NL + 1] if c < NCO - 1 else zero_row
        d["xhalo"] = f(np.concatenate([lo, hi], 0))
        e = np.ones((128, 2), np.float32)
        if c == 0:
            e[:, 0] = 0.0
        if c == NCO - 1:
            e[:, 1] = 0.0
        d["ef"] = e
        sel = np.zeros((128, 16), np.float32)
        if c > 0:
            sel[:, c - 1] = 1.0
        if c < NCO - 1:
            sel[:, 8 + c + 1] = 1.0
        d["selmask"] = sel
        cm = np.zeros((128, 16), np.float32)
        cm[:, 0:c] = 1.0
        cm[:, 8 + c + 1:16] = 1.0
        d["carmask"] = cm
        maps.append(d)
    res = run_bass_kernel_spmd(nc, maps, core_ids=list(range(NCO)))
    out = np.concatenate([res.results[c]["xout"] for c in range(NCO)], 0)
    return out[None].astype(np.float32)


def kernel(**inputs):
    cfg = Cfg()
    return run_fused(cfg, inputs)
```

```python
import numpy as np
from contextlib import ExitStack
import concourse.bass as bass
import concourse.mybir as mybir
from concourse.bass_utils import run_bass_kernel_spmd

F32 = mybir.dt.float32
BF16 = mybir.dt.bfloat16
ALU = mybir.AluOpType
AF = mybir.ActivationFunctionType
AX = mybir.AxisListType
EPS = 1e-6
LRU_C = 8.0


class Cfg:
    def __init__(self, D=2048, NL=1024, NCX=256, F=5632, HEADS=8, SGROUPS=8, NEXP=8, NCORES=8):
        self.D, self.NL, self.NCX, self.F = D, NL, NCX, F
        self.HEADS, self.SGROUPS, self.NEXP, self.NCORES = HEADS, SGROUPS, NEXP, NCORES
        self.KC = D // 128
        self.FC = F // 128
        self.HC = (D // HEADS) // 128
        self.GCH = (D // SGROUPS) // 128
        self.T = NCX + NL


class Prog:
    ENGS = ("pe", "act", "dve", "pool", "sp")

    def __init__(self, nc, es, n_dma_sems=8):
        self.nc, self.es = nc, es
        self.streams = {e: [] for e in self.ENGS}
        self.psem = {e: es.enter_context(nc.semaphore("ps_" + e)) for e in ("pe", "act", "dve", "pool")}
        self.pcount = {e: 0 for e in self.psem}
        self.known = {e: {} for e in self.ENGS}
        self.semobj = {"ps_" + e: s for e, s in self.psem.items()}
        self.dsems, self.dcount, self.dnext = {}, {}, {}
        for q in ("sp", "pool"):
            names = ["ds_%s_%d" % (q, i) for i in range(n_dma_sems)]
            self.dsems[q] = names
            for n in names:
                self.semobj[n] = es.enter_context(nc.semaphore(n))
                self.dcount[n] = 0
            self.dnext[q] = 0
        self.last_write, self.readers = {}, {}
        self.ccsem = es.enter_context(nc.semaphore("cc_sem"))
        self.semobj["cc"] = self.ccsem
        self.ccount = 0

    def collective(self, fn, reads=(), writes=()):
        self._emit_waits("pool", self._deps(reads, writes))
        self.ccount += 1
        name = "cc%d" % self.ccount
        sem = self.es.enter_context(self.nc.semaphore(name))
        self.semobj[name] = sem
        tok = (name, 1)
        self.streams["pool"].append(("op", fn, sem, 1))
        self._commit(tok, reads, writes)
        return tok

    def _deps(self, reads, writes):
        toks = []
        for k in reads:
            toks += self.last_write.get(k, [])
        for k in writes:
            toks += self.last_write.get(k, [])
            toks += self.readers.get(k, [])
        return toks

    def _emit_waits(self, eng, toks):
        need = {}
        own = "ps_" + eng
        for (sn, v) in toks:
            if sn == own and eng == "pe":
                continue
            if self.known[eng].get(sn, 0) >= v:
                continue
            if need.get(sn, 0) < v:
                need[sn] = v
        for sn, v in need.items():
            self.known[eng][sn] = v
            self.streams[eng].append(("wait", self.semobj[sn], v))

    def _commit(self, tok, reads, writes):
        for k in reads:
            self.readers.setdefault(k, []).append(tok)
        for k in writes:
            self.last_write[k] = [tok]
            self.readers[k] = []

    def op(self, eng, fn, reads=(), writes=()):
        self._emit_waits(eng, self._deps(reads, writes))
        self.pcount[eng] += 1
        tok = ("ps_" + eng, self.pcount[eng])
        self.streams[eng].append(("op", fn, self.psem[eng], 1))
        self._commit(tok, reads, writes)
        return tok

    def dma(self, q, fn, reads=(), writes=()):
        toks = self._deps(reads, writes)
        names = self.dsems[q]
        sn = names[self.dnext[q] % len(names)]
        self.dnext[q] += 1
        if self.dcount[sn] > 0:
            toks = toks + [(sn, self.dcount[sn])]
        self._emit_waits(q, toks)
        self.dcount[sn] += 16
        tok = (sn, self.dcount[sn])
        self.streams[q].append(("op", fn, self.semobj[sn], 16))
        self._commit(tok, reads, writes)
        return tok

    def wait_all(self, eng, keys):
        toks = []
        for k in keys:
            toks += self.last_write.get(k, [])
        self._emit_waits(eng, toks)

    def replay(self, block):
        def run(stream):
            def body(e):
                for item in stream:
                    if item[0] == "wait":
                        e.wait_ge(item[1], item[2])
                    else:
                        item[1](e).then_inc(item[2], item[3])
            return body
        block.tensor(run(self.streams["pe"]))
        block.scalar(run(self.streams["act"]))
        block.vector(run(self.streams["dve"]))
        block.gpsimd(run(self.streams["pool"]))
        block.sync(run(self.streams["sp"]))


def col_tiles(c0, c1, maxw=512):
    out = []
    while c0 < c1:
        w = min(maxw, c1 - c0)
        out.append((c0, c0 + w))
        c0 += w
    return out


class Builder:
    def __init__(self, cfg, stage):
        self.cfg, self.stage = cfg, stage
        self.nc = bass.Bass("TRN2", target_bir_lowering=False)
        self.es = ExitStack()
        self.P = Prog(self.nc, self.es)
        self.uid = 0
        self.ins, self.outs = {}, {}
        self.out_keys = []

    def din(self, name, shape):
        t = self.nc.dram_tensor(name, list(shape), F32, kind="ExternalInput").ap()
        self.ins[name] = t
        return t

    def dout(self, name, shape):
        t = self.nc.dram_tensor(name, list(shape), F32, kind="ExternalOutput").ap()
        self.outs[name] = t
        return t

    def sb(self, name, shape, dt, stack=None):
        return (stack or self.es).enter_context(self.nc.sbuf_tensor(name, list(shape), dt))

    def pst(self, name, stack=None):
        return (stack or self.es).enter_context(self.nc.psum_tensor(name, [128, 512], F32))

    def key(self, base):
        self.uid += 1
        return "%s#%d" % (base, self.uid)

    def setup_common(self):
        cfg, P, nc = self.cfg, self.P, self.nc
        KC = cfg.KC
        self.ident_d = self.din("ident", [128, 128])
        self.ident = self.sb("ident_s", [128, 128], F32)
        P.dma("sp", lambda e: e.dma_start(out=self.ident[:], in_=self.ident_d), writes=["ident"])
        self.ones32 = self.sb("ones32", [128, 128], F32)
        self.ones16 = self.sb("ones16", [128, 128], BF16)
        P.op("dve", lambda e: e.memset(self.ones32[:], 1.0), writes=["ones32"])
        P.op("dve", lambda e: e.memset(self.ones16[:], 1.0), writes=["ones16"])
        self.X = self.sb("X", [128, KC, cfg.T], F32)
        self.H = self.sb("H", [128, KC, cfg.T], BF16)
        self.RSTD = self.sb("RSTD", [128, cfg.T], F32)
        self.psA = [self.pst("psA%d" % i) for i in range(3)]
        self.psB = [self.pst("psB%d" % i) for i in range(3)]
        self.psC = [self.pst("psC%d" % i) for i in range(2)]
        self.segs = [(0, cfg.NCX, 1), (cfg.NCX, cfg.T, 0)]

    def load_fm_vec(self, dst_ap, src_1d, q="sp", writes=()):
        with self.nc.allow_non_contiguous_dma(reason="tiny per-feature vectors"):
            pass
        def f(e):
            return e.dma_start(out=dst_ap, in_=src_1d.rearrange("(k p) -> p k", p=128), allow_slow_non_contiguous=True)
        return self.P.dma(q, f, writes=list(writes))

    def compute_mods(self, li, w_mod, b_mod, c_in, cctx_in, norm_g):
        cfg, P = self.cfg, self.P
        KC = cfg.KC
        MOD = self.sb("MOD%d" % li, [128, 6 * KC, 2], F32)
        d = {}
        for nm in ("GS_A", "GS_B"):
            d[nm] = self.sb("%s%d" % (nm, li), [128, KC, 2], F32)
        es = ExitStack()
        S = self.sb("modS%d" % li, [128, KC, 2], F32, es)
        bm = self.sb("modB%d" % li, [128, 6 * KC], F32, es)
        kS = self.key("S")
        with self.nc.allow_non_contiguous_dma(reason="tiny"):
            P.dma("sp", lambda e: e.dma_start(out=S[:, :, 0], in_=c_in.rearrange("(k p) -> p k", p=128), allow_slow_non_contiguous=True), writes=[kS + "a"])
            P.dma("sp", lambda e: e.dma_start(out=S[:, :, 1], in_=cctx_in.rearrange("(k p) -> p k", p=128), allow_slow_non_contiguous=True), writes=[kS + "b"])
            P.dma("sp", lambda e: e.dma_start(out=bm[:], in_=b_mod.rearrange("(k p) -> p k", p=128), allow_slow_non_contiguous=True), writes=[kS + "bm"])
        P.op("act", lambda e: e.activation(out=S[:], in_=S[:], func=AF.Silu), reads=[kS + "a", kS + "b"], writes=[kS])
        CB = 512 if 6 * cfg.D >= 512 else 6 * cfg.D
        nblk = (6 * cfg.D) // CB
        wbuf = [self.sb("modW%d_%d" % (li, i), [128, KC, CB], F32, es) for i in range(2)]
        for b in range(nblk):
            wb = wbuf[b % 2]
            kw = "modw%d" % (b % 2)
            P.dma("sp", lambda e, wb=wb, b=b: e.dma_start(out=wb[:], in_=w_mod[:, b * CB:(b + 1) * CB].rearrange("(k p) n -> p k n", p=128)), writes=[kw])
            for oc in range(CB // 128):
                o = b * (CB // 128) + oc
                ps = self.psC[o % 2]
                kp = "psC%d" % (o % 2)
                def mm(e, wb=wb, oc=oc, ps=ps):
                    r = None
                    for k in range(KC):
                        r = e.matmul(ps[:, 0:2], lhsT=wb[:, k, oc * 128:(oc + 1) * 128], rhs=S[:, k, :], start=(k == 0), stop=(k == KC - 1))
                    return r
                P.op("pe", mm, reads=[kw, kS], writes=[kp])
                P.op("dve", lambda e, o=o, ps=ps: e.tensor_scalar(out=MOD[:, o, :], in0=ps[:, 0:2], scalar1=bm[:, o:o + 1], scalar2=None, op0=ALU.add),
                     reads=[kp, kS + "bm"], writes=["MOD%d" % li])
        G = self.sb("modG%d" % li, [128, 2, KC], F32, es)
        gk = self.fm_rows(lambda i: G[:, i, :], norm_g, 2, kS + "g")
        self.P.op("dve", lambda e: e.tensor_copy(out=G[:], in_=G[:]), reads=gk, writes=[kS + "g"])
        for s in range(2):
            P.op("dve", lambda e, s=s: e.scalar_tensor_tensor(out=d["GS_A"][:, :, s], in0=MOD[:, KC:2 * KC, s], scalar=1.0, in1=G[:, 0, :], op0=ALU.add, op1=ALU.mult),
                 reads=["MOD%d" % li, kS + "g"], writes=["GS_A%d" % li])
            P.op("dve", lambda e, s=s: e.scalar_tensor_tensor(out=d["GS_B"][:, :, s], in0=MOD[:, 4 * KC:5 * KC, s], scalar=1.0, in1=G[:, 1, :], op0=ALU.add, op1=ALU.mult),
                 reads=["MOD%d" % li, kS + "g"], writes=["GS_B%d" % li])
        d["SH_A"] = MOD[:, 0:KC, :]
        d["GT_A"] = MOD[:, 2 * KC:3 * KC, :]
        d["SH_B"] = MOD[:, 3 * KC:4 * KC, :]
        d["GT_B"] = MOD[:, 5 * KC:6 * KC, :]
        d["keys"] = ["MOD%d" % li, "GS_A%d" % li, "GS_B%d" % li]
        P.wait_all("dve", ["GS_A%d" % li, "GS_B%d" % li, "MOD%d" % li])
        self._barrier(["GS_A%d" % li, "GS_B%d" % li, "MOD%d" % li, "modw0", "modw1", kS])
        es.close()
        return d

    def fm_rows(self, dst_fn, src2d, n, wkey, q="sp"):
        for i in range(n):
            self.P.dma(q, lambda e, i=i: e.dma_start(out=dst_fn(i), in_=src2d[i].rearrange("(k p) -> p k", p=128), allow_slow_non_contiguous=True), writes=["%s_r%d" % (wkey, i)])
        return ["%s_r%d" % (wkey, i) for i in range(n)]

    def compute_mods_sharded(self, w_mod, b_mod, c_in, cctx_in, norm_g, nlayers=4):
        cfg, P = self.cfg, self.P
        KC = cfg.KC
        NS = (6 * cfg.D) // 8
        NCH = NS // 128
        mods = []
        for li in range(nlayers):
            MOD = self.sb("MOD%d" % li, [128, 6 * KC, 2], F32)
            d = {"MOD": MOD}
            for nm in ("GS_A", "GS_B"):
                d[nm] = self.sb("%s%d" % (nm, li), [128, KC, 2], F32)
            mods.append(d)
        PART = self.sb("MODPART", [128, nlayers, NCH, 2], F32)
        es = ExitStack()
        S = self.sb("modS", [128, KC, 2], F32, es)
        bm = self.sb("modB", [128, nlayers, NCH], F32, es)
        G = self.sb("modG", [128, nlayers, 2, KC], F32, es)
        CB = 512 if NS % 512 == 0 else NS
        wbuf = [self.sb("modW%d" % i, [128, KC, CB], F32, es) for i in range(2)]
        wcnt = 0
        P.dma("sp", lambda e: e.dma_start(out=S[:, :, 0], in_=c_in.rearrange("(k p) -> p k", p=128), allow_slow_non_contiguous=True), writes=["mSa"])
        P.dma("sp", lambda e: e.dma_start(out=S[:, :, 1], in_=cctx_in.rearrange("(k p) -> p k", p=128), allow_slow_non_contiguous=True), writes=["mSb"])
        P.op("act", lambda e: e.activation(out=S[:], in_=S[:], func=AF.Silu), reads=["mSa", "mSb"], writes=["mS"])
        for li in range(nlayers):
            P.dma("sp", lambda e, li=li: e.dma_start(out=bm[:, li, :], in_=b_mod[li].rearrange("(k p) -> p k", p=128), allow_slow_non_contiguous=True), writes=["mbm%d" % li])
            gk = self.fm_rows(lambda i, li=li: G[:, li, i, :], norm_g[li], 2, "mg%d" % li)
            P.op("dve", lambda e, li=li: e.tensor_copy(out=G[:, li], in_=G[:, li]), reads=gk, writes=["mg%d" % li])
            for blk in range(NS // CB):
                wb = wbuf[wcnt % 2]
                kw = "modw%d" % (wcnt % 2)
                wcnt += 1
                P.dma("sp", lambda e, wb=wb, li=li, blk=blk: e.dma_start(out=wb[:], in_=w_mod[li][:, blk * CB:(blk + 1) * CB].rearrange("(k p) n -> p k n", p=128)), writes=[kw])
                for oi in range(CB // 128):
                    oc = blk * (CB // 128) + oi
                    ps = self.psC[oc % 2]
                    kp = "psC%d" % (oc % 2)
                    def mm(e, wb=wb, oi=oi, ps=ps):
                        r = None
                        for k in range(KC):
                            r = e.matmul(ps[:, 0:2], lhsT=wb[:, k, oi * 128:(oi + 1) * 128], rhs=S[:, k, :], start=(k == 0), stop=(k == KC - 1))
                        return r
                    P.op("pe", mm, reads=[kw, "mS"], writes=[kp])
                    P.op("dve", lambda e, li=li, oc=oc, ps=ps: e.tensor_scalar(out=PART[:, li, oc, :], in0=ps[:, 0:2], scalar1=bm[:, li, oc:oc + 1], scalar2=None, op0=ALU.add),
                         reads=[kp, "mbm%d" % li], writes=["MODPART"])
        n = nlayers * NCH * 2
        GX = exchange(self, PART[:].rearrange("p l c s -> p (l c s)"), n, "mods", "MODPART", es)
        GXv = GX[:].rearrange("p r (l c s) -> p r l c s", l=nlayers, c=NCH)
        for li in range(nlayers):
            d = mods[li]
            MOD = d["MOD"]
            P.op("dve", lambda e, li=li, MOD=MOD: e.tensor_copy(out=MOD[:].rearrange("p (r c) s -> p r c s", r=8), in_=GXv[:, :, li, :, :]), reads=["XG_mods"], writes=["MOD%d" % li])
            for s in range(2):
                P.op("dve", lambda e, s=s, d=d, MOD=MOD, li=li: e.scalar_tensor_tensor(out=d["GS_A"][:, :, s], in0=MOD[:, KC:2 * KC, s], scalar=1.0, in1=G[:, li, 0, :], op0=ALU.add, op1=ALU.mult),
                     reads=["MOD%d" % li, "mg%d" % li], writes=["GS_A%d" % li])
                P.op("dve", lambda e, s=s, d=d, MOD=MOD, li=li: e.scalar_tensor_tensor(out=d["GS_B"][:, :, s], in0=MOD[:, 4 * KC:5 * KC, s], scalar=1.0, in1=G[:, li, 1, :], op0=ALU.add, op1=ALU.mult),
                     reads=["MOD%d" % li, "mg%d" % li], writes=["GS_B%d" % li])
            d["SH_A"] = MOD[:, 0:KC, :]
            d["GT_A"] = MOD[:, 2 * KC:3 * KC, :]
            d["SH_B"] = MOD[:, 3 * KC:4 * KC, :]
            d["GT_B"] = MOD[:, 5 * KC:6 * KC, :]
            d["keys"] = ["MOD%d" % li, "GS_A%d" % li, "GS_B%d" % li]
        self.full_barrier()
        es.close()
        return mods

    def _barrier(self, keys):
        for eng in Prog.ENGS:
            toks = []
            for k in keys:
                toks += self.P.last_write.get(k, []) + self.P.readers.get(k, [])
            self.P._emit_waits(eng, toks)

    def full_barrier(self):
        toks = []
        for e, c in self.P.pcount.items():
            if c:
                toks.append(("ps_" + e, c))
        for sn, c in self.P.dcount.items():
            if c:
                toks.append((sn, c))
        for i in range(self.P.ccount):
            toks.append(("cc%d" % (i + 1), 1))
        for eng in Prog.ENGS:
            self.P._emit_waits(eng, toks)

    def norm_mod(self, GS, SH, modkeys, Hdst=None, extra_cols=None):
        cfg, P = self.cfg, self.P
        KC, T = cfg.KC, cfg.T
        H = self.H if Hdst is None else Hdst
        es = ExitStack()
        sq = [self.sb(self.key("sq"), [128, T], BF16, es) for _ in range(2)]
        tmp = [self.sb(self.key("nt"), [128, T], F32, es) for _ in range(2)]
        tiles = col_tiles(0, T)
        for k in range(KC):
            s = sq[k % 2]
            ks = "sq%d" % (k % 2)
            P.op("act", lambda e, k=k, s=s: e.activation(out=s[:], in_=self.X[:, k, :], func=AF.Square), reads=["X"], writes=[ks])
            def mm(e, k=k, s=s):
                r = None
                for i, (a, b) in enumerate(tiles):
                    r = e.matmul(self.psA[i][:, 0:b - a], lhsT=self.ones16[:], rhs=s[:, a:b], start=(k == 0), stop=(k == KC - 1))
                return r
            P.op("pe", mm, reads=[ks, "ones16"], writes=["psA0", "psA1", "psA2"])
        for i, (a, b) in enumerate(tiles):
            P.op("dve", lambda e, i=i, a=a, b=b: e.tensor_scalar(out=self.RSTD[:, a:b], in0=self.psA[i][:, 0:b - a], scalar1=1.0 / cfg.D, scalar2=EPS, op0=ALU.mult, op1=ALU.add),
                 reads=["psA%d" % i], writes=["RSTD"])
        P.op("act", lambda e: e.activation(out=self.RSTD[:], in_=self.RSTD[:], func=AF.Ln), reads=["RSTD"], writes=["RSTD"])
        P.op("act", lambda e: e.activation(out=self.RSTD[:], in_=self.RSTD[:], func=AF.Exp, scale=-0.5), reads=["RSTD"], writes=["RSTD"])
        for k in range(KC):
            t = tmp[k % 2]
            kt = "nt%d" % (k % 2)
            for (c0, c1, s) in self.segs:
                P.op("dve", lambda e, k=k, t=t, c0=c0, c1=c1, s=s: e.scalar_tensor_tensor(out=t[:, c0:c1], in0=self.X[:, k, c0:c1], scalar=GS[:, k, s:s + 1], in1=self.RSTD[:, c0:c1], op0=ALU.mult, op1=ALU.mult),
                     reads=["X", "RSTD"] + modkeys, writes=[kt + "_%d" % s])
                P.op("act", lambda e, k=k, t=t, c0=c0, c1=c1, s=s: e.activation(out=H[:, k, c0:c1], in_=t[:, c0:c1], func=AF.Identity, bias=SH[:, k, s:s + 1], scale=1.0),
                     reads=[kt + "_%d" % s] + modkeys, writes=["H"])
        self._barrier(["sq0", "sq1", "nt0_0", "nt0_1", "nt1_0", "nt1_1", "H", "RSTD"])
        es.close()

    def swiglu_into_x(self, w1, w3, w2, GT, modkeys, gbc=None, gbc_key=None, tag="ffn", segs=None, xdst=None, use_pool=True):
        cfg, P = self.cfg, self.P
        KC, T, FC = cfg.KC, cfg.T, cfg.FC
        G = 2
        XT = self.X if xdst is None else xdst
        tiles = []
        for (c0, c1, s) in (segs or self.segs):
            tiles += [(a, b, s) for (a, b) in col_tiles(c0, c1)]
        es = ExitStack()
        W1 = [self.sb(self.key("W1"), [128, KC, G * 128], BF16, es) for _ in range(2)]
        W3 = [self.sb(self.key("W3"), [128, KC, G * 128], BF16, es) for _ in range(2)]
        W2 = [self.sb(self.key("W2"), [128, G, cfg.D], BF16, es) for _ in range(2)]
        cmax = max(c1_ for (_, c1_, _) in (segs or self.segs))
        ACT = [self.sb(self.key("ACTB"), [128, G, cmax], BF16, es) for _ in range(2)]
        s1w = min(512, max(c1_ - c0_ for (c0_, c1_, _) in (segs or self.segs)))
        S1 = [self.sb(self.key("S1"), [128, s1w], F32, es) for _ in range(2 if use_pool else 1)]
        TMPB = [self.sb(self.key("TMPB"), [128, 512], F32, es) for _ in range(2)] if use_pool else None
        ng = FC // G
        cnt = 0
        ycnt = 0

        def issue_w(g):
            b = g % 2
            kw = "%s_w%d" % (tag, b)
            P.dma("pool", lambda e, g=g, b=b: e.dma_start(out=W1[b][:], in_=w1[:, g * G * 128:(g + 1) * G * 128].rearrange("(k p) n -> p k n", p=128)), writes=[kw + "1"])
            P.dma("pool", lambda e, g=g, b=b: e.dma_start(out=W3[b][:], in_=w3[:, g * G * 128:(g + 1) * G * 128].rearrange("(k p) n -> p k n", p=128)), writes=[kw + "3"])
            P.dma("pool", lambda e, g=g, b=b: e.dma_start(out=W2[b][:], in_=w2[g * G * 128:(g + 1) * G * 128, :].rearrange("(f p) n -> p f n", p=128)), writes=[kw + "2"])

        issue_w(0)
        for g in range(ng):
            b = g % 2
            kw = "%s_w%d" % (tag, b)
            if g + 1 < ng:
                issue_w(g + 1)
            ka = "%s_act%d" % (tag, b)
            for fi in range(G):
                for (a, bb, s) in tiles:
                    w = bb - a
                    pi = cnt % 2
                    cnt += 1
                    p1, p3 = self.psA[pi], self.psB[pi]
                    k1, k3 = "psA%d" % pi, "psB%d" % pi
                    def mm1(e, b=b, fi=fi, a=a, bb=bb, w=w, p1=p1):
                        r = None
                        for k in range(KC):
                            r = e.matmul(p1[:, 0:w], lhsT=W1[b][:, k, fi * 128:(fi + 1) * 128], rhs=self.H[:, k, a:bb], start=(k == 0), stop=(k == KC - 1))
                        return r
                    def mm3(e, b=b, fi=fi, a=a, bb=bb, w=w, p3=p3):
                        r = None
                        for k in range(KC):
                            r = e.matmul(p3[:, 0:w], lhsT=W3[b][:, k, fi * 128:(fi + 1) * 128], rhs=self.H[:, k, a:bb], start=(k == 0), stop=(k == KC - 1))
                        return r
                    P.op("pe", mm1, reads=[kw + "1", "H"], writes=[k1])
                    P.op("pe", mm3, reads=[kw + "3", "H"], writes=[k3])
                    s1 = S1[pi % len(S1)]
                    ks1 = "%s_s1_%d" % (tag, pi % len(S1))
                    P.op("act", lambda e, s1=s1, p1=p1, w=w: e.activation(out=s1[:, 0:w], in_=p1[:, 0:w], func=AF.Silu), reads=[k1], writes=[ks1])
                    if gbc is None:
                        P.op("dve", lambda e, b=b, fi=fi, a=a, bb=bb, w=w, s1=s1, p3=p3: e.tensor_tensor(out=ACT[b][:, fi, a:bb], in0=s1[:, 0:w], in1=p3[:, 0:w], op=ALU.mult),
                             reads=[ks1, k3], writes=[ka])
                    else:
                        s2 = s1
                        ks2 = ks1
                        P.op("dve", lambda e, w=w, s1=s1, s2=s2, p3=p3: e.tensor_tensor(out=s2[:, 0:w], in0=s1[:, 0:w], in1=p3[:, 0:w], op=ALU.mult),
                             reads=[ks1, k3], writes=[ks2])
                        P.op("dve", lambda e, b=b, fi=fi, a=a, bb=bb, w=w, s2=s2: e.tensor_tensor(out=ACT[b][:, fi, a:bb], in0=s2[:, 0:w], in1=gbc[:, a:bb], op=ALU.mult),
                             reads=[ks2, gbc_key], writes=[ka])
            for d in range(KC):
                for (a, bb, s) in tiles:
                    w = bb - a
                    ybanks = [(self.psC[0], "psC0"), (self.psC[1], "psC1"), (self.psA[2], "psA2"), (self.psB[2], "psB2")]
                    py, ky = ybanks[ycnt % 4]
                    ycnt += 1
                    def mmy(e, b=b, d=d, a=a, bb=bb, w=w, py=py):
                        r = None
                        for fi in range(G):
                            r = e.matmul(py[:, 0:w], lhsT=W2[b][:, fi, d * 128:(d + 1) * 128], rhs=ACT[b][:, fi, a:bb], start=(fi == 0), stop=(fi == G - 1))
                        return r
                    P.op("pe", mmy, reads=[kw + "2", ka], writes=[ky])
                    xk = "X_%d_%d" % (d, a)
                    if use_pool and ycnt % 3 == 0:
                        tb = TMPB[(ycnt // 3) % 2]
                        kt = "%s_tmpb%d" % (tag, (ycnt // 3) % 2)
                        P.op("act", lambda e, d=d, w=w, s=s, py=py, tb=tb: e.mul(out=tb[:, 0:w], in_=py[:, 0:w], mul=GT[:, d, s:s + 1]), reads=[ky] + modkeys, writes=[kt])
                        P.op("pool", lambda e, d=d, a=a, bb=bb, w=w, tb=tb: e.tensor_tensor(out=XT[:, d, a:bb], in0=XT[:, d, a:bb], in1=tb[:, 0:w], op=ALU.add), reads=[kt, xk], writes=[xk])
                    else:
                        P.op("dve", lambda e, d=d, a=a, bb=bb, w=w, s=s, py=py: e.scalar_tensor_tensor(out=XT[:, d, a:bb], in0=py[:, 0:w], scalar=GT[:, d, s:s + 1], in1=XT[:, d, a:bb], op0=ALU.mult, op1=ALU.add),
                             reads=[ky, xk] + modkeys, writes=[xk])
        self.full_barrier()
        es.close()

    def outproj_into_x(self, Wt, nk, rhs_fn, rhs_key, wkey, GT, modkeys, c0, c1, s):
        cfg, P = self.cfg, self.P
        cnt = 0
        for d in range(cfg.KC):
            for (a, bb) in col_tiles(c0, c1):
                w = bb - a
                pi = cnt % 2
                cnt += 1
                py = self.psC[pi]
                ky = "psC%d" % pi
                def mmy(e, d=d, a=a, bb=bb, w=w, py=py):
                    r = None
                    for ic in range(nk):
                        r = e.matmul(py[:, 0:w], lhsT=Wt[:, ic, d * 128:(d + 1) * 128], rhs=rhs_fn(ic, a - c0, bb - c0), start=(ic == 0), stop=(ic == nk - 1))
                    return r
                P.op("pe", mmy, reads=[wkey, rhs_key], writes=[ky])
                P.op("dve", lambda e, d=d, a=a, bb=bb, w=w, py=py: e.scalar_tensor_tensor(out=self.X[:, d, a:bb], in0=py[:, 0:w], scalar=GT[:, d, s:s + 1], in1=self.X[:, d, a:bb], op0=ALU.mult, op1=ALU.add),
                     reads=[ky, "X_%d_%d" % (d, a)] + modkeys, writes=["X_%d_%d" % (d, a)])

    def lru_params(self, j, prm):
        cfg, P = self.cfg, self.P
        KC = cfg.KC
        d = {}
        d["CW"] = self.sb("lruCW%d" % j, [128, 4, KC], F32)
        d["CB"] = self.sb("lruCB%d" % j, [128, KC], F32)
        d["BA"] = self.sb("lruBA%d" % j, [128, 2, KC], F32)
        d["BX"] = self.sb("lruBX%d" % j, [128, 2, KC], F32)
        d["C1"] = self.sb("lruC1%d" % j, [128, 2, KC], F32)
        d["C2"] = self.sb("lruC2%d" % j, [128, 2, KC], F32)
        with self.nc.allow_non_contiguous_dma(reason="tiny"):
            k_ = self.fm_rows(lambda i: d["CW"][:, i, :], prm["lru_conv_w"], 4, "lruprm%d_cw" % j)
            P.op("dve", lambda e: e.tensor_copy(out=d["CW"][:], in_=d["CW"][:]), reads=k_, writes=["lruprm%d_" % j + "cw"])
            P.dma("sp", lambda e: e.dma_start(out=d["CB"][:], in_=prm["lru_conv_b"].rearrange("(k p) -> p k", p=128), allow_slow_non_contiguous=True), writes=["lruprm%d_" % j + "cb"])
            k_ = self.fm_rows(lambda i: d["BA"][:, i, :], prm["lru_b_a"], 2, "lruprm%d_" % j + "ba")
            P.op("dve", lambda e: e.tensor_copy(out=d["BA"][:], in_=d["BA"][:]), reads=k_, writes=["lruprm%d_" % j + "ba"])
            k_ = self.fm_rows(lambda i: d["BX"][:, i, :], prm["lru_b_x"], 2, "lruprm%d_" % j + "bx")
            P.op("dve", lambda e: e.tensor_copy(out=d["BX"][:], in_=d["BX"][:]), reads=k_, writes=["lruprm%d_" % j + "bx"])
            k_ = self.fm_rows(lambda i: d["C1"][:, i, :], prm["lru_lambda"], 2, "lruprm%d_" % j + "c1")
            P.op("dve", lambda e: e.tensor_copy(out=d["C1"][:], in_=d["C1"][:]), reads=k_, writes=["lruprm%d_" % j + "c1"])
        P.op("act", lambda e: e.activation(out=d["C1"][:], in_=d["C1"][:], func=AF.Exp, scale=-1.0), reads=["lruprm%d_" % j + "c1"], writes=["lruprm%d_" % j + "c1"])
        P.op("act", lambda e: e.activation(out=d["C1"][:], in_=d["C1"][:], func=AF.Ln, bias=1.0, scale=1.0), reads=["lruprm%d_" % j + "c1"], writes=["lruprm%d_" % j + "c1"])
        P.op("dve", lambda e: e.tensor_scalar(out=d["C2"][:], in0=d["C1"][:], scalar1=-2.0 * LRU_C, scalar2=None, op0=ALU.mult), reads=["lruprm%d_" % j + "c1"], writes=["lruprm%d_" % j + "c2"])
        P.op("dve", lambda e: e.tensor_scalar(out=d["C1"][:], in0=d["C1"][:], scalar1=-LRU_C, scalar2=None, op0=ALU.mult), reads=["lruprm%d_" % j + "c1", "lruprm%d_" % j + "c2"], writes=["lruprm%d_" % j + "c1"])
        d["keys"] = ["lruprm%d_" % j + "cw", "lruprm%d_" % j + "cb", "lruprm%d_" % j + "ba", "lruprm%d_" % j + "bx", "lruprm%d_" % j + "c1", "lruprm%d_" % j + "c2"]
        return d

    def lru_pass(self, prm, lp, mods, c0, c1, is_ctx, phase, HH=None, EF=None, CARRY=None, CFIN=None, PSOUT=None, do_out=True, SPILL=None):
        cfg, P = self.cfg, self.P
        KC, HC = cfg.KC, cfg.HC
        W = c1 - c0
        s = 1 if is_ctx else 0
        es = ExitStack()
        WX = self.sb(self.key("WX"), [128, KC, HC * 128], BF16, es)
        WGt = self.sb(self.key("WG"), [128, 2, 2, HC, HC * 128], BF16, es)
        XB = self.sb(self.key("XB"), [128, HC, W + 3], F32, es)
        XC = self.sb(self.key("XC"), [128, HC, W], F32, es)
        XCb = self.sb(self.key("XCb"), [128, HC, W], BF16, es)
        spill_mode = (phase == 2 and not is_ctx and SPILL is not None)
        nbuf = 1 if spill_mode else 2
        Rl = [None if spill_mode else self.sb(self.key("R"), [128, W], F32, es) for _ in range(nbuf)]
        Al = [self.sb(self.key("A"), [128, W], F32, es) for _ in range(nbuf)]
        A2l = [self.sb(self.key("A2"), [128, W], F32, es) for _ in range(nbuf)]
        HS = self.sb(self.key("HS"), [128, 2, W], F32, es)
        SRl = [self.sb(self.key("SR"), [128, 1], F32, es) for _ in range(2)]
        need_out = do_out and phase == 2
        use_spill_load = (phase == 2 and not is_ctx and SPILL is not None)
        if need_out:
            WO = self.sb(self.key("WO"), [128, HC, cfg.D], BF16, es)
            GG = self.sb(self.key("GG"), [128, HC, W], BF16, es)
            YG = self.sb(self.key("YG"), [128, HC, W], BF16, es)
        tiles = col_tiles(0, W)
        w_in, w_a, w_x, w_out = prm["lru_w_in"], prm["lru_w_a"], prm["lru_w_x"], prm["lru_w_out"]
        lk = lp["keys"]
        for hd in range(cfg.HEADS):
            hcol = hd * HC * 128
            if not use_spill_load:
                P.dma("pool", lambda e, hcol=hcol: e.dma_start(out=WX[:], in_=w_in[:, hcol:hcol + HC * 128].rearrange("(k p) n -> p k n", p=128)), writes=["WX"])
                for dr in range(2):
                    P.dma("pool", lambda e, dr=dr, hd=hd: e.dma_start(out=WGt[:, dr, 0], in_=w_a[dr, hd].rearrange("(i p) n -> p i n", p=128)), writes=["WG%d0" % dr])
                    P.dma("pool", lambda e, dr=dr, hd=hd: e.dma_start(out=WGt[:, dr, 1], in_=w_x[dr, hd].rearrange("(i p) n -> p i n", p=128)), writes=["WG%d1" % dr])
            for oc in (range(HC) if not use_spill_load else ()):
                ch = hd * HC + oc
                for ti, (a, b) in enumerate(tiles):
                    ps = self.psA[ti % 3]
                    kp = "psA%d" % (ti % 3)
                    def mm(e, oc=oc, a=a, b=b, ps=ps):
                        r = None
                        for k in range(KC):
                            r = e.matmul(ps[:, 0:b - a], lhsT=WX[:, k, oc * 128:(oc + 1) * 128], rhs=self.H[:, k, c0 + a:c0 + b], start=(k == 0), stop=(k == KC - 1))
                        return r
                    P.op("pe", mm, reads=["WX", "H"], writes=[kp])
                    P.op("act", lambda e, oc=oc, a=a, b=b, ps=ps: e.copy(out=XB[:, oc, 2 + a:2 + b], in_=ps[:, 0:b - a]), reads=[kp], writes=["XB"])
                if is_ctx:
                    P.op("dve", lambda e, oc=oc: e.memset(XB[:, oc, 0:2], 0.0), writes=["XB"])
                    P.op("dve", lambda e, oc=oc: e.memset(XB[:, oc, W + 2:W + 3], 0.0), writes=["XB"])
                else:
                    ps = self.psB[0]
                    def mmh(e, oc=oc, ps=ps):
                        r = None
                        for k in range(KC):
                            r = e.matmul(ps[:, 0:3], lhsT=WX[:, k, oc * 128:(oc + 1) * 128], rhs=HH[:, k, :], start=(k == 0), stop=(k == KC - 1))
                        return r
                    P.op("pe", mmh, reads=["WX", "HH"], writes=["psB0"])
                    P.op("dve", lambda e, oc=oc, ps=ps: e.tensor_scalar(out=XB[:, oc, 0:2], in0=ps[:, 0:2], scalar1=EF[:, 0:1], scalar2=None, op0=ALU.mult), reads=["psB0", "EF"], writes=["XB"])
                    P.op("dve", lambda e, oc=oc, ps=ps: e.tensor_scalar(out=XB[:, oc, W + 2:W + 3], in0=ps[:, 2:3], scalar1=EF[:, 1:2], scalar2=None, op0=ALU.mult), reads=["psB0", "EF"], writes=["XB"])
                P.op("dve", lambda e, oc=oc, ch=ch: e.tensor_scalar(out=XC[:, oc, :], in0=XB[:, oc, 0:W], scalar1=lp["CW"][:, 0, ch:ch + 1], scalar2=lp["CB"][:, ch:ch + 1], op0=ALU.mult, op1=ALU.add),
                     reads=["XB"] + lk, writes=["XC"])
                for kk in range(1, 4):
                    P.op("dve", lambda e, oc=oc, ch=ch, kk=kk: e.scalar_tensor_tensor(out=XC[:, oc, :], in0=XB[:, oc, kk:kk + W], scalar=lp["CW"][:, kk, ch:ch + 1], in1=XC[:, oc, :], op0=ALU.mult, op1=ALU.add),
                         reads=["XB", "XC"] + lk, writes=["XC"])
                P.op("act", lambda e, oc=oc: e.copy(out=XCb[:, oc, :], in_=XC[:, oc, :]), reads=["XC"], writes=["XCb"])
            if need_out:
                P.dma("pool", lambda e, hcol=hcol: e.dma_start(out=WX[:], in_=w_in[:, cfg.D + hcol:cfg.D + hcol + HC * 128].rearrange("(k p) n -> p k n", p=128)), writes=["WX"])
                P.dma("pool", lambda e, hcol=hcol: e.dma_start(out=WO[:], in_=w_out[hcol:hcol + HC * 128, :].rearrange("(i p) n -> p i n", p=128)), writes=["WO"])
                for oc in range(HC):
                    for ti, (a, b) in enumerate(tiles):
                        ps = self.psA[ti % 3]
                        kp = "psA%d" % (ti % 3)
                        def mmg(e, oc=oc, a=a, b=b, ps=ps):
                            r = None
                            for k in range(KC):
                                r = e.matmul(ps[:, 0:b - a], lhsT=WX[:, k, oc * 128:(oc + 1) * 128], rhs=self.H[:, k, c0 + a:c0 + b], start=(k == 0), stop=(k == KC - 1))
                            return r
                        P.op("pe", mmg, reads=["WX", "H"], writes=[kp])
                        P.op("act", lambda e, oc=oc, a=a, b=b, ps=ps: e.activation(out=GG[:, oc, a:b], in_=ps[:, 0:b - a], func=AF.Gelu_apprx_tanh), reads=[kp], writes=["GG"])
            for oc in range(HC):
                ch = hd * HC + oc
                def chain(dr, R, A, A2, SR, kR, kA, kA2, kSR, oc=oc, ch=ch):
                    I = R
                    si = (ch * 2 + dr) * 2
                    if use_spill_load:
                        P.dma("sp", lambda e, si=si: e.dma_start(out=A[:], in_=SPILL[si]), reads=["spill%d" % si], writes=[kA])
                        P.dma("sp", lambda e, si=si: e.dma_start(out=A2[:], in_=SPILL[si + 1]), reads=["spill%d" % (si + 1)], writes=[kA2])
                    else:
                        def gate_mm(gi, oc=oc, dr=dr, ch=ch):
                            for ti, (a, b) in enumerate(tiles):
                                ps = self.psB[ti % 3]
                                kp = "psB%d" % (ti % 3)
                                def mmz(e, oc=oc, dr=dr, gi=gi, a=a, b=b, ps=ps):
                                    r = None
                                    for ic in range(HC):
                                        r = e.matmul(ps[:, 0:b - a], lhsT=WGt[:, dr, gi, ic, oc * 128:(oc + 1) * 128], rhs=XCb[:, ic, a:b], start=(ic == 0), stop=(ic == HC - 1))
                                    return r
                                P.op("pe", mmz, reads=["WG%d%d" % (dr, gi), "XCb"], writes=[kp])
                                bias = lp["BA"] if gi == 0 else lp["BX"]
                                P.op("act", lambda e, ch=ch, dr=dr, a=a, b=b, ps=ps, bias=bias: e.activation(out=R[:, a:b], in_=ps[:, 0:b - a], func=AF.Sigmoid, bias=bias[:, dr, ch:ch + 1], scale=1.0), reads=[kp] + lk, writes=[kR])
                        gate_mm(0)
                        P.op("act", lambda e, ch=ch, dr=dr: e.activation(out=A[:], in_=R[:], func=AF.Exp, scale=lp["C1"][:, dr, ch:ch + 1]), reads=[kR] + lk, writes=[kA])
                        P.op("act", lambda e, ch=ch, dr=dr: e.activation(out=A2[:], in_=R[:], func=AF.Exp, scale=lp["C2"][:, dr, ch:ch + 1]), reads=[kR] + lk, writes=[kA2])
                        if phase == 1:
                            P.op("dve", lambda e: e.reduce_sum(out=SR[:], in_=R[:], axis=AX.X), reads=[kR], writes=[kSR])
                            P.op("act", lambda e, dr=dr, ch=ch: e.activation(out=PSOUT[:, dr, 0, ch:ch + 1], in_=SR[:], func=AF.Exp, scale=lp["C1"][:, dr, ch:ch + 1]), reads=[kSR] + lk, writes=["PSOUT"])
                        gate_mm(1)
                        P.op("act", lambda e: e.activation(out=A2[:], in_=A2[:], func=AF.Ln, bias=1.0, scale=-1.0), reads=[kA2], writes=[kA2])
                        P.op("act", lambda e: e.activation(out=A2[:], in_=A2[:], func=AF.Exp, scale=0.5), reads=[kA2], writes=[kA2])
                        P.op("dve", lambda e: e.tensor_tensor(out=A2[:], in0=A2[:], in1=I[:], op=ALU.mult), reads=[kA2, kR], writes=[kA2])
                        P.op("dve", lambda e, oc=oc: e.tensor_tensor(out=A2[:], in0=A2[:], in1=XC[:, oc, :], op=ALU.mult), reads=[kA2, "XC"], writes=[kA2])
                        if phase == 1 and SPILL is not None:
                            P.dma("sp", lambda e, si=si: e.dma_start(out=SPILL[si], in_=A[:]), reads=[kA], writes=["spill%d" % si])
                            P.dma("sp", lambda e, si=si: e.dma_start(out=SPILL[si + 1], in_=A2[:]), reads=[kA2], writes=["spill%d" % (si + 1)])
                    if phase == 1:
                        init = 0.0
                        rk = []
                    elif is_ctx:
                        init = 0.0
                        rk = []
                    else:
                        init = CARRY[:, dr, ch:ch + 1]
                        rk = ["CARRY"]
                    if dr == 0:
                        P.op("dve", lambda e, init=init: e.tensor_tensor_scan(out=HS[:, 0, :], data0=A[:], data1=A2[:], initial=init, op0=ALU.mult, op1=ALU.add), reads=[kA, kA2] + rk, writes=["HS0"])
                    else:
                        P.op("dve", lambda e, init=init: e.tensor_tensor_scan(out=HS[:, 1, ::-1], data0=A[:, ::-1], data1=A2[:, ::-1], initial=init, op0=ALU.mult, op1=ALU.add), reads=[kA, kA2] + rk, writes=["HS1"])
                    fin = (W - 1) if dr == 0 else 0
                    if phase == 1:
                        P.op("dve", lambda e, dr=dr, ch=ch, fin=fin: e.tensor_copy(out=PSOUT[:, dr, 1, ch:ch + 1], in_=HS[:, dr, fin:fin + 1]), reads=["HS%d" % dr], writes=["PSOUT"])
                    elif is_ctx:
                        P.op("dve", lambda e, dr=dr, ch=ch, fin=fin: e.tensor_copy(out=CFIN[:, dr, ch:ch + 1], in_=HS[:, dr, fin:fin + 1]), reads=["HS%d" % dr], writes=["CFIN"])
                for dr in range(2):
                    b_ = dr % nbuf
                    chain(dr, Rl[b_], Al[b_], A2l[b_], SRl[dr], "R%d" % b_, "A%d" % b_, "A2%d" % b_, "SR%d" % dr)
                if need_out:
                    P.op("dve", lambda e: e.tensor_tensor(out=HS[:, 0, :], in0=HS[:, 0, :], in1=HS[:, 1, :], op=ALU.add), reads=["HS0", "HS1"], writes=["HS0"])
                    P.op("dve", lambda e, oc=oc: e.tensor_tensor(out=YG[:, oc, :], in0=HS[:, 0, :], in1=GG[:, oc, :], op=ALU.mult), reads=["HS0", "GG"], writes=["YG"])
            if need_out:
                self.outproj_into_x(WO, HC, lambda ic, a, b: YG[:, ic, a:b], "YG", "WO", mods["GT_A"], mods["keys"], c0, c1, s)
        self.full_barrier()
        es.close()

    def sgu_params(self, prm, pstack):
        cfg, P = self.cfg, self.P
        KC, SG = cfg.KC, cfg.SGROUPS
        d = {}
        d["WST"] = self.sb(self.key("sguWST"), [128, SG, 128], BF16, pstack)
        d["LG"] = self.sb(self.key("sguLG"), [128, KC], F32, pstack)
        d["CC"] = self.sb(self.key("sguCC"), [128, KC, 128], F32, pstack)
        es = ExitStack()
        WSn = self.sb(self.key("sguWSn"), [128, SG, 128], F32, es)
        LB = self.sb(self.key("sguLB"), [128, KC], F32, es)
        BS = self.sb(self.key("sguBS"), [128, SG, 128], F32, es)
        P.dma("sp", lambda e: e.dma_start(out=WSn[:], in_=prm["sgu_w_s"].rearrange("g p q -> p g q")), writes=["sguWSn"])
        with self.nc.allow_non_contiguous_dma(reason="tiny"):
            P.dma("sp", lambda e: e.dma_start(out=d["LG"][:], in_=prm["sgu_ln_g"].rearrange("(k p) -> p k", p=128), allow_slow_non_contiguous=True), writes=["sguLG"])
            P.dma("sp", lambda e: e.dma_start(out=LB[:], in_=prm["sgu_ln_b"].rearrange("(k p) -> p k", p=128), allow_slow_non_contiguous=True), writes=["sguLB"])
        bsrc = prm["sgu_b_s"]
        bs_bc = bass.AP(bsrc.tensor, bsrc.offset, [[0, 128], [1, SG * 128]])
        P.dma("sp", lambda e: e.dma_start(out=BS[:].rearrange("p g q -> p (g q)"), in_=bs_bc), writes=["sguBS"])
        for g in range(SG):
            ps = self.psA[g % 2]
            kp = "psA%d" % (g % 2)
            P.op("pe", lambda e, g=g, ps=ps: e.transpose(ps[:, 0:128], WSn[:, g, :], self.ident[:]), reads=["sguWSn", "ident"], writes=[kp])
            P.op("act", lambda e, g=g, ps=ps: e.copy(out=d["WST"][:, g, :], in_=ps[:, 0:128]), reads=[kp], writes=["sguWST"])
            ps2 = self.psB[g % 2]
            kp2 = "psB%d" % (g % 2)
            P.op("pe", lambda e, g=g, ps2=ps2: e.matmul(ps2[:, 0:128], lhsT=self.ones16[:], rhs=d["WST"][:, g, :], start=True, stop=True), reads=["sguWST", "ones16"], writes=[kp2])
            for cc in range(cfg.GCH):
                ch = g * cfg.GCH + cc
                P.op("dve", lambda e, g=g, ch=ch, ps2=ps2: e.scalar_tensor_tensor(out=d["CC"][:, ch, :], in0=ps2[:, 0:128], scalar=LB[:, ch:ch + 1], in1=BS[:, g, :], op0=ALU.mult, op1=ALU.add),
                     reads=[kp2, "sguLB", "sguBS"], writes=["sguCC"])
        d["keys"] = ["sguWST", "sguLG", "sguCC"]
        self.full_barrier()
        es.close()
        return d

    def sgu_pass(self, prm, sp, mods, c0, c1, is_ctx):
        cfg, P = self.cfg, self.P
        KC, GCH, D = cfg.KC, cfg.GCH, cfg.D
        W = c1 - c0
        NT = W // 128
        s = 1 if is_ctx else 0
        w_in, w_out = prm["sgu_w_in"], prm["sgu_w_out"]
        es = ExitStack()
        VT = self.sb(self.key("VT"), [128, NT, D], BF16, es)
        VW = GCH * 128
        WV = [self.sb(self.key("WV"), [128, KC, VW], BF16, es) for _ in range(2)]
        SQ = self.sb(self.key("SQ"), [128, D], BF16, es)
        ST = self.sb(self.key("ST"), [128, NT, 4], F32, es)
        WU = WV[0]
        WO = WV[1][:].rearrange("p k n -> p (k n)").rearrange("p (i n) -> p i n", i=GCH)
        U = self.sb(self.key("U"), [128, GCH, W], BF16, es)
        VM = self.sb(self.key("VM"), [128, 512], F32, es)
        if GCH * W <= D:
            PR = SQ[:, 0:GCH * W].rearrange("p (c w) -> p c w", c=GCH)
        else:
            PR = self.sb(self.key("PR"), [128, GCH, W], BF16, es)
        for vt in range(D // VW):
            wb = WV[vt % 2]
            kw = "WV%d" % (vt % 2)
            P.dma("pool", lambda e, vt=vt, wb=wb: e.dma_start(out=wb[:], in_=w_in[:, D + vt * VW:D + (vt + 1) * VW].rearrange("(k p) n -> p k n", p=128)), writes=[kw])
            for n in range(NT):
                ps = self.psA[n % 3]
                kp = "psA%d" % (n % 3)
                def mmv(e, n=n, wb=wb, ps=ps):
                    r = None
                    for k in range(KC):
                        r = e.matmul(ps[:, 0:VW], lhsT=self.H[:, k, c0 + n * 128:c0 + (n + 1) * 128], rhs=wb[:, k, :], start=(k == 0), stop=(k == KC - 1))
                    return r
                P.op("pe", mmv, reads=[kw, "H"], writes=[kp])
                P.op("act", lambda e, n=n, vt=vt, ps=ps: e.activation(out=VT[:, n, vt * VW:(vt + 1) * VW], in_=ps[:, 0:VW], func=AF.Gelu_apprx_tanh), reads=[kp], writes=["VT"])
        for n in range(NT):
            P.op("dve", lambda e, n=n: e.reduce_sum(out=ST[:, n, 0:1], in_=VT[:, n, :], axis=AX.X), reads=["VT"], writes=["ST"])
            P.op("dve", lambda e, n=n: e.tensor_tensor(out=SQ[:], in0=VT[:, n, :], in1=VT[:, n, :], op=ALU.mult), reads=["VT"], writes=["SQ"])
            P.op("dve", lambda e, n=n: e.reduce_sum(out=ST[:, n, 1:2], in_=SQ[:], axis=AX.X), reads=["SQ"], writes=["ST"])
        P.op("dve", lambda e: e.tensor_scalar(out=ST[:, :, 2:3], in0=ST[:, :, 0:1], scalar1=1.0 / D, scalar2=None, op0=ALU.mult), reads=["ST"], writes=["ST"])
        P.op("dve", lambda e: e.tensor_tensor(out=ST[:, :, 3:4], in0=ST[:, :, 2:3], in1=ST[:, :, 2:3], op=ALU.mult), reads=["ST"], writes=["ST"])
        P.op("dve", lambda e: e.scalar_tensor_tensor(out=ST[:, :, 3:4], in0=ST[:, :, 1:2], scalar=1.0 / D, in1=ST[:, :, 3:4], op0=ALU.mult, op1=ALU.subtract), reads=["ST"], writes=["ST"])
        P.op("dve", lambda e: e.tensor_scalar(out=ST[:, :, 3:4], in0=ST[:, :, 3:4], scalar1=EPS, scalar2=None, op0=ALU.add), reads=["ST"], writes=["ST"])
        P.op("act", lambda e: e.activation(out=ST[:, :, 3:4], in_=ST[:, :, 3:4], func=AF.Ln), reads=["ST"], writes=["ST"])
        P.op("act", lambda e: e.activation(out=ST[:, :, 3:4], in_=ST[:, :, 3:4], func=AF.Exp, scale=-0.5), reads=["ST"], writes=["ST"])
        for n in range(NT):
            P.op("dve", lambda e, n=n: e.tensor_scalar(out=VT[:, n, :], in0=VT[:, n, :], scalar1=ST[:, n, 2:3], scalar2=ST[:, n, 3:4], op0=ALU.subtract, op1=ALU.mult), reads=["VT", "ST"], writes=["VT"])
        NB = 4
        for g in range(cfg.SGROUPS):
            gcol = g * GCH * 128
            P.dma("pool", lambda e, gcol=gcol: e.dma_start(out=WU[:], in_=w_in[:, gcol:gcol + GCH * 128].rearrange("(k p) n -> p k n", p=128)), writes=["WV0"])
            P.dma("pool", lambda e, gcol=gcol: e.dma_start(out=WO, in_=w_out[gcol:gcol + GCH * 128, :].rearrange("(i p) n -> p i n", p=128)), writes=["WV1"])
            for cc in range(GCH):
                ch = g * GCH + cc
                for ti, (a, b) in enumerate(col_tiles(0, W)):
                    ps = self.psA[ti % 3]
                    kp = "psA%d" % (ti % 3)
                    def mmu(e, cc=cc, a=a, b=b, ps=ps):
                        r = None
                        for k in range(KC):
                            r = e.matmul(ps[:, 0:b - a], lhsT=WU[:, k, cc * 128:(cc + 1) * 128], rhs=self.H[:, k, c0 + a:c0 + b], start=(k == 0), stop=(k == KC - 1))
                        return r
                    P.op("pe", mmu, reads=["WV0", "H"], writes=[kp])
                    P.op("act", lambda e, cc=cc, a=a, b=b, ps=ps: e.activation(out=U[:, cc, a:b], in_=ps[:, 0:b - a], func=AF.Gelu_apprx_tanh), reads=[kp], writes=["U"])
                for n0 in range(0, NT, NB):
                    nb = min(NB, NT - n0)
                    pi = (n0 // NB) % 3
                    ps = self.psB[pi]
                    kp = "psB%d" % pi
                    def mms(e, g=g, ch=ch, n0=n0, nb=nb, ps=ps):
                        r = None
                        for i in range(nb):
                            r = e.matmul(ps[:, i * 128:(i + 1) * 128], lhsT=VT[:, n0 + i, ch * 128:(ch + 1) * 128], rhs=sp["WST"][:, g, :], start=True, stop=True)
                        return r
                    P.op("pe", mms, reads=["VT"] + sp["keys"], writes=[kp])
                    for i in range(nb):
                        P.op("dve", lambda e, ch=ch, i=i, ps=ps: e.scalar_tensor_tensor(out=VM[:, i * 128:(i + 1) * 128], in0=ps[:, i * 128:(i + 1) * 128], scalar=sp["LG"][:, ch:ch + 1], in1=sp["CC"][:, ch, :], op0=ALU.mult, op1=ALU.add),
                             reads=[kp] + sp["keys"], writes=["VM"])
                    P.op("dve", lambda e, cc=cc, n0=n0, nb=nb: e.tensor_tensor(out=PR[:, cc, n0 * 128:(n0 + nb) * 128], in0=VM[:, 0:nb * 128], in1=U[:, cc, n0 * 128:(n0 + nb) * 128], op=ALU.mult),
                         reads=["VM", "U", "SQ"], writes=["PR", "SQ"])
            self.outproj_into_x(WO, GCH, lambda ic, a, b: PR[:, ic, a:b], "PR", "WV1", mods["GT_A"], mods["keys"], c0, c1, s)
        self.full_barrier()
        es.close()

    def moe_gates(self, router, mods, GATE=None):
        cfg, P = self.cfg, self.P
        KC, T, NE = cfg.KC, cfg.T, cfg.NEXP
        NTC = T // 128
        if GATE is None:
            GATE = self.sb("GATE", [128, NTC, NE], F32)
        es = ExitStack()
        Rt = self.sb(self.key("Rt"), [128, KC, NE], F32, es)
        RG = self.sb(self.key("RG"), [128, 2, KC, NE], F32, es)
        RS = self.sb(self.key("RS"), [128, 2, KC, NE], F32, es)
        CONST = self.sb(self.key("CONST"), [128, 2, NE], F32, es)
        RT = self.sb(self.key("RTK"), [128, NTC], F32, es)
        L = self.sb(self.key("L"), [128, NTC, NE], F32, es)
        L2 = self.sb(self.key("L2"), [128, NTC, NE], F32, es)
        E1 = self.sb(self.key("E1"), [128, NTC, NE], F32, es)
        E2 = self.sb(self.key("E2"), [128, NTC, NE], F32, es)
        M = self.sb(self.key("M"), [128, 4, NTC], F32, es)
        P.dma("sp", lambda e: e.dma_start(out=Rt[:], in_=router.rearrange("(k p) n -> p k n", p=128)), writes=["Rt"])
        for s in range(2):
            for k in range(KC):
                P.op("dve", lambda e, s=s, k=k: e.tensor_scalar(out=RG[:, s, k, :], in0=Rt[:, k, :], scalar1=mods["GS_B"][:, k, s:s + 1], scalar2=None, op0=ALU.mult), reads=["Rt"] + mods["keys"], writes=["RG"])
                P.op("dve", lambda e, s=s, k=k: e.tensor_scalar(out=RS[:, s, k, :], in0=Rt[:, k, :], scalar1=mods["SH_B"][:, k, s:s + 1], scalar2=None, op0=ALU.mult), reads=["Rt"] + mods["keys"], writes=["RS"])
            ps = self.psC[s]
            def mmc(e, s=s, ps=ps):
                r = None
                for k in range(KC):
                    r = e.matmul(ps[:, 0:NE], lhsT=self.ones32[:], rhs=RS[:, s, k, :], start=(k == 0), stop=(k == KC - 1))
                return r
            P.op("pe", mmc, reads=["RS", "ones32"], writes=["psC%d" % s])
            P.op("dve", lambda e, s=s, ps=ps: e.tensor_copy(out=CONST[:, s, :], in_=ps[:, 0:NE]), reads=["psC%d" % s], writes=["CONST"])
        for n in range(NTC):
            s = 1 if n * 128 < cfg.NCX else 0
            ps = self.psA[n % 3]
            kp = "psA%d" % (n % 3)
            def mml(e, n=n, s=s, ps=ps):
                r = None
                for k in range(KC):
                    r = e.matmul(ps[:, 0:NE], lhsT=self.X[:, k, n * 128:(n + 1) * 128], rhs=RG[:, s, k, :], start=(k == 0), stop=(k == KC - 1))
                r = e.matmul(ps[:, 16:17], lhsT=self.RSTD[0:1, n * 128:(n + 1) * 128], rhs=self.ones32[0:1, 0:1], start=True, stop=True)
                return r
            P.op("pe", mml, reads=["X", "RG", "RSTD", "ones32"], writes=[kp])
            P.op("dve", lambda e, n=n, ps=ps: e.tensor_copy(out=RT[:, n:n + 1], in_=ps[:, 16:17]), reads=[kp], writes=["RTK"])
            P.op("dve", lambda e, n=n, s=s, ps=ps: e.scalar_tensor_tensor(out=L[:, n, :], in0=ps[:, 0:NE], scalar=RT[:, n:n + 1], in1=CONST[:, s, :], op0=ALU.mult, op1=ALU.add), reads=[kp, "RTK", "CONST"], writes=["L"])
        P.op("dve", lambda e: e.reduce_max(out=M[:, 0, :], in_=L[:], axis=AX.X), reads=["L"], writes=["M0"])
        P.op("dve", lambda e: e.tensor_tensor(out=E1[:], in0=L[:], in1=M[:, 0, :].unsqueeze(2).to_broadcast([128, NTC, NE]), op=ALU.is_equal), reads=["L", "M0"], writes=["E1"])
        P.op("dve", lambda e: e.scalar_tensor_tensor(out=L2[:], in0=E1[:], scalar=-1e30, in1=L[:], op0=ALU.mult, op1=ALU.add), reads=["E1", "L"], writes=["L2"])
        P.op("dve", lambda e: e.reduce_max(out=M[:, 1, :], in_=L2[:], axis=AX.X), reads=["L2"], writes=["M1"])
        P.op("dve", lambda e: e.tensor_tensor(out=E2[:], in0=L2[:], in1=M[:, 1, :].unsqueeze(2).to_broadcast([128, NTC, NE]), op=ALU.is_equal), reads=["L2", "M1"], writes=["E2"])
        P.op("dve", lambda e: e.tensor_tensor(out=M[:, 2, :], in0=M[:, 1, :], in1=M[:, 0, :], op=ALU.subtract), reads=["M0", "M1"], writes=["M2"])
        P.op("act", lambda e: e.activation(out=M[:, 3, :], in_=M[:, 2, :], func=AF.Exp), reads=["M2"], writes=["M3"])
        P.op("dve", lambda e: e.tensor_scalar(out=M[:, 2, :], in0=M[:, 3, :], scalar1=1.0, scalar2=None, op0=ALU.add), reads=["M3"], writes=["M2"])
        P.op("dve", lambda e: e.reciprocal(out=M[:, 2, :], in_=M[:, 2, :]), reads=["M2"], writes=["M2"])
        P.op("dve", lambda e: e.tensor_tensor(out=M[:, 3, :], in0=M[:, 3, :], in1=M[:, 2, :], op=ALU.mult), reads=["M2", "M3"], writes=["M3"])
        P.op("dve", lambda e: e.tensor_tensor(out=E1[:], in0=E1[:], in1=M[:, 2, :].unsqueeze(2).to_broadcast([128, NTC, NE]), op=ALU.mult), reads=["E1", "M2"], writes=["E1"])
        P.op("dve", lambda e: e.tensor_tensor(out=E2[:], in0=E2[:], in1=M[:, 3, :].unsqueeze(2).to_broadcast([128, NTC, NE]), op=ALU.mult), reads=["E2", "M3"], writes=["E2"])
        P.op("dve", lambda e: e.tensor_tensor(out=GATE[:], in0=E1[:], in1=E2[:], op=ALU.add), reads=["E1", "E2"], writes=["GATE"])
        self.full_barrier()
        es.close()
        return GATE

    def gate_bcast_keyed(self, GATE, ex, GBC, DG, gkey):
        return self.gate_bcast(GATE, ex, GBC, DG, gkey)

    def gate_bcast(self, GATE, ex, GBC, DG, gkey="GATE"):
        cfg, P = self.cfg, self.P
        NTC = cfg.T // 128
        for n0 in range(0, NTC, 4):
            nb = min(4, NTC - n0)
            pi = (n0 // 4) % 3
            ps = self.psB[pi]
            kp = "psB%d" % pi
            for i in range(nb):
                P.op("dve", lambda e, i=i, n0=n0: e.tensor_scalar(out=DG[:, i, :], in0=self.ident[:], scalar1=GATE[:, n0 + i, ex:ex + 1], scalar2=None, op0=ALU.mult), reads=["ident", gkey], writes=["DG%d" % i])
            def mmb(e, nb=nb, ps=ps):
                r = None
                for i in range(nb):
                    r = e.matmul(ps[:, i * 128:(i + 1) * 128], lhsT=self.ones32[:], rhs=DG[:, i, :], start=True, stop=True)
                return r
            P.op("pe", mmb, reads=["ones32"] + ["DG%d" % i for i in range(nb)], writes=[kp])
            P.op("act", lambda e, n0=n0, nb=nb, ps=ps: e.copy(out=GBC[:, n0 * 128:(n0 + nb) * 128], in_=ps[:, 0:nb * 128]), reads=[kp], writes=["GBC"])

    def finish(self):
        with self.nc.Block() as block:
            self.P.replay(block)
        self.es.close()
        return self.nc


LRU_KEYS = ["lru_w_in", "lru_conv_w", "lru_conv_b", "lru_w_a", "lru_b_a", "lru_w_x", "lru_b_x", "lru_lambda", "lru_w_out"]


def build_stage(cfg, stage):
    B = Builder(cfg, stage)
    nc, P = B.nc, B.P
    D, KC, T, NL, NCX, F = cfg.D, cfg.KC, cfg.T, cfg.NL, cfg.NCX, cfg.F
    HD = D // cfg.HEADS
    xin = B.din("xin", [NL, D])
    xhalo = B.din("xhalo", [3, D])
    cin = B.din("cin", [NCX, D])
    ef = B.din("ef", [128, 2])
    c_v = B.din("c", [D])
    cctx_v = B.din("c_ctx", [D])
    li0 = 0 if stage in (1, 2) else 2
    layers = [li0] if stage in (1, 3) else [li0, li0 + 1]
    w_mod = {li: B.din("w_mod%d" % li, [D, 6 * D]) for li in layers}
    b_mod = {li: B.din("b_mod%d" % li, [6 * D]) for li in layers}
    norm_g = {li: B.din("norm_g%d" % li, [2, D]) for li in layers}
    prm = {}
    shapes = {"lru_w_in": [D, 2 * D], "lru_conv_w": [4, D], "lru_conv_b": [D], "lru_w_a": [2, cfg.HEADS, HD, HD], "lru_b_a": [2, D],
              "lru_w_x": [2, cfg.HEADS, HD, HD], "lru_b_x": [2, D], "lru_lambda": [2, D], "lru_w_out": [D, D]}
    for k in LRU_KEYS:
        prm[k] = B.din(k, shapes[k])
    if stage in (2, 4):
        pscar = B.din("carry_ps", [2, 7, 2, D])
        prm["ffn_w1"] = B.din("ffn_w1", [D, F])
        prm["ffn_w3"] = B.din("ffn_w3", [D, F])
        prm["ffn_w2"] = B.din("ffn_w2", [F, D])
        prm["sgu_w_in"] = B.din("sgu_w_in", [D, 2 * D])
        prm["sgu_ln_g"] = B.din("sgu_ln_g", [D])
        prm["sgu_ln_b"] = B.din("sgu_ln_b", [D])
        prm["sgu_w_s"] = B.din("sgu_w_s", [cfg.SGROUPS, 128, 128])
        prm["sgu_b_s"] = B.din("sgu_b_s", [cfg.SGROUPS, 128])
        prm["sgu_w_out"] = B.din("sgu_w_out", [D, D])
        prm["moe_router"] = B.din("moe_router", [D, cfg.NEXP])
        prm["moe_w1"] = B.din("moe_w1", [cfg.NEXP, D, F])
        prm["moe_w3"] = B.din("moe_w3", [cfg.NEXP, D, F])
        prm["moe_w2"] = B.din("moe_w2", [cfg.NEXP, F, D])
    if stage == 4:
        final_g = B.din("final_g", [D])
    B.setup_common()
    X = B.X
    XH = B.sb("XH", [128, KC, 3], F32)
    HH = B.sb("HH", [128, KC, 3], BF16)
    EF = B.sb("EF", [128, 2], F32)
    es = ExitStack()
    TM = [B.sb("TM%d" % i, [128, D], F32, es) for i in range(2)]
    P.dma("sp", lambda e: e.dma_start(out=EF[:], in_=ef), writes=["EF"])
    NTC = T // 128
    for n in range(NTC):
        tm = TM[n % 2]
        kt = "TM%d" % (n % 2)
        src = cin[n * 128:(n + 1) * 128, :] if n * 128 < NCX else xin[n * 128 - NCX:(n + 1) * 128 - NCX, :]
        P.dma("sp", lambda e, tm=tm, src=src: e.dma_start(out=tm[:], in_=src), writes=[kt])
        for k0 in range(0, KC, 4):
            nk = min(4, KC - k0)
            pi = (k0 // 4) % 3
            ps = B.psA[pi]
            kp = "psA%d" % pi
            def tr(e, tm=tm, k0=k0, nk=nk, ps=ps):
                r = None
                for i in range(nk):
                    r = e.transpose(ps[:, i * 128:(i + 1) * 128], tm[:, (k0 + i) * 128:(k0 + i + 1) * 128], B.ident[:])
                return r
            P.op("pe", tr, reads=[kt, "ident"], writes=[kp])
            P.op("act", lambda e, n=n, k0=k0, nk=nk, ps=ps: e.copy(out=X[:, k0:k0 + nk, n * 128:(n + 1) * 128], in_=ps[:, 0:nk * 128].rearrange("p (k t) -> p k t", k=nk)), reads=[kp], writes=["X"])
    with nc.allow_non_contiguous_dma(reason="3 halo rows"):
        k_ = B.fm_rows(lambda i: XH[:, :, i], xhalo, 3, "XH")
        P.op("dve", lambda e: e.tensor_copy(out=XH[:], in_=XH[:]), reads=k_, writes=["XH"])
    B.full_barrier()
    es.close()

    mods = {li: B.compute_mods(li, w_mod[li], b_mod[li], c_v, cctx_v, norm_g[li]) for li in layers}
    m0 = mods[li0]
    B.norm_mod(m0["GS_A"], m0["SH_A"], m0["keys"])
    es = ExitStack()
    sqh = B.sb("sqh", [128, KC, 3], F32, es)
    rh = B.sb("rh", [128, 3], F32, es)
    th = B.sb("th", [128, KC, 3], F32, es)
    P.op("dve", lambda e: e.tensor_tensor(out=sqh[:], in0=XH[:], in1=XH[:], op=ALU.mult), reads=["XH"], writes=["sqh"])
    def mmh(e):
        r = None
        for k in range(KC):
            r = e.matmul(B.psC[0][:, 0:3], lhsT=B.ones32[:], rhs=sqh[:, k, :], start=(k == 0), stop=(k == KC - 1))
        return r
    P.op("pe", mmh, reads=["sqh", "ones32"], writes=["psC0"])
    P.op("dve", lambda e: e.tensor_scalar(out=rh[:], in0=B.psC[0][:, 0:3], scalar1=1.0 / D, scalar2=EPS, op0=ALU.mult, op1=ALU.add), reads=["psC0"], writes=["rh"])
    P.op("act", lambda e: e.activation(out=rh[:], in_=rh[:], func=AF.Ln), reads=["rh"], writes=["rh"])
    P.op("act", lambda e: e.activation(out=rh[:], in_=rh[:], func=AF.Exp, scale=-0.5), reads=["rh"], writes=["rh"])
    for k in range(KC):
        P.op("dve", lambda e, k=k: e.scalar_tensor_tensor(out=th[:, k, :], in0=XH[:, k, :], scalar=m0["GS_A"][:, k, 0:1], in1=rh[:], op0=ALU.mult, op1=ALU.mult), reads=["XH", "rh"] + m0["keys"], writes=["th"])
        P.op("act", lambda e, k=k: e.activation(out=HH[:, k, :], in_=th[:, k, :], func=AF.Identity, bias=m0["SH_A"][:, k, 0:1], scale=1.0), reads=["th"] + m0["keys"], writes=["HH"])
    B.full_barrier()
    es.close()

    lp = B.lru_params(0, prm)
    CFIN = B.sb("CFIN", [128, 2, KC], F32)
    ctx_out = (li0 == 0)
    if stage in (1, 3):
        PSO = B.sb("PSO", [128, 2, 2, KC], F32)
        B.lru_pass(prm, lp, m0, NCX, T, False, 1, HH=HH, EF=EF, PSOUT=PSO)
        pso = B.dout("ps_out", [2, 2, D])
        with nc.allow_non_contiguous_dma(reason="tiny"):
            ok_ = []
            for a_ in range(2):
                for b_ in range(2):
                    P.dma("sp", lambda e, a_=a_, b_=b_: e.dma_start(out=pso[a_, b_].rearrange("(k p) -> p k", p=128), in_=PSO[:, a_, b_, :], allow_slow_non_contiguous=True), reads=["PSOUT"], writes=["o_ps%d%d" % (a_, b_)])
                    ok_.append("o_ps%d%d" % (a_, b_))
        P.wait_all("sp", ok_)
        return B.finish(), B

    B.lru_pass(prm, lp, m0, 0, NCX, True, 2, CFIN=CFIN, do_out=ctx_out)
    CAR = B.sb("CAR", [128, 2, KC], F32)
    PSC = B.sb("PSC", [128, 2, 7, 2, KC], F32)
    with nc.allow_non_contiguous_dma(reason="tiny"):
        for dr in range(2):
            k_ = B.fm_rows(lambda i, dr=dr: PSC[:, dr, i // 2, i % 2, :], pscar[dr].rearrange("s b d -> (s b) d"), 14, "PSC%d" % dr)
            P.op("dve", lambda e, dr=dr: e.tensor_copy(out=PSC[:, dr], in_=PSC[:, dr]), reads=k_, writes=["PSC%d" % dr])
    P.op("dve", lambda e: e.tensor_copy(out=CAR[:], in_=CFIN[:]), reads=["CFIN"], writes=["CARRY"])
    for dr in range(2):
        for st in range(7):
            P.op("dve", lambda e, dr=dr, st=st: e.tensor_tensor(out=CAR[:, dr, :], in0=CAR[:, dr, :], in1=PSC[:, dr, st, 0, :], op=ALU.mult), reads=["CARRY", "PSC%d" % dr], writes=["CARRY"])
            P.op("dve", lambda e, dr=dr, st=st: e.tensor_tensor(out=CAR[:, dr, :], in0=CAR[:, dr, :], in1=PSC[:, dr, st, 1, :], op=ALU.add), reads=["CARRY", "PSC%d" % dr], writes=["CARRY"])
    B.lru_pass(prm, lp, m0, NCX, T, False, 2, HH=HH, EF=EF, CARRY=CAR, do_out=True)
    B.norm_mod(m0["GS_B"], m0["SH_B"], m0["keys"])
    B.swiglu_into_x(prm["ffn_w1"], prm["ffn_w3"], prm["ffn_w2"], m0["GT_B"], m0["keys"], tag="ffn")
    m1 = mods[li0 + 1]
    sp_es = ExitStack()
    sp = B.sgu_params(prm, sp_es)
    B.norm_mod(m1["GS_A"], m1["SH_A"], m1["keys"])
    if li0 == 0:
        B.sgu_pass(prm, sp, m1, 0, NCX, True)
    B.sgu_pass(prm, sp, m1, NCX, T, False)
    B.full_barrier()
    sp_es.close()
    B.norm_mod(m1["GS_B"], m1["SH_B"], m1["keys"])
    GATE = B.moe_gates(prm["moe_router"], m1)
    GBC = B.sb("GBC", [128, T], F32)
    DG = B.sb("DG", [128, 4, 128], F32)
    for ex in range(cfg.NEXP):
        B.gate_bcast(GATE, ex, GBC, DG)
        B.swiglu_into_x(prm["moe_w1"][ex], prm["moe_w3"][ex], prm["moe_w2"][ex], m1["GT_B"], m1["keys"], gbc=GBC, gbc_key="GBC", tag="moe")
    es = ExitStack()
    OT = [B.sb("OT%d" % i, [128, D], F32, es) for i in range(2)]
    if stage == 4:
        FG = B.sb("FG", [128, KC], F32, es)
        with nc.allow_non_contiguous_dma(reason="tiny"):
            P.dma("sp", lambda e: e.dma_start(out=FG[:], in_=final_g.rearrange("(k p) -> p k", p=128), allow_slow_non_contiguous=True), writes=["FG"])
        sq = [B.sb("fsq%d" % i, [128, T], BF16, es) for i in range(2)]
        tiles = col_tiles(0, T)
        for k in range(KC):
            s_ = sq[k % 2]
            ks = "fsq%d" % (k % 2)
            P.op("act", lambda e, k=k, s_=s_: e.activation(out=s_[:], in_=X[:, k, :], func=AF.Square), reads=["X"], writes=[ks])
            def mm(e, k=k, s_=s_):
                r = None
                for i, (a, b) in enumerate(tiles):
                    r = e.matmul(B.psA[i][:, 0:b - a], lhsT=B.ones16[:], rhs=s_[:, a:b], start=(k == 0), stop=(k == KC - 1))
                return r
            P.op("pe", mm, reads=[ks, "ones16"], writes=["psA0", "psA1", "psA2"])
        for i, (a, b) in enumerate(tiles):
            P.op("dve", lambda e, i=i, a=a, b=b: e.tensor_scalar(out=B.RSTD[:, a:b], in0=B.psA[i][:, 0:b - a], scalar1=1.0 / D, scalar2=EPS, op0=ALU.mult, op1=ALU.add), reads=["psA%d" % i], writes=["RSTD"])
        P.op("act", lambda e: e.activation(out=B.RSTD[:], in_=B.RSTD[:], func=AF.Ln), reads=["RSTD"], writes=["RSTD"])
        P.op("act", lambda e: e.activation(out=B.RSTD[:], in_=B.RSTD[:], func=AF.Exp, scale=-0.5), reads=["RSTD"], writes=["RSTD"])
        for k in range(KC):
            P.op("dve", lambda e, k=k: e.scalar_tensor_tensor(out=X[:, k, NCX:T], in0=X[:, k, NCX:T], scalar=FG[:, k:k + 1], in1=B.RSTD[:, NCX:T], op0=ALU.mult, op1=ALU.mult), reads=["X", "RSTD", "FG"], writes=["X"])
    xout = B.dout("xout", [NL, D])
    cout = B.dout("cout", [NCX, D])
    okeys = []
    for n in range(NTC):
        ot = OT[n % 2]
        ko = "OT%d" % (n % 2)
        for k0 in range(0, KC, 4):
            nk = min(4, KC - k0)
            pi = (k0 // 4) % 3
            ps = B.psA[pi]
            kp = "psA%d" % pi
            def tr(e, n=n, k0=k0, nk=nk, ps=ps):
                r = None
                for i in range(nk):
                    r = e.transpose(ps[:, i * 128:(i + 1) * 128], X[:, k0 + i, n * 128:(n + 1) * 128], B.ident[:])
                return r
            P.op("pe", tr, reads=["X", "ident"], writes=[kp])
            P.op("act", lambda e, ot=ot, k0=k0, nk=nk, ps=ps: e.copy(out=ot[:, k0 * 128:(k0 + nk) * 128], in_=ps[:, 0:nk * 128]), reads=[kp], writes=[ko])
        dst = cout[n * 128:(n + 1) * 128, :] if n * 128 < NCX else xout[n * 128 - NCX:(n + 1) * 128 - NCX, :]
        kk = "o_x%d" % n
        P.dma("sp", lambda e, ot=ot, dst=dst: e.dma_start(out=dst, in_=ot[:]), reads=[ko], writes=[kk])
        okeys.append(kk)
    P.wait_all("sp", okeys)
    es.close()
    return B.finish(), B


_PROG_CACHE = {}


def _get_prog(cfg, stage):
    key = (cfg.D, cfg.NL, cfg.NCX, cfg.F, cfg.HEADS, cfg.SGROUPS, cfg.NEXP, stage)
    if key not in _PROG_CACHE:
        _PROG_CACHE[key] = build_stage(cfg, stage)
    return _PROG_CACHE[key]


def run_module(cfg, inp):
    NCO, NL, D = cfg.NCORES, cfg.NL, cfg.D
    f = lambda a: np.ascontiguousarray(a, dtype=np.float32)
    x = f(inp["x"][0])
    ctx = f(inp["ctx"][0])
    ident = np.eye(128, dtype=np.float32)
    zero_row = np.zeros((1, D), np.float32)

    def halos(xfull, c):
        lo = xfull[c * NL - 2:c * NL] if c > 0 else np.concatenate([zero_row, zero_row], 0)
        hi = xfull[(c + 1) * NL:(c + 1) * NL + 1] if c < NCO - 1 else zero_row
        return f(np.concatenate([lo, hi], 0))

    def efl(c):
        e = np.ones((128, 2), np.float32)
        if c == 0:
            e[:, 0] = 0.0
        if c == NCO - 1:
            e[:, 1] = 0.0
        return e

    def lru_inputs(j):
        d = {}
        for k in LRU_KEYS:
            a = inp[k][j]
            if k in ("lru_b_a", "lru_b_x"):
                a = a.reshape(2, D)
            d[k] = f(a)
        return d

    def base(c, xfull, cfull, lis):
        d = {"xin": f(xfull[c * NL:(c + 1) * NL]), "xhalo": halos(xfull, c), "cin": cfull, "ef": efl(c), "ident": ident,
             "c": f(inp["c"][0]), "c_ctx": f(inp["c_ctx"])}
        for li in lis:
            d["w_mod%d" % li] = f(inp["w_mod"][li])
            d["b_mod%d" % li] = f(inp["b_mod"][li])
            d["norm_g%d" % li] = f(inp["norm_g"][li])
        return d

    def carries(ps_all, c):
        out = np.zeros((2, 7, 2, D), np.float32)
        out[:, :, 0, :] = 1.0
        seq_f = list(range(0, c))
        seq_r = list(range(NCO - 1, c, -1))
        for i, k in enumerate(seq_f):
            out[0, i] = ps_all[k][0]
        for i, k in enumerate(seq_r):
            out[1, i] = ps_all[k][1]
        return out

    cores = list(range(NCO))
    for half in range(2):
        li0, j = 2 * half, half
        lw = lru_inputs(j)
        nc1, _ = _get_prog(cfg, 1 if half == 0 else 3)
        maps = []
        for c in cores:
            d = base(c, x, ctx, [li0])
            d.update(lw)
            maps.append(d)
        res = run_bass_kernel_spmd(nc1, maps, core_ids=cores)
        ps_all = [res.results[c]["ps_out"] for c in cores]
        nc2, _ = _get_prog(cfg, 2 if half == 0 else 4)
        maps = []
        for c in cores:
            d = base(c, x, ctx, [li0, li0 + 1])
            d.update(lw)
            d["carry_ps"] = carries(ps_all, c)
            d["ffn_w1"], d["ffn_w3"], d["ffn_w2"] = f(inp["ffn_w1"][j]), f(inp["ffn_w3"][j]), f(inp["ffn_w2"][j])
            for k in ("sgu_w_in", "sgu_ln_g", "sgu_ln_b", "sgu_w_s", "sgu_b_s", "sgu_w_out", "moe_router", "moe_w1", "moe_w3", "moe_w2"):
                d[k] = f(inp[k][j])
            if half == 1:
                d["final_g"] = f(inp["final_g"])
            maps.append(d)
        res = run_bass_kernel_spmd(nc2, maps, core_ids=cores)
        x = np.concatenate([res.results[c]["xout"] for c in cores], 0)
        ctx = f(res.results[0]["cout"])
    return x[None].astype(np.float32)


QUADS = [[0, 1, 2, 3], [4, 5, 6, 7]]
XPAIRS = [[0, 4], [1, 5], [2, 6], [3, 7]]


def exchange(B, src_ap, n, tag, rkey, stack=None):
    nc, P = B.nc, B.P
    b0 = nc.dram_tensor("xb0_" + tag, [128, n], F32)
    b1 = nc.dram_tensor("xb1_" + tag, [4 * 128, n], F32)
    b2 = nc.dram_tensor("xb2_" + tag, [8 * 128, n], F32)
    G = B.sb("XG_" + tag, [128, 8, n], F32, stack)
    P.dma("sp", lambda e: e.dma_start(out=b0.ap(), in_=src_ap), reads=[rkey], writes=["xb0_" + tag])
    P.collective(lambda e: e.collective_compute("AllGather", ALU.bypass, replica_groups=QUADS, ins=[b0.ap().opt()], outs=[b1.ap().opt()]),
                 reads=["xb0_" + tag], writes=["xb1_" + tag])
    P.collective(lambda e: e.collective_compute("AllGather", ALU.bypass, replica_groups=XPAIRS, ins=[b1.ap().opt()], outs=[b2.ap().opt()]),
                 reads=["xb1_" + tag], writes=["xb2_" + tag])
    P.dma("sp", lambda e: e.dma_start(out=G[:], in_=b2.ap().rearrange("(r p) n -> p r n", p=128)), reads=["xb2_" + tag], writes=["XG_" + tag])
    return G


def build_fused(cfg):
    B = Builder(cfg, 0)
    nc, P = B.nc, B.P
    D, KC, T, NL, NCX, F = cfg.D, cfg.KC, cfg.T, cfg.NL, cfg.NCX, cfg.F
    HD = D // cfg.HEADS
    xin = B.din("xin", [NL, D])
    xhalo = B.din("xhalo", [3, D])
    cin = B.din("cin", [NCX, D])
    ef = B.din("ef", [128, 2])
    selm = B.din("selmask", [128, 16])
    carm = B.din("carmask", [128, 16])
    c_v = B.din("c", [D])
    cctx_v = B.din("c_ctx", [D])
    w_mod = {li: B.din("w_mod%d" % li, [D, (6 * D) // 8]) for li in range(4)}
    b_mod = {li: B.din("b_mod%d" % li, [(6 * D) // 8]) for li in range(4)}
    norm_g = {li: B.din("norm_g%d" % li, [2, D]) for li in range(4)}
    final_g = B.din("final_g", [D])
    shapes = {"lru_w_in": [D, 2 * D], "lru_conv_w": [4, D], "lru_conv_b": [D], "lru_w_a": [2, cfg.HEADS, HD, HD], "lru_b_a": [2, D],
              "lru_w_x": [2, cfg.HEADS, HD, HD], "lru_b_x": [2, D], "lru_lambda": [2, D], "lru_w_out": [D, D],
              "ffn_w1": [D, F], "ffn_w3": [D, F], "ffn_w2": [F, D], "sgu_w_in": [D, 2 * D], "sgu_ln_g": [D], "sgu_ln_b": [D],
              "sgu_w_s": [cfg.SGROUPS, 128, 128], "sgu_b_s": [cfg.SGROUPS, 128], "sgu_w_out": [D, D], "moe_router": [D, cfg.NEXP],
              "moe_w1": [cfg.NEXP, D, F], "moe_w3": [cfg.NEXP, D, F], "moe_w2": [cfg.NEXP, F, D]}
    prms = []
    for j in range(2):
        prms.append({k: B.din("%s_%d" % (k, j), shp) for k, shp in shapes.items()})
    cx_w1 = B.din("moe_ctx_w1", [D, F])
    cx_w3 = B.din("moe_ctx_w3", [D, F])
    cx_w2 = B.din("moe_ctx_w2", [F, D])
    ohexp_d = B.din("ohexp", [128, cfg.NEXP])
    B.setup_common()
    X = B.X
    XH = B.sb("XH", [128, KC, 3], F32)
    HH = B.sb("HH", [128, KC, 3], BF16)
    EF = B.sb("EF", [128, 2], F32)
    SEL = B.sb("SEL", [128, 16], F32)
    CMK = B.sb("CMK", [128, 16], F32)
    CFIN = B.sb("CFIN", [128, 2, KC], F32)
    PSO = B.sb("PSO", [128, 2, 2, KC], F32)
    CAR = B.sb("CAR", [128, 2, KC], F32)
    T1 = B.sb("T1c", [128, KC], F32)
    HSRC = B.sb("HSRC", [128, KC, 3], F32)
    P.dma("sp", lambda e: e.dma_start(out=EF[:], in_=ef), writes=["EF"])
    P.dma("sp", lambda e: e.dma_start(out=SEL[:], in_=selm), writes=["SEL"])
    P.dma("sp", lambda e: e.dma_start(out=CMK[:], in_=carm), writes=["CMK"])
    OHE = B.sb("OHE", [128, cfg.NEXP], F32)
    P.dma("sp", lambda e: e.dma_start(out=OHE[:], in_=ohexp_d), writes=["OHE"])
    es = ExitStack()
    TM = [B.sb("TM%d" % i, [128, D], F32, es) for i in range(2)]
    NTC = T // 128
    for n in range(NTC):
        tm = TM[n % 2]
        kt = "TM%d" % (n % 2)
        src = cin[n * 128:(n + 1) * 128, :] if n * 128 < NCX else xin[n * 128 - NCX:(n + 1) * 128 - NCX, :]
        P.dma("sp", lambda e, tm=tm, src=src: e.dma_start(out=tm[:], in_=src), writes=[kt])
        for k0 in range(0, KC, 4):
            nk = min(4, KC - k0)
            pi = (k0 // 4) % 3
            ps = B.psA[pi]
            kp = "psA%d" % pi
            def tr(e, tm=tm, k0=k0, nk=nk, ps=ps):
                r = None
                for i in range(nk):
                    r = e.transpose(ps[:, i * 128:(i + 1) * 128], tm[:, (k0 + i) * 128:(k0 + i + 1) * 128], B.ident[:])
                return r
            P.op("pe", tr, reads=[kt, "ident"], writes=[kp])
            P.op("act", lambda e, n=n, k0=k0, nk=nk, ps=ps: e.copy(out=X[:, k0:k0 + nk, n * 128:(n + 1) * 128], in_=ps[:, 0:nk * 128].rearrange("p (k t) -> p k t", k=nk)), reads=[kp], writes=["X"])
    k_ = B.fm_rows(lambda i: XH[:, :, i], xhalo, 3, "XH")
    P.op("dve", lambda e: e.tensor_copy(out=XH[:], in_=XH[:]), reads=k_, writes=["XH"])
    B.full_barrier()
    es.close()

    mods = B.compute_mods_sharded(w_mod, b_mod, c_v, cctx_v, norm_g)

    def run_half(half):
        li0, j = 2 * half, half
        prm = prms[j]
        m0 = mods[li0]
        if half == 1:
            P.op("dve", lambda e: e.tensor_copy(out=HSRC[:, :, 0:1], in_=X[:, :, NCX:NCX + 1]), reads=["X"], writes=["HSRC"])
            P.op("dve", lambda e: e.tensor_copy(out=HSRC[:, :, 1:3], in_=X[:, :, T - 2:T]), reads=["X"], writes=["HSRC"])
            x_es = ExitStack()
            GH = exchange(B, HSRC[:].rearrange("p k t -> p (k t)"), KC * 3, "halo", "HSRC", x_es)
            GHv = GH[:].rearrange("p r (k t) -> p r k t", t=3)
            P.op("dve", lambda e: e.memset(XH[:], 0.0), reads=["XH"], writes=["XH"])
            for r in range(8):
                P.op("dve", lambda e, r=r: e.scalar_tensor_tensor(out=XH[:, :, 0:2], in0=GHv[:, r, :, 1:3], scalar=SEL[:, r:r + 1], in1=XH[:, :, 0:2], op0=ALU.mult, op1=ALU.add), reads=["XG_halo", "SEL", "XH"], writes=["XH"])
                P.op("dve", lambda e, r=r: e.scalar_tensor_tensor(out=XH[:, :, 2:3], in0=GHv[:, r, :, 0:1], scalar=SEL[:, 8 + r:9 + r], in1=XH[:, :, 2:3], op0=ALU.mult, op1=ALU.add), reads=["XG_halo", "SEL", "XH"], writes=["XH"])
            B.full_barrier()
            x_es.close()
        B.norm_mod(m0["GS_A"], m0["SH_A"], m0["keys"])
        es = ExitStack()
        sqh = B.sb(B.key("sqh"), [128, KC, 3], F32, es)
        rh = B.sb(B.key("rh"), [128, 3], F32, es)
        th = B.sb(B.key("th"), [128, KC, 3], F32, es)
        P.op("dve", lambda e: e.tensor_tensor(out=sqh[:], in0=XH[:], in1=XH[:], op=ALU.mult), reads=["XH"], writes=["sqh"])
        def mmh(e):
            r = None
            for k in range(KC):
                r = e.matmul(B.psC[0][:, 0:3], lhsT=B.ones32[:], rhs=sqh[:, k, :], start=(k == 0), stop=(k == KC - 1))
            return r
        P.op("pe", mmh, reads=["sqh", "ones32"], writes=["psC0"])
        P.op("dve", lambda e: e.tensor_scalar(out=rh[:], in0=B.psC[0][:, 0:3], scalar1=1.0 / D, scalar2=EPS, op0=ALU.mult, op1=ALU.add), reads=["psC0"], writes=["rh"])
        P.op("act", lambda e: e.activation(out=rh[:], in_=rh[:], func=AF.Ln), reads=["rh"], writes=["rh"])
        P.op("act", lambda e: e.activation(out=rh[:], in_=rh[:], func=AF.Exp, scale=-0.5), reads=["rh"], writes=["rh"])
        for k in range(KC):
            P.op("dve", lambda e, k=k: e.scalar_tensor_tensor(out=th[:, k, :], in0=XH[:, k, :], scalar=m0["GS_A"][:, k, 0:1], in1=rh[:], op0=ALU.mult, op1=ALU.mult), reads=["XH", "rh"] + m0["keys"], writes=["th"])
            P.op("act", lambda e, k=k: e.activation(out=HH[:, k, :], in_=th[:, k, :], func=AF.Identity, bias=m0["SH_A"][:, k, 0:1], scale=1.0), reads=["th"] + m0["keys"], writes=["HH"])
        B.full_barrier()
        es.close()

        lp = B.lru_params(j, prm)
        spill = nc.dram_tensor("lru_spill%d" % j, [KC * 4, 128, NL], F32).ap()
        B.lru_pass(prm, lp, m0, 0, NCX, True, 2, CFIN=CFIN, do_out=(half == 0))
        B.lru_pass(prm, lp, m0, NCX, T, False, 1, HH=HH, EF=EF, PSOUT=PSO, SPILL=spill)
        x_es2 = ExitStack()
        GPS = exchange(B, PSO[:].rearrange("p a b k -> p (a b k)"), 4 * KC, "ps%d" % half, "PSOUT", x_es2)
        GPv = GPS[:].rearrange("p r (a b k) -> p r a b k", a=2, b=2)
        gk = "XG_ps%d" % half
        P.op("dve", lambda e: e.tensor_copy(out=CAR[:], in_=CFIN[:]), reads=["CFIN", "CARRY"], writes=["CARRY"])
        for dr in range(2):
            order = list(range(8)) if dr == 0 else list(range(7, -1, -1))
            for r in order:
                P.op("dve", lambda e, dr=dr, r=r: e.tensor_tensor(out=T1[:], in0=CAR[:, dr, :], in1=GPv[:, r, dr, 0, :], op=ALU.mult), reads=["CARRY", gk], writes=["T1c"])
                P.op("dve", lambda e, dr=dr, r=r: e.tensor_tensor(out=T1[:], in0=T1[:], in1=GPv[:, r, dr, 1, :], op=ALU.add), reads=["T1c", gk], writes=["T1c"])
                P.op("dve", lambda e, dr=dr, r=r: e.tensor_tensor(out=T1[:], in0=T1[:], in1=CAR[:, dr, :], op=ALU.subtract), reads=["T1c", "CARRY"], writes=["T1c"])
                P.op("dve", lambda e, dr=dr, r=r: e.scalar_tensor_tensor(out=CAR[:, dr, :], in0=T1[:], scalar=CMK[:, dr * 8 + r:dr * 8 + r + 1], in1=CAR[:, dr, :], op0=ALU.mult, op1=ALU.add), reads=["T1c", "CARRY", "CMK"], writes=["CARRY"])
        B.full_barrier()
        x_es2.close()
        B.lru_pass(prm, lp, m0, NCX, T, False, 2, HH=HH, EF=EF, CARRY=CAR, do_out=True, SPILL=spill)
        B.norm_mod(m0["GS_B"], m0["SH_B"], m0["keys"])
        fsegs = None if half == 0 else [(NCX, T, 0)]
        B.swiglu_into_x(prm["ffn_w1"], prm["ffn_w3"], prm["ffn_w2"], m0["GT_B"], m0["keys"], tag="ffn", segs=fsegs)
        m1 = mods[li0 + 1]
        sp_es = ExitStack()
        sp = B.sgu_params(prm, sp_es)
        B.norm_mod(m1["GS_A"], m1["SH_A"], m1["keys"])
        if half == 0:
            B.sgu_pass(prm, sp, m1, 0, NCX, True)
        B.sgu_pass(prm, sp, m1, NCX, T, False)
        B.full_barrier()
        sp_es.close()
        B.norm_mod(m1["GS_B"], m1["SH_B"], m1["keys"])
        g_es = ExitStack()
        GATE = B.sb(B.key("GATE"), [128, T // 128, cfg.NEXP], F32, g_es)
        GBC = B.sb(B.key("GBC"), [128, T], F32, g_es)
        DG = B.sb(B.key("DG"), [128, 4, 128], F32, g_es)
        B.moe_gates(prm["moe_router"], m1, GATE=GATE)
        lat_segs = [(NCX, T, 0)]
        for ex in range(cfg.NEXP):
            B.gate_bcast(GATE, ex, GBC, DG)
            B.swiglu_into_x(prm["moe_w1"][ex], prm["moe_w3"][ex], prm["moe_w2"][ex], m1["GT_B"], m1["keys"], gbc=GBC, gbc_key="GBC", tag="moe", segs=lat_segs)
        if half == 0:
            NTC_ = T // 128
            GATEC = B.sb(B.key("GATEC"), [128, NTC_, 1], F32, g_es)
            DX = B.sb(B.key("DX"), [128, KC, NCX], F32, g_es)
            P.op("dve", lambda e: e.memset(DX[:], 0.0), writes=["DX"])
            P.op("dve", lambda e: e.tensor_scalar(out=GATEC[:, :, 0], in0=GATE[:, :, 0], scalar1=OHE[:, 0:1], scalar2=None, op0=ALU.mult), reads=["GATE", "OHE"], writes=["GATEC"])
            for ex in range(1, cfg.NEXP):
                P.op("dve", lambda e, ex=ex: e.scalar_tensor_tensor(out=GATEC[:, :, 0], in0=GATE[:, :, ex], scalar=OHE[:, ex:ex + 1], in1=GATEC[:, :, 0], op0=ALU.mult, op1=ALU.add), reads=["GATE", "OHE", "GATEC"], writes=["GATEC"])
            B.P.wait_all("dve", ["GATEC"])
            B.full_barrier()
            B.gate_bcast_keyed(GATEC, 0, GBC, DG, "GATEC")
            B.full_barrier()
            B.swiglu_into_x(cx_w1, cx_w3, cx_w2, m1["GT_B"], m1["keys"], gbc=GBC, gbc_key="GBC", tag="moe", segs=[(0, NCX, 1)], xdst=DX, use_pool=False)
            PIECE = 4 if KC % 4 == 0 else KC
            for p0 in range(0, KC, PIECE):
                p_es = ExitStack()
                GD = exchange(B, DX[:, p0:p0 + PIECE, :].rearrange("p k t -> p (k t)"), PIECE * NCX, "dx%d" % p0, "DX", p_es)
                GDv = GD[:].rearrange("p r (k t) -> p r k t", k=PIECE)
                for r in range(8):
                    P.op("dve", lambda e, r=r, p0=p0, GDv=GDv: e.tensor_tensor(out=X[:, p0:p0 + PIECE, 0:NCX], in0=X[:, p0:p0 + PIECE, 0:NCX], in1=GDv[:, r, :, :], op=ALU.add), reads=["XG_dx%d" % p0, "X"], writes=["X"])
                B.full_barrier()
                p_es.close()
        B.full_barrier()
        g_es.close()

    for half_ in range(2):
        run_half(half_)

    es = ExitStack()
    OT = [B.sb("OT%d" % i, [128, D], F32, es) for i in range(2)]
    FG = B.sb("FG", [128, KC], F32, es)
    P.dma("sp", lambda e: e.dma_start(out=FG[:], in_=final_g.rearrange("(k p) -> p k", p=128), allow_slow_non_contiguous=True), writes=["FG"])
    sq = [B.sb("fsq%d" % i, [128, T], BF16, es) for i in range(2)]
    tiles = col_tiles(0, T)
    for k in range(KC):
        s_ = sq[k % 2]
        ks = "fsq%d" % (k % 2)
        P.op("act", lambda e, k=k, s_=s_: e.activation(out=s_[:], in_=X[:, k, :], func=AF.Square), reads=["X"], writes=[ks])
        def mm(e, k=k, s_=s_):
            r = None
            for i, (a, b) in enumerate(tiles):
                r = e.matmul(B.psA[i][:, 0:b - a], lhsT=B.ones16[:], rhs=s_[:, a:b], start=(k == 0), stop=(k == KC - 1))
            return r
        P.op("pe", mm, reads=[ks, "ones16"], writes=["psA0", "psA1", "psA2"])
    for i, (a, b) in enumerate(tiles):
        P.op("dve", lambda e, i=i, a=a, b=b: e.tensor_scalar(out=B.RSTD[:, a:b], in0=B.psA[i][:, 0:b - a], scalar1=1.0 / D, scalar2=EPS, op0=ALU.mult, op1=ALU.add), reads=["psA%d" % i], writes=["RSTD"])
    P.op("act", lambda e: e.activation(out=B.RSTD[:], in_=B.RSTD[:], func=AF.Ln), reads=["RSTD"], writes=["RSTD"])
    P.op("act", lambda e: e.activation(out=B.RSTD[:], in_=B.RSTD[:], func=AF.Exp, scale=-0.5), reads=["RSTD"], writes=["RSTD"])
    for k in range(KC):
        P.op("dve", lambda e, k=k: e.scalar_tensor_tensor(out=X[:, k, NCX:T], in0=X[:, k, NCX:T], scalar=FG[:, k:k + 1], in1=B.RSTD[:, NCX:T], op0=ALU.mult, op1=ALU.mult), reads=["X", "RSTD", "FG"], writes=["X"])
    xout = B.dout("xout", [NL, D])
    okeys = []
    for n in range(NCX // 128, NTC):
        ot = OT[n % 2]
        ko = "OT%d" % (n % 2)
        for k0 in range(0, KC, 4):
            nk = min(4, KC - k0)
            pi = (k0 // 4) % 3
            ps = B.psA[pi]
            kp = "psA%d" % pi
            def tr(e, n=n, k0=k0, nk=nk, ps=ps):
                r = None
                for i in range(nk):
                    r = e.transpose(ps[:, i * 128:(i + 1) * 128], X[:, k0 + i, n * 128:(n + 1) * 128], B.ident[:])
                return r
            P.op("pe", tr, reads=["X", "ident"], writes=[kp])
            P.op("act", lambda e, ot=ot, k0=k0, nk=nk, ps=ps: e.copy(out=ot[:, k0 * 128:(k0 + nk) * 128], in_=ps[:, 0:nk * 128]), reads=[kp], writes=[ko])
        dst = xout[n * 128 - NCX:(n + 1) * 128 - NCX, :]
        kk = "o_x%d" % n
        P.dma("sp", lambda e, ot=ot, dst=dst: e.dma_start(out=dst, in_=ot[:]), reads=[ko], writes=[kk])
        okeys.append(kk)
    P.wait_all("sp", okeys)
    es.close()
    return B.finish(), B


def run_fused(cfg, inp):
    NCO, NL, D = cfg.NCORES, cfg.NL, cfg.D
    assert NCO == 8
    f = lambda a: np.ascontiguousarray(a, dtype=np.float32)
    x = f(inp["x"][0])
    ctx = f(inp["ctx"][0])
    ident = np.eye(128, dtype=np.float32)
    zero_row = np.zeros((1, D), np.float32)
    key = ("fused", cfg.D, cfg.NL, cfg.NCX, cfg.F, cfg.HEADS, cfg.SGROUPS, cfg.NEXP)
    if key not in _PROG_CACHE:
        _PROG_CACHE[key] = build_fused(cfg)
    nc, _ = _PROG_CACHE[key]
    shared = {"cin": ctx, "ident": ident, "c": f(inp["c"][0]), "c_ctx": f(inp["c_ctx"]), "final_g": f(inp["final_g"])}
    NS = (6 * D) // 8
    for li in range(4):
        shared["norm_g%d" % li] = f(inp["norm_g"][li])
    for j in range(2):
        for k in LRU_KEYS + ["ffn_w1", "ffn_w3", "ffn_w2", "sgu_w_in", "sgu_ln_g", "sgu_ln_b", "sgu_w_s", "sgu_b_s", "sgu_w_out", "moe_router", "moe_w1", "moe_w3", "moe_w2"]:
            a = inp[k][j]
            if k in ("lru_b_a", "lru_b_x"):
                a = a.reshape(2, D)
            shared["%s_%d" % (k, j)] = f(a)
    maps = []
    for c in range(NCO):
        d = dict(shared)
        d["xin"] = f(x[c * NL:(c + 1) * NL])
        for li in range(4):
            d["w_mod%d" % li] = f(inp["w_mod"][li][:, c * NS:(c + 1) * NS])
            d["b_mod%d" % li] = f(inp["b_mod"][li][c * NS:(c + 1) * NS])
        lo = x[c * NL - 2:c * NL] if c > 0 else np.concatenate([zero_row, zero_row], 0)
        hi = x[(c + 1) * NL:(c + 1) * NL + 1] if c < NCO - 1 else zero_row
        d["xhalo"] = f(np.concatenate([lo, hi], 0))
        e = np.ones((128, 2), np.float32)
        if c == 0:
            e[:, 0] = 0.0
        if c == NCO - 1:
            e[:, 1] = 0.0
        d["ef"] = e
        sel = np.zeros((128, 16), np.float32)
        if c > 0:
            sel[:, c - 1] = 1.0
        if c < NCO - 1:
            sel[:, 8 + c + 1] = 1.0
        d["selmask"] = sel
        cm = np.zeros((128, 16), np.float32)
        cm[:, 0:c] = 1.0
        cm[:, 8 + c + 1:16] = 1.0
        d["carmask"] = cm
        d["moe_ctx_w1"] = f(inp["moe_w1"][0][c])
        d["moe_ctx_w3"] = f(inp["moe_w3"][0][c])
        d["moe_ctx_w2"] = f(inp["moe_w2"][0][c])
        oh = np.zeros((128, cfg.NEXP), np.float32)
        oh[:, c] = 1.0
        d["ohexp"] = oh
        maps.append(d)
    res = run_bass_kernel_spmd(nc, maps, core_ids=list(range(NCO)))
    out = np.concatenate([res.results[c]["xout"] for c in range(NCO)], 0)
    return out[None].astype(np.float32)


def kernel(**inputs):
    cfg = Cfg()
    return run_fused(cfg, inputs)
```

```python
import numpy as np
from contextlib import ExitStack
import concourse.bass as bass
import concourse.mybir as mybir
from concourse.bass_utils import run_bass_kernel_spmd

F32 = mybir.dt.float32
BF16 = mybir.dt.bfloat16
ALU = mybir.AluOpType
AF = mybir.ActivationFunctionType
AX = mybir.AxisListType
EPS = 1e-6
LRU_C = 8.0


class Cfg:
    def __init__(self, D=2048, NL=1024, NCX=256, F=5632, HEADS=8, SGROUPS=8, NEXP=8, NCORES=8):
        self.D, self.NL, self.NCX, self.F = D, NL, NCX, F
        self.HEADS, self.SGROUPS, self.NEXP, self.NCORES = HEADS, SGROUPS, NEXP, NCORES
        self.KC = D // 128
        self.FC = F // 128
        self.HC = (D // HEADS) // 128
        self.GCH = (D // SGROUPS) // 128
        self.T = NCX + NL


class Prog:
    ENGS = ("pe", "act", "dve", "pool", "sp")

    def __init__(self, nc, es, n_dma_sems=8):
        self.nc, self.es = nc, es
        self.streams = {e: [] for e in self.ENGS}
        self.psem = {e: es.enter_context(nc.semaphore("ps_" + e)) for e in ("pe", "act", "dve", "pool")}
        self.pcount = {e: 0 for e in self.psem}
        self.known = {e: {} for e in self.ENGS}
        self.semobj = {"ps_" + e: s for e, s in self.psem.items()}
        self.dsems, self.dcount, self.dnext = {}, {}, {}
        for q in ("sp", "pool"):
            names = ["ds_%s_%d" % (q, i) for i in range(n_dma_sems)]
            self.dsems[q] = names
            for n in names:
                self.semobj[n] = es.enter_context(nc.semaphore(n))
                self.dcount[n] = 0
            self.dnext[q] = 0
        self.last_write, self.readers = {}, {}
        self.ccsem = es.enter_context(nc.semaphore("cc_sem"))
        self.semobj["cc"] = self.ccsem
        self.ccount = 0

    def collective(self, fn, reads=(), writes=()):
        self._emit_waits("pool", self._deps(reads, writes))
        self.ccount += 1
        name = "cc%d" % self.ccount
        sem = self.es.enter_context(self.nc.semaphore(name))
        self.semobj[name] = sem
        tok = (name, 1)
        self.streams["pool"].append(("op", fn, sem, 1))
        self._commit(tok, reads, writes)
        return tok

    def _deps(self, reads, writes):
        toks = []
        for k in reads:
            toks += self.last_write.get(k, [])
        for k in writes:
            toks += self.last_write.get(k, [])
            toks += self.readers.get(k, [])
        return toks

    def _emit_waits(self, eng, toks):
        need = {}
        own = "ps_" + eng
        for (sn, v) in toks:
            if sn == own and eng == "pe":
                continue
            if self.known[eng].get(sn, 0) >= v:
                continue
            if need.get(sn, 0) < v:
                need[sn] = v
        for sn, v in need.items():
            self.known[eng][sn] = v
            self.streams[eng].append(("wait", self.semobj[sn], v))

    def _commit(self, tok, reads, writes):
        for k in reads:
            self.readers.setdefault(k, []).append(tok)
        for k in writes:
            self.last_write[k] = [tok]
            self.readers[k] = []

    def op(self, eng, fn, reads=(), writes=()):
        self._emit_waits(eng, self._deps(reads, writes))
        self.pcount[eng] += 1
        tok = ("ps_" + eng, self.pcount[eng])
        self.streams[eng].append(("op", fn, self.psem[eng], 1))
        self._commit(tok, reads, writes)
        return tok

    def dma(self, q, fn, reads=(), writes=()):
        toks = self._deps(reads, writes)
        names = self.dsems[q]
        sn = names[self.dnext[q] % len(names)]
        self.dnext[q] += 1
        if self.dcount[sn] > 0:
            toks = toks + [(sn, self.dcount[sn])]
        self._emit_waits(q, toks)
        self.dcount[sn] += 16
        tok = (sn, self.dcount[sn])
        self.streams[q].append(("op", fn, self.semobj[sn], 16))
        self._commit(tok, reads, writes)
        return tok

    def wait_all(self, eng, keys):
        toks = []
        for k in keys:
            toks += self.last_write.get(k, [])
        self._emit_waits(eng, toks)

    def replay(self, block):
        def run(stream):
            def body(e):
                for item in stream:
                    if item[0] == "wait":
                        e.wait_ge(item[1], item[2])
                    else:
                        item[1](e).then_inc(item[2], item[3])
            return body
        block.tensor(run(self.streams["pe"]))
        block.scalar(run(self.streams["act"]))
        block.vector(run(self.streams["dve"]))
        block.gpsimd(run(self.streams["pool"]))
        block.sync(run(self.streams["sp"]))


def col_tiles(c0, c1, maxw=512):
    out = []
    while c0 < c1:
        w = min(maxw, c1 - c0)
        out.append((c0, c0 + w))
        c0 += w
    return out


class Builder:
    def __init__(self, cfg, stage):
        self.cfg, self.stage = cfg, stage
        self.nc = bass.Bass("TRN2", target_bir_lowering=False)
        self.es = ExitStack()
        self.P = Prog(self.nc, self.es)
        self.uid = 0
        self.ins, self.outs = {}, {}
        self.out_keys = []

    def din(self, name, shape):
        t = self.nc.dram_tensor(name, list(shape), F32, kind="ExternalInput").ap()
        self.ins[name] = t
        return t

    def dout(self, name, shape):
        t = self.nc.dram_tensor(name, list(shape), F32, kind="ExternalOutput").ap()
        self.outs[name] = t
        return t

    def sb(self, name, shape, dt, stack=None):
        return (stack or self.es).enter_context(self.nc.sbuf_tensor(name, list(shape), dt))

    def pst(self, name, stack=None):
        return (stack or self.es).enter_context(self.nc.psum_tensor(name, [128, 512], F32))

    def key(self, base):
        self.uid += 1
        return "%s#%d" % (base, self.uid)

    def setup_common(self):
        cfg, P, nc = self.cfg, self.P, self.nc
        KC = cfg.KC
        self.ident_d = self.din("ident", [128, 128])
        self.ident = self.sb("ident_s", [128, 128], F32)
        P.dma("sp", lambda e: e.dma_start(out=self.ident[:], in_=self.ident_d), writes=["ident"])
        self.ones32 = self.sb("ones32", [128, 128], F32)
        self.ones16 = self.sb("ones16", [128, 128], BF16)
        P.op("dve", lambda e: e.memset(self.ones32[:], 1.0), writes=["ones32"])
        P.op("dve", lambda e: e.memset(self.ones16[:], 1.0), writes=["ones16"])
        self.X = self.sb("X", [128, KC, cfg.T], F32)
        self.H = self.sb("H", [128, KC, cfg.T], BF16)
        self.RSTD = self.sb("RSTD", [128, cfg.T], F32)
        self.psA = [self.pst("psA%d" % i) for i in range(3)]
        self.psB = [self.pst("psB%d" % i) for i in range(3)]
        self.psC = [self.pst("psC%d" % i) for i in range(2)]
        self.segs = [(0, cfg.NCX, 1), (cfg.NCX, cfg.T, 0)]

    def load_fm_vec(self, dst_ap, src_1d, q="sp", writes=()):
        with self.nc.allow_non_contiguous_dma(reason="tiny per-feature vectors"):
            pass
        def f(e):
            return e.dma_start(out=dst_ap, in_=src_1d.rearrange("(k p) -> p k", p=128), allow_slow_non_contiguous=True)
        return self.P.dma(q, f, writes=list(writes))

    def compute_mods(self, li, w_mod, b_mod, c_in, cctx_in, norm_g):
        cfg, P = self.cfg, self.P
        KC = cfg.KC
        MOD = self.sb("MOD%d" % li, [128, 6 * KC, 2], F32)
        d = {}
        for nm in ("GS_A", "GS_B"):
            d[nm] = self.sb("%s%d" % (nm, li), [128, KC, 2], F32)
        es = ExitStack()
        S = self.sb("modS%d" % li, [128, KC, 2], F32, es)
        bm = self.sb("modB%d" % li, [128, 6 * KC], F32, es)
        kS = self.key("S")
        with self.nc.allow_non_contiguous_dma(reason="tiny"):
            P.dma("sp", lambda e: e.dma_start(out=S[:, :, 0], in_=c_in.rearrange("(k p) -> p k", p=128), allow_slow_non_contiguous=True), writes=[kS + "a"])
            P.dma("sp", lambda e: e.dma_start(out=S[:, :, 1], in_=cctx_in.rearrange("(k p) -> p k", p=128), allow_slow_non_contiguous=True), writes=[kS + "b"])
            P.dma("sp", lambda e: e.dma_start(out=bm[:], in_=b_mod.rearrange("(k p) -> p k", p=128), allow_slow_non_contiguous=True), writes=[kS + "bm"])
        P.op("act", lambda e: e.activation(out=S[:], in_=S[:], func=AF.Silu), reads=[kS + "a", kS + "b"], writes=[kS])
        CB = 512 if 6 * cfg.D >= 512 else 6 * cfg.D
        nblk = (6 * cfg.D) // CB
        wbuf = [self.sb("modW%d_%d" % (li, i), [128, KC, CB], F32, es) for i in range(2)]
        for b in range(nblk):
            wb = wbuf[b % 2]
            kw = "modw%d" % (b % 2)
            P.dma("sp", lambda e, wb=wb, b=b: e.dma_start(out=wb[:], in_=w_mod[:, b * CB:(b + 1) * CB].rearrange("(k p) n -> p k n", p=128)), writes=[kw])
            for oc in range(CB // 128):
                o = b * (CB // 128) + oc
                ps = self.psC[o % 2]
                kp = "psC%d" % (o % 2)
                def mm(e, wb=wb, oc=oc, ps=ps):
                    r = None
                    for k in range(KC):
                        r = e.matmul(ps[:, 0:2], lhsT=wb[:, k, oc * 128:(oc + 1) * 128], rhs=S[:, k, :], start=(k == 0), stop=(k == KC - 1))
                    return r
                P.op("pe", mm, reads=[kw, kS], writes=[kp])
                P.op("dve", lambda e, o=o, ps=ps: e.tensor_scalar(out=MOD[:, o, :], in0=ps[:, 0:2], scalar1=bm[:, o:o + 1], scalar2=None, op0=ALU.add),
                     reads=[kp, kS + "bm"], writes=["MOD%d" % li])
        G = self.sb("modG%d" % li, [128, 2, KC], F32, es)
        gk = self.fm_rows(lambda i: G[:, i, :], norm_g, 2, kS + "g")
        self.P.op("dve", lambda e: e.tensor_copy(out=G[:], in_=G[:]), reads=gk, writes=[kS + "g"])
        for s in range(2):
            P.op("dve", lambda e, s=s: e.scalar_tensor_tensor(out=d["GS_A"][:, :, s], in0=MOD[:, KC:2 * KC, s], scalar=1.0, in1=G[:, 0, :], op0=ALU.add, op1=ALU.mult),
                 reads=["MOD%d" % li, kS + "g"], writes=["GS_A%d" % li])
            P.op("dve", lambda e, s=s: e.scalar_tensor_tensor(out=d["GS_B"][:, :, s], in0=MOD[:, 4 * KC:5 * KC, s], scalar=1.0, in1=G[:, 1, :], op0=ALU.add, op1=ALU.mult),
                 reads=["MOD%d" % li, kS + "g"], writes=["GS_B%d" % li])
        d["SH_A"] = MOD[:, 0:KC, :]
        d["GT_A"] = MOD[:, 2 * KC:3 * KC, :]
        d["SH_B"] = MOD[:, 3 * KC:4 * KC, :]
        d["GT_B"] = MOD[:, 5 * KC:6 * KC, :]
        d["keys"] = ["MOD%d" % li, "GS_A%d" % li, "GS_B%d" % li]
        P.wait_all("dve", ["GS_A%d" % li, "GS_B%d" % li, "MOD%d" % li])
        self._barrier(["GS_A%d" % li, "GS_B%d" % li, "MOD%d" % li, "modw0", "modw1", kS])
        es.close()
        return d

    def fm_rows(self, dst_fn, src2d, n, wkey, q="sp"):
        for i in range(n):
            self.P.dma(q, lambda e, i=i: e.dma_start(out=dst_fn(i), in_=src2d[i].rearrange("(k p) -> p k", p=128), allow_slow_non_contiguous=True), writes=["%s_r%d" % (wkey, i)])
        return ["%s_r%d" % (wkey, i) for i in range(n)]

    def compute_mods_sharded(self, w_mod, b_mod, c_in, cctx_in, norm_g, nlayers=4):
        cfg, P = self.cfg, self.P
        KC = cfg.KC
        NS = (6 * cfg.D) // 8
        NCH = NS // 128
        mods = []
        for li in range(nlayers):
            MOD = self.sb("MOD%d" % li, [128, 6 * KC, 2], F32)
            d = {"MOD": MOD}
            for nm in ("GS_A", "GS_B"):
                d[nm] = self.sb("%s%d" % (nm, li), [128, KC, 2], F32)
            mods.append(d)
        PART = self.sb("MODPART", [128, nlayers, NCH, 2], F32)
        es = ExitStack()
        S = self.sb("modS", [128, KC, 2], F32, es)
        bm = self.sb("modB", [128, nlayers, NCH], F32, es)
        G = self.sb("modG", [128, nlayers, 2, KC], F32, es)
        CB = 512 if NS % 512 == 0 else NS
        wbuf = [self.sb("modW%d" % i, [128, KC, CB], F32, es) for i in range(2)]
        wcnt = 0
        P.dma("sp", lambda e: e.dma_start(out=S[:, :, 0], in_=c_in.rearrange("(k p) -> p k", p=128), allow_slow_non_contiguous=True), writes=["mSa"])
        P.dma("sp", lambda e: e.dma_start(out=S[:, :, 1], in_=cctx_in.rearrange("(k p) -> p k", p=128), allow_slow_non_contiguous=True), writes=["mSb"])
        P.op("act", lambda e: e.activation(out=S[:], in_=S[:], func=AF.Silu), reads=["mSa", "mSb"], writes=["mS"])
        for li in range(nlayers):
            P.dma("sp", lambda e, li=li: e.dma_start(out=bm[:, li, :], in_=b_mod[li].rearrange("(k p) -> p k", p=128), allow_slow_non_contiguous=True), writes=["mbm%d" % li])
            gk = self.fm_rows(lambda i, li=li: G[:, li, i, :], norm_g[li], 2, "mg%d" % li)
            P.op("dve", lambda e, li=li: e.tensor_copy(out=G[:, li], in_=G[:, li]), reads=gk, writes=["mg%d" % li])
            for blk in range(NS // CB):
                wb = wbuf[wcnt % 2]
                kw = "modw%d" % (wcnt % 2)
                wcnt += 1
                P.dma("sp", lambda e, wb=wb, li=li, blk=blk: e.dma_start(out=wb[:], in_=w_mod[li][:, blk * CB:(blk + 1) * CB].rearrange("(k p) n -> p k n", p=128)), writes=[kw])
                for oi in range(CB // 128):
                    oc = blk * (CB // 128) + oi
                    ps = self.psC[oc % 2]
                    kp = "psC%d" % (oc % 2)
                    def mm(e, wb=wb, oi=oi, ps=ps):
                        r = None
                        for k in range(KC):
                            r = e.matmul(ps[:, 0:2], lhsT=wb[:, k, oi * 128:(oi + 1) * 128], rhs=S[:, k, :], start=(k == 0), stop=(k == KC - 1))
                        return r
                    P.op("pe", mm, reads=[kw, "mS"], writes=[kp])
                    P.op("dve", lambda e, li=li, oc=oc, ps=ps: e.tensor_scalar(out=PART[:, li, oc, :], in0=ps[:, 0:2], scalar1=bm[:, li, oc:oc + 1], scalar2=None, op0=ALU.add),
                         reads=[kp, "mbm%d" % li], writes=["MODPART"])
        n = nlayers * NCH * 2
        GX = exchange(self, PART[:].rearrange("p l c s -> p (l c s)"), n, "mods", "MODPART", es)
        GXv = GX[:].rearrange("p r (l c s) -> p r l c s", l=nlayers, c=NCH)
        for li in range(nlayers):
            d = mods[li]
            MOD = d["MOD"]
            P.op("dve", lambda e, li=li, MOD=MOD: e.tensor_copy(out=MOD[:].rearrange("p (r c) s -> p r c s", r=8), in_=GXv[:, :, li, :, :]), reads=["XG_mods"], writes=["MOD%d" % li])
            for s in range(2):
                P.op("dve", lambda e, s=s, d=d, MOD=MOD, li=li: e.scalar_tensor_tensor(out=d["GS_A"][:, :, s], in0=MOD[:, KC:2 * KC, s], scalar=1.0, in1=G[:, li, 0, :], op0=ALU.add, op1=ALU.mult),
                     reads=["MOD%d" % li, "mg%d" % li], writes=["GS_A%d" % li])
                P.op("dve", lambda e, s=s, d=d, MOD=MOD, li=li: e.scalar_tensor_tensor(out=d["GS_B"][:, :, s], in0=MOD[:, 4 * KC:5 * KC, s], scalar=1.0, in1=G[:, li, 1, :], op0=ALU.add, op1=ALU.mult),
                     reads=["MOD%d" % li, "mg%d" % li], writes=["GS_B%d" % li])
            d["SH_A"] = MOD[:, 0:KC, :]
            d["GT_A"] = MOD[:, 2 * KC:3 * KC, :]
            d["SH_B"] = MOD[:, 3 * KC:4 * KC, :]
            d["GT_B"] = MOD[:, 5 * KC:6 * KC, :]
            d["keys"] = ["MOD%d" % li, "GS_A%d" % li, "GS_B%d" % li]
        self.full_barrier()
        es.close()
        return mods

    def _barrier(self, keys):
        for eng in Prog.ENGS:
            toks = []
            for k in keys:
                toks += self.P.last_write.get(k, []) + self.P.readers.get(k, [])
            self.P._emit_waits(eng, toks)

    def full_barrier(self):
        toks = []
        for e, c in self.P.pcount.items():
            if c:
                toks.append(("ps_" + e, c))
        for sn, c in self.P.dcount.items():
            if c:
                toks.append((sn, c))
        for i in range(self.P.ccount):
            toks.append(("cc%d" % (i + 1), 1))
        for eng in Prog.ENGS:
            self.P._emit_waits(eng, toks)

    def norm_mod(self, GS, SH, modkeys, Hdst=None, extra_cols=None):
        cfg, P = self.cfg, self.P
        KC, T = cfg.KC, cfg.T
        H = self.H if Hdst is None else Hdst
        es = ExitStack()
        sq = [self.sb(self.key("sq"), [128, T], BF16, es) for _ in range(2)]
        tmp = [self.sb(self.key("nt"), [128, T], F32, es) for _ in range(2)]
        tiles = col_tiles(0, T)
        for k in range(KC):
            s = sq[k % 2]
            ks = "sq%d" % (k % 2)
            P.op("act", lambda e, k=k, s=s: e.activation(out=s[:], in_=self.X[:, k, :], func=AF.Square), reads=["X"], writes=[ks])
            def mm(e, k=k, s=s):
                r = None
                for i, (a, b) in enumerate(tiles):
                    r = e.matmul(self.psA[i][:, 0:b - a], lhsT=self.ones16[:], rhs=s[:, a:b], start=(k == 0), stop=(k == KC - 1))
                return r
            P.op("pe", mm, reads=[ks, "ones16"], writes=["psA0", "psA1", "psA2"])
        for i, (a, b) in enumerate(tiles):
            P.op("dve", lambda e, i=i, a=a, b=b: e.tensor_scalar(out=self.RSTD[:, a:b], in0=self.psA[i][:, 0:b - a], scalar1=1.0 / cfg.D, scalar2=EPS, op0=ALU.mult, op1=ALU.add),
                 reads=["psA%d" % i], writes=["RSTD"])
        P.op("act", lambda e: e.activation(out=self.RSTD[:], in_=self.RSTD[:], func=AF.Ln), reads=["RSTD"], writes=["RSTD"])
        P.op("act", lambda e: e.activation(out=self.RSTD[:], in_=self.RSTD[:], func=AF.Exp, scale=-0.5), reads=["RSTD"], writes=["RSTD"])
        for k in range(KC):
            t = tmp[k % 2]
            kt = "nt%d" % (k % 2)
            for (c0, c1, s) in self.segs:
                P.op("dve", lambda e, k=k, t=t, c0=c0, c1=c1, s=s: e.scalar_tensor_tensor(out=t[:, c0:c1], in0=self.X[:, k, c0:c1], scalar=GS[:, k, s:s + 1], in1=self.RSTD[:, c0:c1], op0=ALU.mult, op1=ALU.mult),
                     reads=["X", "RSTD"] + modkeys, writes=[kt + "_%d" % s])
                P.op("act", lambda e, k=k, t=t, c0=c0, c1=c1, s=s: e.activation(out=H[:, k, c0:c1], in_=t[:, c0:c1], func=AF.Identity, bias=SH[:, k, s:s + 1], scale=1.0),
                     reads=[kt + "_%d" % s] + modkeys, writes=["H"])
        self._barrier(["sq0", "sq1", "nt0_0", "nt0_1", "nt1_0", "nt1_1", "H", "RSTD"])
        es.close()

    def swiglu_into_x(self, w1, w3, w2, GT, modkeys, gbc=None, gbc_key=None, tag="ffn", segs=None, xdst=None, use_pool=True):
        cfg, P = self.cfg, self.P
        KC, T, FC = cfg.KC, cfg.T, cfg.FC
        G = 2
        XT = self.X if xdst is None else xdst
        tiles = []
        for (c0, c1, s) in (segs or self.segs):
            tiles += [(a, b, s) for (a, b) in col_tiles(c0, c1)]
        es = ExitStack()
        W1 = [self.sb(self.key("W1"), [128, KC, G * 128], BF16, es) for _ in range(2)]
        W3 = [self.sb(self.key("W3"), [128, KC, G * 128], BF16, es) for _ in range(2)]
        W2 = [self.sb(self.key("W2"), [128, G, cfg.D], BF16, es) for _ in range(2)]
        cmax = max(c1_ for (_, c1_, _) in (segs or self.segs))
        ACT = [self.sb(self.key("ACTB"), [128, G, cmax], BF16, es) for _ in range(2)]
        s1w = min(512, max(c1_ - c0_ for (c0_, c1_, _) in (segs or self.segs)))
        S1 = [self.sb(self.key("S1"), [128, s1w], F32, es) for _ in range(2 if use_pool else 1)]
        TMPB = [self.sb(self.key("TMPB"), [128, 512], F32, es) for _ in range(2)] if use_pool else None
        ng = FC // G
        cnt = 0
        ycnt = 0

        def issue_w(g):
            b = g % 2
            kw = "%s_w%d" % (tag, b)
            P.dma("pool", lambda e, g=g, b=b: e.dma_start(out=W1[b][:], in_=w1[:, g * G * 128:(g + 1) * G * 128].rearrange("(k p) n -> p k n", p=128)), writes=[kw + "1"])
            P.dma("pool", lambda e, g=g, b=b: e.dma_start(out=W3[b][:], in_=w3[:, g * G * 128:(g + 1) * G * 128].rearrange("(k p) n -> p k n", p=128)), writes=[kw + "3"])
            P.dma("pool", lambda e, g=g, b=b: e.dma_start(out=W2[b][:], in_=w2[g * G * 128:(g + 1) * G * 128, :].rearrange("(f p) n -> p f n", p=128)), writes=[kw + "2"])

        issue_w(0)
        for g in range(ng):
            b = g % 2
            kw = "%s_w%d" % (tag, b)
            if g + 1 < ng:
                issue_w(g + 1)
            ka = "%s_act%d" % (tag, b)
            for fi in range(G):
                for (a, bb, s) in tiles:
                    w = bb - a
                    pi = cnt % 2
                    cnt += 1
                    p1, p3 = self.psA[pi], self.psB[pi]
                    k1, k3 = "psA%d" % pi, "psB%d" % pi
                    def mm1(e, b=b, fi=fi, a=a, bb=bb, w=w, p1=p1):
                        r = None
                        for k in range(KC):
                            r = e.matmul(p1[:, 0:w], lhsT=W1[b][:, k, fi * 128:(fi + 1) * 128], rhs=self.H[:, k, a:bb], start=(k == 0), stop=(k == KC - 1))
                        return r
                    def mm3(e, b=b, fi=fi, a=a, bb=bb, w=w, p3=p3):
                        r = None
                        for k in range(KC):
                            r = e.matmul(p3[:, 0:w], lhsT=W3[b][:, k, fi * 128:(fi + 1) * 128], rhs=self.H[:, k, a:bb], start=(k == 0), stop=(k == KC - 1))
                        return r
                    P.op("pe", mm1, reads=[kw + "1", "H"], writes=[k1])
                    P.op("pe", mm3, reads=[kw + "3", "H"], writes=[k3])
                    s1 = S1[pi % len(S1)]
                    ks1 = "%s_s1_%d" % (tag, pi % len(S1))
                    P.op("act", lambda e, s1=s1, p1=p1, w=w: e.activation(out=s1[:, 0:w], in_=p1[:, 0:w], func=AF.Silu), reads=[k1], writes=[ks1])
                    if gbc is None:
                        P.op("dve", lambda e, b=b, fi=fi, a=a, bb=bb, w=w, s1=s1, p3=p3: e.tensor_tensor(out=ACT[b][:, fi, a:bb], in0=s1[:, 0:w], in1=p3[:, 0:w], op=ALU.mult),
                             reads=[ks1, k3], writes=[ka])
                    else:
                        s2 = s1
                        ks2 = ks1
                        P.op("dve", lambda e, w=w, s1=s1, s2=s2, p3=p3: e.tensor_tensor(out=s2[:, 0:w], in0=s1[:, 0:w], in1=p3[:, 0:w], op=ALU.mult),
                             reads=[ks1, k3], writes=[ks2])
                        P.op("dve", lambda e, b=b, fi=fi, a=a, bb=bb, w=w, s2=s2: e.tensor_tensor(out=ACT[b][:, fi, a:bb], in0=s2[:, 0:w], in1=gbc[:, a:bb], op=ALU.mult),
                             reads=[ks2, gbc_key], writes=[ka])
            for d in range(KC):
                for (a, bb, s) in tiles:
                    w = bb - a
                    ybanks = [(self.psC[0], "psC0"), (self.psC[1], "psC1"), (self.psA[2], "psA2"), (self.psB[2], "psB2")]
                    py, ky = ybanks[ycnt % 4]
                    ycnt += 1
                    def mmy(e, b=b, d=d, a=a, bb=bb, w=w, py=py):
                        r = None
                        for fi in range(G):
                            r = e.matmul(py[:, 0:w], lhsT=W2[b][:, fi, d * 128:(d + 1) * 128], rhs=ACT[b][:, fi, a:bb], start=(fi == 0), stop=(fi == G - 1))
                        return r
                    P.op("pe", mmy, reads=[kw + "2", ka], writes=[ky])
                    xk = "X_%d_%d" % (d, a)
                    if use_pool and ycnt % 3 == 0:
                        tb = TMPB[(ycnt // 3) % 2]
                        kt = "%s_tmpb%d" % (tag, (ycnt // 3) % 2)
                        P.op("act", lambda e, d=d, w=w, s=s, py=py, tb=tb: e.mul(out=tb[:, 0:w], in_=py[:, 0:w], mul=GT[:, d, s:s + 1]), reads=[ky] + modkeys, writes=[kt])
                        P.op("pool", lambda e, d=d, a=a, bb=bb, w=w, tb=tb: e.tensor_tensor(out=XT[:, d, a:bb], in0=XT[:, d, a:bb], in1=tb[:, 0:w], op=ALU.add), reads=[kt, xk], writes=[xk])
                    else:
                        P.op("dve", lambda e, d=d, a=a, bb=bb, w=w, s=s, py=py: e.scalar_tensor_tensor(out=XT[:, d, a:bb], in0=py[:, 0:w], scalar=GT[:, d, s:s + 1], in1=XT[:, d, a:bb], op0=ALU.mult, op1=ALU.add),
                             reads=[ky, xk] + modkeys, writes=[xk])
        self.full_barrier()
        es.close()

    def outproj_into_x(self, Wt, nk, rhs_fn, rhs_key, wkey, GT, modkeys, c0, c1, s):
        cfg, P = self.cfg, self.P
        cnt = 0
        for d in range(cfg.KC):
            for (a, bb) in col_tiles(c0, c1):
                w = bb - a
                pi = cnt % 2
                cnt += 1
                py = self.psC[pi]
                ky = "psC%d" % pi
                def mmy(e, d=d, a=a, bb=bb, w=w, py=py):
                    r = None
                    for ic in range(nk):
                        r = e.matmul(py[:, 0:w], lhsT=Wt[:, ic, d * 128:(d + 1) * 128], rhs=rhs_fn(ic, a - c0, bb - c0), start=(ic == 0), stop=(ic == nk - 1))
                    return r
                P.op("pe", mmy, reads=[wkey, rhs_key], writes=[ky])
                P.op("dve", lambda e, d=d, a=a, bb=bb, w=w, py=py: e.scalar_tensor_tensor(out=self.X[:, d, a:bb], in0=py[:, 0:w], scalar=GT[:, d, s:s + 1], in1=self.X[:, d, a:bb], op0=ALU.mult, op1=ALU.add),
                     reads=[ky, "X_%d_%d" % (d, a)] + modkeys, writes=["X_%d_%d" % (d, a)])

    def lru_params(self, j, prm):
        cfg, P = self.cfg, self.P
        KC = cfg.KC
        d = {}
        d["CW"] = self.sb("lruCW%d" % j, [128, 4, KC], F32)
        d["CB"] = self.sb("lruCB%d" % j, [128, KC], F32)
        d["BA"] = self.sb("lruBA%d" % j, [128, 2, KC], F32)
        d["BX"] = self.sb("lruBX%d" % j, [128, 2, KC], F32)
        d["C1"] = self.sb("lruC1%d" % j, [128, 2, KC], F32)
        d["C2"] = self.sb("lruC2%d" % j, [128, 2, KC], F32)
        with self.nc.allow_non_contiguous_dma(reason="tiny"):
            k_ = self.fm_rows(lambda i: d["CW"][:, i, :], prm["lru_conv_w"], 4, "lruprm%d_cw" % j)
            P.op("dve", lambda e: e.tensor_copy(out=d["CW"][:], in_=d["CW"][:]), reads=k_, writes=["lruprm%d_" % j + "cw"])
            P.dma("sp", lambda e: e.dma_start(out=d["CB"][:], in_=prm["lru_conv_b"].rearrange("(k p) -> p k", p=128), allow_slow_non_contiguous=True), writes=["lruprm%d_" % j + "cb"])
            k_ = self.fm_rows(lambda i: d["BA"][:, i, :], prm["lru_b_a"], 2, "lruprm%d_" % j + "ba")
            P.op("dve", lambda e: e.tensor_copy(out=d["BA"][:], in_=d["BA"][:]), reads=k_, writes=["lruprm%d_" % j + "ba"])
            k_ = self.fm_rows(lambda i: d["BX"][:, i, :], prm["lru_b_x"], 2, "lruprm%d_" % j + "bx")
            P.op("dve", lambda e: e.tensor_copy(out=d["BX"][:], in_=d["BX"][:]), reads=k_, writes=["lruprm%d_" % j + "bx"])
            k_ = self.fm_rows(lambda i: d["C1"][:, i, :], prm["lru_lambda"], 2, "lruprm%d_" % j + "c1")
            P.op("dve", lambda e: e.tensor_copy(out=d["C1"][:], in_=d["C1"][:]), reads=k_, writes=["lruprm%d_" % j + "c1"])
        P.op("act", lambda e: e.activation(out=d["C1"][:], in_=d["C1"][:], func=AF.Exp, scale=-1.0), reads=["lruprm%d_" % j + "c1"], writes=["lruprm%d_" % j + "c1"])
        P.op("act", lambda e: e.activation(out=d["C1"][:], in_=d["C1"][:], func=AF.Ln, bias=1.0, scale=1.0), reads=["lruprm%d_" % j + "c1"], writes=["lruprm%d_" % j + "c1"])
        P.op("dve", lambda e: e.tensor_scalar(out=d["C2"][:], in0=d["C1"][:], scalar1=-2.0 * LRU_C, scalar2=None, op0=ALU.mult), reads=["lruprm%d_" % j + "c1"], writes=["lruprm%d_" % j + "c2"])
        P.op("dve", lambda e: e.tensor_scalar(out=d["C1"][:], in0=d["C1"][:], scalar1=-LRU_C, scalar2=None, op0=ALU.mult), reads=["lruprm%d_" % j + "c1", "lruprm%d_" % j + "c2"], writes=["lruprm%d_" % j + "c1"])
        d["keys"] = ["lruprm%d_" % j + "cw", "lruprm%d_" % j + "cb", "lruprm%d_" % j + "ba", "lruprm%d_" % j + "bx", "lruprm%d_" % j + "c1", "lruprm%d_" % j + "c2"]
        return d

    def lru_pass(self, prm, lp, mods, c0, c1, is_ctx, phase, HH=None, EF=None, CARRY=None, CFIN=None, PSOUT=None, do_out=True, SPILL=None):
        cfg, P = self.cfg, self.P
        KC, HC = cfg.KC, cfg.HC
        W = c1 - c0
        s = 1 if is_ctx else 0
        es = ExitStack()
        WX = self.sb(self.key("WX"), [128, KC, HC * 128], BF16, es)
        WGt = self.sb(self.key("WG"), [128, 2, 2, HC, HC * 128], BF16, es)
        XB = self.sb(self.key("XB"), [128, HC, W + 3], F32, es)
        XC = self.sb(self.key("XC"), [128, HC, W], F32, es)
        XCb = self.sb(self.key("XCb"), [128, HC, W], BF16, es)
        spill_mode = (phase == 2 and not is_ctx and SPILL is not None)
        nbuf = 1 if spill_mode else 2
        Rl = [None if spill_mode else self.sb(self.key("R"), [128, W], F32, es) for _ in range(nbuf)]
        Al = [self.sb(self.key("A"), [128, W], F32, es) for _ in range(nbuf)]
        A2l = [self.sb(self.key("A2"), [128, W], F32, es) for _ in range(nbuf)]
        HS = self.sb(self.key("HS"), [128, 2, W], F32, es)
        SRl = [self.sb(self.key("SR"), [128, 1], F32, es) for _ in range(2)]
        need_out = do_out and phase == 2
        use_spill_load = (phase == 2 and not is_ctx and SPILL is not None)
        if need_out:
            WO = self.sb(self.key("WO"), [128, HC, cfg.D], BF16, es)
            GG = self.sb(self.key("GG"), [128, HC, W], BF16, es)
            YG = self.sb(self.key("YG"), [128, HC, W], BF16, es)
        tiles = col_tiles(0, W)
        w_in, w_a, w_x, w_out = prm["lru_w_in"], prm["lru_w_a"], prm["lru_w_x"], prm["lru_w_out"]
        lk = lp["keys"]
        for hd in range(cfg.HEADS):
            hcol = hd * HC * 128
            if not use_spill_load:
                P.dma("pool", lambda e, hcol=hcol: e.dma_start(out=WX[:], in_=w_in[:, hcol:hcol + HC * 128].rearrange("(k p) n -> p k n", p=128)), writes=["WX"])
                for dr in range(2):
                    P.dma("pool", lambda e, dr=dr, hd=hd: e.dma_start(out=WGt[:, dr, 0], in_=w_a[dr, hd].rearrange("(i p) n -> p i n", p=128)), writes=["WG%d0" % dr])
                    P.dma("pool", lambda e, dr=dr, hd=hd: e.dma_start(out=WGt[:, dr, 1], in_=w_x[dr, hd].rearrange("(i p) n -> p i n", p=128)), writes=["WG%d1" % dr])
            for oc in (range(HC) if not use_spill_load else ()):
                ch = hd * HC + oc
                for ti, (a, b) in enumerate(tiles):
                    ps = self.psA[ti % 3]
                    kp = "psA%d" % (ti % 3)
                    def mm(e, oc=oc, a=a, b=b, ps=ps):
                        r = None
                        for k in range(KC):
                            r = e.matmul(ps[:, 0:b - a], lhsT=WX[:, k, oc * 128:(oc + 1) * 128], rhs=self.H[:, k, c0 + a:c0 + b], start=(k == 0), stop=(k == KC - 1))
                        return r
                    P.op("pe", mm, reads=["WX", "H"], writes=[kp])
                    P.op("act", lambda e, oc=oc, a=a, b=b, ps=ps: e.copy(out=XB[:, oc, 2 + a:2 + b], in_=ps[:, 0:b - a]), reads=[kp], writes=["XB"])
                if is_ctx:
                    P.op("dve", lambda e, oc=oc: e.memset(XB[:, oc, 0:2], 0.0), writes=["XB"])
                    P.op("dve", lambda e, oc=oc: e.memset(XB[:, oc, W + 2:W + 3], 0.0), writes=["XB"])
                else:
                    ps = self.psB[0]
                    def mmh(e, oc=oc, ps=ps):
                        r = None
                        for k in range(KC):
                            r = e.matmul(ps[:, 0:3], lhsT=WX[:, k, oc * 128:(oc + 1) * 128], rhs=HH[:, k, :], start=(k == 0), stop=(k == KC - 1))
                        return r
                    P.op("pe", mmh, reads=["WX", "HH"], writes=["psB0"])
                    P.op("dve", lambda e, oc=oc, ps=ps: e.tensor_scalar(out=XB[:, oc, 0:2], in0=ps[:, 0:2], scalar1=EF[:, 0:1], scalar2=None, op0=ALU.mult), reads=["psB0", "EF"], writes=["XB"])
                    P.op("dve", lambda e, oc=oc, ps=ps: e.tensor_scalar(out=XB[:, oc, W + 2:W + 3], in0=ps[:, 2:3], scalar1=EF[:, 1:2], scalar2=None, op0=ALU.mult), reads=["psB0", "EF"], writes=["XB"])
                P.op("dve", lambda e, oc=oc, ch=ch: e.tensor_scalar(out=XC[:, oc, :], in0=XB[:, oc, 0:W], scalar1=lp["CW"][:, 0, ch:ch + 1], scalar2=lp["CB"][:, ch:ch + 1], op0=ALU.mult, op1=ALU.add),
                     reads=["XB"] + lk, writes=["XC"])
                for kk in range(1, 4):
                    P.op("dve", lambda e, oc=oc, ch=ch, kk=kk: e.scalar_tensor_tensor(out=XC[:, oc, :], in0=XB[:, oc, kk:kk + W], scalar=lp["CW"][:, kk, ch:ch + 1], in1=XC[:, oc, :], op0=ALU.mult, op1=ALU.add),
                         reads=["XB", "XC"] + lk, writes=["XC"])
                P.op("act", lambda e, oc=oc: e.copy(out=XCb[:, oc, :], in_=XC[:, oc, :]), reads=["XC"], writes=["XCb"])
            if need_out:
                P.dma("pool", lambda e, hcol=hcol: e.dma_start(out=WX[:], in_=w_in[:, cfg.D + hcol:cfg.D + hcol + HC * 128].rearrange("(k p) n -> p k n", p=128)), writes=["WX"])
                P.dma("pool", lambda e, hcol=hcol: e.dma_start(out=WO[:], in_=w_out[hcol:hcol + HC * 128, :].rearrange("(i p) n -> p i n", p=128)), writes=["WO"])
                for oc in range(HC):
                    for ti, (a, b) in enumerate(tiles):
                        ps = self.psA[ti % 3]
                        kp = "psA%d" % (ti % 3)
                        def mmg(e, oc=oc, a=a, b=b, ps=ps):
                            r = None
                            for k in range(KC):
                                r = e.matmul(ps[:, 0:b - a], lhsT=WX[:, k, oc * 128:(oc + 1) * 128], rhs=self.H[:, k, c0 + a:c0 + b], start=(k == 0), stop=(k == KC - 1))
                            return r
                        P.op("pe", mmg, reads=["WX", "H"], writes=[kp])
                        P.op("act", lambda e, oc=oc, a=a, b=b, ps=ps: e.activation(out=GG[:, oc, a:b], in_=ps[:, 0:b - a], func=AF.Gelu_apprx_tanh), reads=[kp], writes=["GG"])
            for oc in range(HC):
                ch = hd * HC + oc
                def chain(dr, R, A, A2, SR, kR, kA, kA2, kSR, oc=oc, ch=ch):
                    I = HS[:, 1, :]
                    si = (ch * 2 + dr) * 2
                    if use_spill_load:
                        P.dma("sp", lambda e, si=si: e.dma_start(out=A[:], in_=SPILL[si]), reads=["spill%d" % si], writes=[kA])
                        P.dma("sp", lambda e, si=si: e.dma_start(out=A2[:], in_=SPILL[si + 1]), reads=["spill%d" % (si + 1)], writes=[kA2])
                    else:
                        def gate_mm(gi, dst, kdst, oc=oc, dr=dr, ch=ch):
                            for ti, (a, b) in enumerate(tiles):
                                ps = self.psB[ti % 3]
                                kp = "psB%d" % (ti % 3)
                                def mmz(e, oc=oc, dr=dr, gi=gi, a=a, b=b, ps=ps):
                                    r = None
                                    for ic in range(HC):
                                        r = e.matmul(ps[:, 0:b - a], lhsT=WGt[:, dr, gi, ic, oc * 128:(oc + 1) * 128], rhs=XCb[:, ic, a:b], start=(ic == 0), stop=(ic == HC - 1))
                                    return r
                                P.op("pe", mmz, reads=["WG%d%d" % (dr, gi), "XCb"], writes=[kp])
                                bias = lp["BA"] if gi == 0 else lp["BX"]
                                P.op("act", lambda e, ch=ch, dr=dr, a=a, b=b, ps=ps, bias=bias, dst=dst: e.activation(out=dst[:, a:b], in_=ps[:, 0:b - a], func=AF.Sigmoid, bias=bias[:, dr, ch:ch + 1], scale=1.0), reads=[kp] + lk, writes=[kdst])
                        gate_mm(0, R, kR)
                        gate_mm(1, I, "HS1")
                        P.op("act", lambda e, ch=ch, dr=dr: e.activation(out=A[:], in_=R[:], func=AF.Exp, scale=lp["C1"][:, dr, ch:ch + 1]), reads=[kR] + lk, writes=[kA])
                        P.op("act", lambda e, ch=ch, dr=dr: e.activation(out=A2[:], in_=R[:], func=AF.Exp, scale=lp["C2"][:, dr, ch:ch + 1]), reads=[kR] + lk, writes=[kA2])
                        if phase == 1:
                            P.op("dve", lambda e: e.reduce_sum(out=SR[:], in_=R[:], axis=AX.X), reads=[kR], writes=[kSR])
                            P.op("act", lambda e, dr=dr, ch=ch: e.activation(out=PSOUT[:, dr, 0, ch:ch + 1], in_=SR[:], func=AF.Exp, scale=lp["C1"][:, dr, ch:ch + 1]), reads=[kSR] + lk, writes=["PSOUT"])
                        P.op("act", lambda e: e.activation(out=A2[:], in_=A2[:], func=AF.Ln, bias=1.0, scale=-1.0), reads=[kA2], writes=[kA2])
                        P.op("act", lambda e: e.activation(out=A2[:], in_=A2[:], func=AF.Exp, scale=0.5), reads=[kA2], writes=[kA2])
                        P.op("dve", lambda e: e.tensor_tensor(out=A2[:], in0=A2[:], in1=I, op=ALU.mult), reads=[kA2, "HS1"], writes=[kA2])
                        P.op("dve", lambda e, oc=oc: e.tensor_tensor(out=A2[:], in0=A2[:], in1=XC[:, oc, :], op=ALU.mult), reads=[kA2, "XC"], writes=[kA2])
                        if phase == 1 and SPILL is not None:
                            P.dma("sp", lambda e, si=si: e.dma_start(out=SPILL[si], in_=A[:]), reads=[kA], writes=["spill%d" % si])
                            P.dma("sp", lambda e, si=si: e.dma_start(out=SPILL[si + 1], in_=A2[:]), reads=[kA2], writes=["spill%d" % (si + 1)])
                    if phase == 1:
                        init = 0.0
                        rk = []
                    elif is_ctx:
                        init = 0.0
                        rk = []
                    else:
                        init = CARRY[:, dr, ch:ch + 1]
                        rk = ["CARRY"]
                    if dr == 0:
                        P.op("dve", lambda e, init=init: e.tensor_tensor_scan(out=HS[:, 0, :], data0=A[:], data1=A2[:], initial=init, op0=ALU.mult, op1=ALU.add), reads=[kA, kA2] + rk, writes=["HS0"])
                    else:
                        P.op("dve", lambda e, init=init: e.tensor_tensor_scan(out=HS[:, 1, ::-1], data0=A[:, ::-1], data1=A2[:, ::-1], initial=init, op0=ALU.mult, op1=ALU.add), reads=[kA, kA2] + rk, writes=["HS1"])
                    fin = (W - 1) if dr == 0 else 0
                    if phase == 1:
                        P.op("dve", lambda e, dr=dr, ch=ch, fin=fin: e.tensor_copy(out=PSOUT[:, dr, 1, ch:ch + 1], in_=HS[:, dr, fin:fin + 1]), reads=["HS%d" % dr], writes=["PSOUT"])
                    elif is_ctx:
                        P.op("dve", lambda e, dr=dr, ch=ch, fin=fin: e.tensor_copy(out=CFIN[:, dr, ch:ch + 1], in_=HS[:, dr, fin:fin + 1]), reads=["HS%d" % dr], writes=["CFIN"])
                for dr in range(2):
                    b_ = dr % nbuf
                    chain(dr, Rl[b_], Al[b_], A2l[b_], SRl[dr], "R%d" % b_, "A%d" % b_, "A2%d" % b_, "SR%d" % dr)
                if need_out:
                    P.op("dve", lambda e: e.tensor_tensor(out=HS[:, 0, :], in0=HS[:, 0, :], in1=HS[:, 1, :], op=ALU.add), reads=["HS0", "HS1"], writes=["HS0"])
                    P.op("dve", lambda e, oc=oc: e.tensor_tensor(out=YG[:, oc, :], in0=HS[:, 0, :], in1=GG[:, oc, :], op=ALU.mult), reads=["HS0", "GG"], writes=["YG"])
            if need_out:
                self.outproj_into_x(WO, HC, lambda ic, a, b: YG[:, ic, a:b], "YG", "WO", mods["GT_A"], mods["keys"], c0, c1, s)
        self.full_barrier()
        es.close()

    def sgu_params(self, prm, pstack):
        cfg, P = self.cfg, self.P
        KC, SG = cfg.KC, cfg.SGROUPS
        d = {}
        d["WST"] = self.sb(self.key("sguWST"), [128, SG, 128], BF16, pstack)
        d["LG"] = self.sb(self.key("sguLG"), [128, KC], F32, pstack)
        d["CC"] = self.sb(self.key("sguCC"), [128, KC, 128], F32, pstack)
        es = ExitStack()
        WSn = self.sb(self.key("sguWSn"), [128, SG, 128], F32, es)
        LB = self.sb(self.key("sguLB"), [128, KC], F32, es)
        BS = self.sb(self.key("sguBS"), [128, SG, 128], F32, es)
        P.dma("sp", lambda e: e.dma_start(out=WSn[:], in_=prm["sgu_w_s"].rearrange("g p q -> p g q")), writes=["sguWSn"])
        with self.nc.allow_non_contiguous_dma(reason="tiny"):
            P.dma("sp", lambda e: e.dma_start(out=d["LG"][:], in_=prm["sgu_ln_g"].rearrange("(k p) -> p k", p=128), allow_slow_non_contiguous=True), writes=["sguLG"])
            P.dma("sp", lambda e: e.dma_start(out=LB[:], in_=prm["sgu_ln_b"].rearrange("(k p) -> p k", p=128), allow_slow_non_contiguous=True), writes=["sguLB"])
        bsrc = prm["sgu_b_s"]
        bs_bc = bass.AP(bsrc.tensor, bsrc.offset, [[0, 128], [1, SG * 128]])
        P.dma("sp", lambda e: e.dma_start(out=BS[:].rearrange("p g q -> p (g q)"), in_=bs_bc), writes=["sguBS"])
        for g in range(SG):
            ps = self.psA[g % 2]
            kp = "psA%d" % (g % 2)
            P.op("pe", lambda e, g=g, ps=ps: e.transpose(ps[:, 0:128], WSn[:, g, :], self.ident[:]), reads=["sguWSn", "ident"], writes=[kp])
            P.op("act", lambda e, g=g, ps=ps: e.copy(out=d["WST"][:, g, :], in_=ps[:, 0:128]), reads=[kp], writes=["sguWST"])
            ps2 = self.psB[g % 2]
            kp2 = "psB%d" % (g % 2)
            P.op("pe", lambda e, g=g, ps2=ps2: e.matmul(ps2[:, 0:128], lhsT=self.ones16[:], rhs=d["WST"][:, g, :], start=True, stop=True), reads=["sguWST", "ones16"], writes=[kp2])
            for cc in range(cfg.GCH):
                ch = g * cfg.GCH + cc
                P.op("dve", lambda e, g=g, ch=ch, ps2=ps2: e.scalar_tensor_tensor(out=d["CC"][:, ch, :], in0=ps2[:, 0:128], scalar=LB[:, ch:ch + 1], in1=BS[:, g, :], op0=ALU.mult, op1=ALU.add),
                     reads=[kp2, "sguLB", "sguBS"], writes=["sguCC"])
        d["keys"] = ["sguWST", "sguLG", "sguCC"]
        self.full_barrier()
        es.close()
        return d

    def sgu_pass(self, prm, sp, mods, c0, c1, is_ctx):
        cfg, P = self.cfg, self.P
        KC, GCH, D = cfg.KC, cfg.GCH, cfg.D
        W = c1 - c0
        NT = W // 128
        s = 1 if is_ctx else 0
        w_in, w_out = prm["sgu_w_in"], prm["sgu_w_out"]
        es = ExitStack()
        VT = self.sb(self.key("VT"), [128, NT, D], BF16, es)
        VW = GCH * 128
        WV = [self.sb(self.key("WV"), [128, KC, VW], BF16, es) for _ in range(2)]
        SQ = self.sb(self.key("SQ"), [128, D], BF16, es)
        ST = self.sb(self.key("ST"), [128, NT, 4], F32, es)
        WU = WV[0]
        WO = WV[1][:].rearrange("p k n -> p (k n)").rearrange("p (i n) -> p i n", i=GCH)
        U = self.sb(self.key("U"), [128, GCH, W], BF16, es)
        VM = self.sb(self.key("VM"), [128, 512], F32, es)
        if GCH * W <= D:
            PR = SQ[:, 0:GCH * W].rearrange("p (c w) -> p c w", c=GCH)
        else:
            PR = self.sb(self.key("PR"), [128, GCH, W], BF16, es)
        for vt in range(D // VW):
            wb = WV[vt % 2]
            kw = "WV%d" % (vt % 2)
            P.dma("pool", lambda e, vt=vt, wb=wb: e.dma_start(out=wb[:], in_=w_in[:, D + vt * VW:D + (vt + 1) * VW].rearrange("(k p) n -> p k n", p=128)), writes=[kw])
            for n in range(NT):
                ps = self.psA[n % 3]
                kp = "psA%d" % (n % 3)
                def mmv(e, n=n, wb=wb, ps=ps):
                    r = None
                    for k in range(KC):
                        r = e.matmul(ps[:, 0:VW], lhsT=self.H[:, k, c0 + n * 128:c0 + (n + 1) * 128], rhs=wb[:, k, :], start=(k == 0), stop=(k == KC - 1))
                    return r
                P.op("pe", mmv, reads=[kw, "H"], writes=[kp])
                P.op("act", lambda e, n=n, vt=vt, ps=ps: e.activation(out=VT[:, n, vt * VW:(vt + 1) * VW], in_=ps[:, 0:VW], func=AF.Gelu_apprx_tanh), reads=[kp], writes=["VT"])
        for n in range(NT):
            P.op("dve", lambda e, n=n: e.reduce_sum(out=ST[:, n, 0:1], in_=VT[:, n, :], axis=AX.X), reads=["VT"], writes=["ST"])
            P.op("dve", lambda e, n=n: e.tensor_tensor(out=SQ[:], in0=VT[:, n, :], in1=VT[:, n, :], op=ALU.mult), reads=["VT"], writes=["SQ"])
            P.op("dve", lambda e, n=n: e.reduce_sum(out=ST[:, n, 1:2], in_=SQ[:], axis=AX.X), reads=["SQ"], writes=["ST"])
        P.op("dve", lambda e: e.tensor_scalar(out=ST[:, :, 2:3], in0=ST[:, :, 0:1], scalar1=1.0 / D, scalar2=None, op0=ALU.mult), reads=["ST"], writes=["ST"])
        P.op("dve", lambda e: e.tensor_tensor(out=ST[:, :, 3:4], in0=ST[:, :, 2:3], in1=ST[:, :, 2:3], op=ALU.mult), reads=["ST"], writes=["ST"])
        P.op("dve", lambda e: e.scalar_tensor_tensor(out=ST[:, :, 3:4], in0=ST[:, :, 1:2], scalar=1.0 / D, in1=ST[:, :, 3:4], op0=ALU.mult, op1=ALU.subtract), reads=["ST"], writes=["ST"])
        P.op("dve", lambda e: e.tensor_scalar(out=ST[:, :, 3:4], in0=ST[:, :, 3:4], scalar1=EPS, scalar2=None, op0=ALU.add), reads=["ST"], writes=["ST"])
        P.op("act", lambda e: e.activation(out=ST[:, :, 3:4], in_=ST[:, :, 3:4], func=AF.Ln), reads=["ST"], writes=["ST"])
        P.op("act", lambda e: e.activation(out=ST[:, :, 3:4], in_=ST[:, :, 3:4], func=AF.Exp, scale=-0.5), reads=["ST"], writes=["ST"])
        for n in range(NT):
            P.op("dve", lambda e, n=n: e.tensor_scalar(out=VT[:, n, :], in0=VT[:, n, :], scalar1=ST[:, n, 2:3], scalar2=ST[:, n, 3:4], op0=ALU.subtract, op1=ALU.mult), reads=["VT", "ST"], writes=["VT"])
        NB = 4
        for g in range(cfg.SGROUPS):
            gcol = g * GCH * 128
            P.dma("pool", lambda e, gcol=gcol: e.dma_start(out=WU[:], in_=w_in[:, gcol:gcol + GCH * 128].rearrange("(k p) n -> p k n", p=128)), writes=["WV0"])
            P.dma("pool", lambda e, gcol=gcol: e.dma_start(out=WO, in_=w_out[gcol:gcol + GCH * 128, :].rearrange("(i p) n -> p i n", p=128)), writes=["WV1"])
            for cc in range(GCH):
                ch = g * GCH + cc
                for ti, (a, b) in enumerate(col_tiles(0, W)):
                    ps = self.psA[ti % 3]
                    kp = "psA%d" % (ti % 3)
                    def mmu(e, cc=cc, a=a, b=b, ps=ps):
                        r = None
                        for k in range(KC):
                            r = e.matmul(ps[:, 0:b - a], lhsT=WU[:, k, cc * 128:(cc + 1) * 128], rhs=self.H[:, k, c0 + a:c0 + b], start=(k == 0), stop=(k == KC - 1))
                        return r
                    P.op("pe", mmu, reads=["WV0", "H"], writes=[kp])
                    P.op("act", lambda e, cc=cc, a=a, b=b, ps=ps: e.activation(out=U[:, cc, a:b], in_=ps[:, 0:b - a], func=AF.Gelu_apprx_tanh), reads=[kp], writes=["U"])
                for n0 in range(0, NT, NB):
                    nb = min(NB, NT - n0)
                    pi = (n0 // NB) % 3
                    ps = self.psB[pi]
                    kp = "psB%d" % pi
                    def mms(e, g=g, ch=ch, n0=n0, nb=nb, ps=ps):
                        r = None
                        for i in range(nb):
                            r = e.matmul(ps[:, i * 128:(i + 1) * 128], lhsT=VT[:, n0 + i, ch * 128:(ch + 1) * 128], rhs=sp["WST"][:, g, :], start=True, stop=True)
                        return r
                    P.op("pe", mms, reads=["VT"] + sp["keys"], writes=[kp])
                    for i in range(nb):
                        P.op("dve", lambda e, ch=ch, i=i, ps=ps: e.scalar_tensor_tensor(out=VM[:, i * 128:(i + 1) * 128], in0=ps[:, i * 128:(i + 1) * 128], scalar=sp["LG"][:, ch:ch + 1], in1=sp["CC"][:, ch, :], op0=ALU.mult, op1=ALU.add),
                             reads=[kp] + sp["keys"], writes=["VM"])
                    P.op("dve", lambda e, cc=cc, n0=n0, nb=nb: e.tensor_tensor(out=PR[:, cc, n0 * 128:(n0 + nb) * 128], in0=VM[:, 0:nb * 128], in1=U[:, cc, n0 * 128:(n0 + nb) * 128], op=ALU.mult),
                         reads=["VM", "U", "SQ"], writes=["PR", "SQ"])
            self.outproj_into_x(WO, GCH, lambda ic, a, b: PR[:, ic, a:b], "PR", "WV1", mods["GT_A"], mods["keys"], c0, c1, s)
        self.full_barrier()
        es.close()

    def moe_gates(self, router, mods, GATE=None):
        cfg, P = self.cfg, self.P
        KC, T, NE = cfg.KC, cfg.T, cfg.NEXP
        NTC = T // 128
        if GATE is None:
            GATE = self.sb("GATE", [128, NTC, NE], F32)
        es = ExitStack()
        Rt = self.sb(self.key("Rt"), [128, KC, NE], F32, es)
        RG = self.sb(self.key("RG"), [128, 2, KC, NE], F32, es)
        RS = self.sb(self.key("RS"), [128, 2, KC, NE], F32, es)
        CONST = self.sb(self.key("CONST"), [128, 2, NE], F32, es)
        RT = self.sb(self.key("RTK"), [128, NTC], F32, es)
        L = self.sb(self.key("L"), [128, NTC, NE], F32, es)
        L2 = self.sb(self.key("L2"), [128, NTC, NE], F32, es)
        E1 = self.sb(self.key("E1"), [128, NTC, NE], F32, es)
        E2 = self.sb(self.key("E2"), [128, NTC, NE], F32, es)
        M = self.sb(self.key("M"), [128, 4, NTC], F32, es)
        P.dma("sp", lambda e: e.dma_start(out=Rt[:], in_=router.rearrange("(k p) n -> p k n", p=128)), writes=["Rt"])
        for s in range(2):
            for k in range(KC):
                P.op("dve", lambda e, s=s, k=k: e.tensor_scalar(out=RG[:, s, k, :], in0=Rt[:, k, :], scalar1=mods["GS_B"][:, k, s:s + 1], scalar2=None, op0=ALU.mult), reads=["Rt"] + mods["keys"], writes=["RG"])
                P.op("dve", lambda e, s=s, k=k: e.tensor_scalar(out=RS[:, s, k, :], in0=Rt[:, k, :], scalar1=mods["SH_B"][:, k, s:s + 1], scalar2=None, op0=ALU.mult), reads=["Rt"] + mods["keys"], writes=["RS"])
            ps = self.psC[s]
            def mmc(e, s=s, ps=ps):
                r = None
                for k in range(KC):
                    r = e.matmul(ps[:, 0:NE], lhsT=self.ones32[:], rhs=RS[:, s, k, :], start=(k == 0), stop=(k == KC - 1))
                return r
            P.op("pe", mmc, reads=["RS", "ones32"], writes=["psC%d" % s])
            P.op("dve", lambda e, s=s, ps=ps: e.tensor_copy(out=CONST[:, s, :], in_=ps[:, 0:NE]), reads=["psC%d" % s], writes=["CONST"])
        for n in range(NTC):
            s = 1 if n * 128 < cfg.NCX else 0
            ps = self.psA[n % 3]
            kp = "psA%d" % (n % 3)
            def mml(e, n=n, s=s, ps=ps):
                r = None
                for k in range(KC):
                    r = e.matmul(ps[:, 0:NE], lhsT=self.X[:, k, n * 128:(n + 1) * 128], rhs=RG[:, s, k, :], start=(k == 0), stop=(k == KC - 1))
                r = e.matmul(ps[:, 16:17], lhsT=self.RSTD[0:1, n * 128:(n + 1) * 128], rhs=self.ones32[0:1, 0:1], start=True, stop=True)
                return r
            P.op("pe", mml, reads=["X", "RG", "RSTD", "ones32"], writes=[kp])
            P.op("dve", lambda e, n=n, ps=ps: e.tensor_copy(out=RT[:, n:n + 1], in_=ps[:, 16:17]), reads=[kp], writes=["RTK"])
            P.op("dve", lambda e, n=n, s=s, ps=ps: e.scalar_tensor_tensor(out=L[:, n, :], in0=ps[:, 0:NE], scalar=RT[:, n:n + 1], in1=CONST[:, s, :], op0=ALU.mult, op1=ALU.add), reads=[kp, "RTK", "CONST"], writes=["L"])
        P.op("dve", lambda e: e.reduce_max(out=M[:, 0, :], in_=L[:], axis=AX.X), reads=["L"], writes=["M0"])
        P.op("dve", lambda e: e.tensor_tensor(out=E1[:], in0=L[:], in1=M[:, 0, :].unsqueeze(2).to_broadcast([128, NTC, NE]), op=ALU.is_equal), reads=["L", "M0"], writes=["E1"])
        P.op("dve", lambda e: e.scalar_tensor_tensor(out=L2[:], in0=E1[:], scalar=-1e30, in1=L[:], op0=ALU.mult, op1=ALU.add), reads=["E1", "L"], writes=["L2"])
        P.op("dve", lambda e: e.reduce_max(out=M[:, 1, :], in_=L2[:], axis=AX.X), reads=["L2"], writes=["M1"])
        P.op("dve", lambda e: e.tensor_tensor(out=E2[:], in0=L2[:], in1=M[:, 1, :].unsqueeze(2).to_broadcast([128, NTC, NE]), op=ALU.is_equal), reads=["L2", "M1"], writes=["E2"])
        P.op("dve", lambda e: e.tensor_tensor(out=M[:, 2, :], in0=M[:, 1, :], in1=M[:, 0, :], op=ALU.subtract), reads=["M0", "M1"], writes=["M2"])
        P.op("act", lambda e: e.activation(out=M[:, 3, :], in_=M[:, 2, :], func=AF.Exp), reads=["M2"], writes=["M3"])
        P.op("dve", lambda e: e.tensor_scalar(out=M[:, 2, :], in0=M[:, 3, :], scalar1=1.0, scalar2=None, op0=ALU.add), reads=["M3"], writes=["M2"])
        P.op("dve", lambda e: e.reciprocal(out=M[:, 2, :], in_=M[:, 2, :]), reads=["M2"], writes=["M2"])
        P.op("dve", lambda e: e.tensor_tensor(out=M[:, 3, :], in0=M[:, 3, :], in1=M[:, 2, :], op=ALU.mult), reads=["M2", "M3"], writes=["M3"])
        P.op("dve", lambda e: e.tensor_tensor(out=E1[:], in0=E1[:], in1=M[:, 2, :].unsqueeze(2).to_broadcast([128, NTC, NE]), op=ALU.mult), reads=["E1", "M2"], writes=["E1"])
        P.op("dve", lambda e: e.tensor_tensor(out=E2[:], in0=E2[:], in1=M[:, 3, :].unsqueeze(2).to_broadcast([128, NTC, NE]), op=ALU.mult), reads=["E2", "M3"], writes=["E2"])
        P.op("dve", lambda e: e.tensor_tensor(out=GATE[:], in0=E1[:], in1=E2[:], op=ALU.add), reads=["E1", "E2"], writes=["GATE"])
        self.full_barrier()
        es.close()
        return GATE

    def gate_bcast_keyed(self, GATE, ex, GBC, DG, gkey):
        return self.gate_bcast(GATE, ex, GBC, DG, gkey)

    def gate_bcast(self, GATE, ex, GBC, DG, gkey="GATE"):
        cfg, P = self.cfg, self.P
        NTC = cfg.T // 128
        for n0 in range(0, NTC, 4):
            nb = min(4, NTC - n0)
            pi = (n0 // 4) % 3
            ps = self.psB[pi]
            kp = "psB%d" % pi
            for i in range(nb):
                P.op("dve", lambda e, i=i, n0=n0: e.tensor_scalar(out=DG[:, i, :], in0=self.ident[:], scalar1=GATE[:, n0 + i, ex:ex + 1], scalar2=None, op0=ALU.mult), reads=["ident", gkey], writes=["DG%d" % i])
            def mmb(e, nb=nb, ps=ps):
                r = None
                for i in range(nb):
                    r = e.matmul(ps[:, i * 128:(i + 1) * 128], lhsT=self.ones32[:], rhs=DG[:, i, :], start=True, stop=True)
                return r
            P.op("pe", mmb, reads=["ones32"] + ["DG%d" % i for i in range(nb)], writes=[kp])
            P.op("act", lambda e, n0=n0, nb=nb, ps=ps: e.copy(out=GBC[:, n0 * 128:(n0 + nb) * 128], in_=ps[:, 0:nb * 128]), reads=[kp], writes=["GBC"])

    def finish(self):
        with self.nc.Block() as block:
            self.P.replay(block)
        self.es.close()
        return self.nc


LRU_KEYS = ["lru_w_in", "lru_conv_w", "lru_conv_b", "lru_w_a", "lru_b_a", "lru_w_x", "lru_b_x", "lru_lambda", "lru_w_out"]


def build_stage(cfg, stage):
    B = Builder(cfg, stage)
    nc, P = B.nc, B.P
    D, KC, T, NL, NCX, F = cfg.D, cfg.KC, cfg.T, cfg.NL, cfg.NCX, cfg.F
    HD = D // cfg.HEADS
    xin = B.din("xin", [NL, D])
    xhalo = B.din("xhalo", [3, D])
    cin = B.din("cin", [NCX, D])
    ef = B.din("ef", [128, 2])
    c_v = B.din("c", [D])
    cctx_v = B.din("c_ctx", [D])
    li0 = 0 if stage in (1, 2) else 2
    layers = [li0] if stage in (1, 3) else [li0, li0 + 1]
    w_mod = {li: B.din("w_mod%d" % li, [D, 6 * D]) for li in layers}
    b_mod = {li: B.din("b_mod%d" % li, [6 * D]) for li in layers}
    norm_g = {li: B.din("norm_g%d" % li, [2, D]) for li in layers}
    prm = {}
    shapes = {"lru_w_in": [D, 2 * D], "lru_conv_w": [4, D], "lru_conv_b": [D], "lru_w_a": [2, cfg.HEADS, HD, HD], "lru_b_a": [2, D],
              "lru_w_x": [2, cfg.HEADS, HD, HD], "lru_b_x": [2, D], "lru_lambda": [2, D], "lru_w_out": [D, D]}
    for k in LRU_KEYS:
        prm[k] = B.din(k, shapes[k])
    if stage in (2, 4):
        pscar = B.din("carry_ps", [2, 7, 2, D])
        prm["ffn_w1"] = B.din("ffn_w1", [D, F])
        prm["ffn_w3"] = B.din("ffn_w3", [D, F])
        prm["ffn_w2"] = B.din("ffn_w2", [F, D])
        prm["sgu_w_in"] = B.din("sgu_w_in", [D, 2 * D])
        prm["sgu_ln_g"] = B.din("sgu_ln_g", [D])
        prm["sgu_ln_b"] = B.din("sgu_ln_b", [D])
        prm["sgu_w_s"] = B.din("sgu_w_s", [cfg.SGROUPS, 128, 128])
        prm["sgu_b_s"] = B.din("sgu_b_s", [cfg.SGROUPS, 128])
        prm["sgu_w_out"] = B.din("sgu_w_out", [D, D])
        prm["moe_router"] = B.din("moe_router", [D, cfg.NEXP])
        prm["moe_w1"] = B.din("moe_w1", [cfg.NEXP, D, F])
        prm["moe_w3"] = B.din("moe_w3", [cfg.NEXP, D, F])
        prm["moe_w2"] = B.din("moe_w2", [cfg.NEXP, F, D])
    if stage == 4:
        final_g = B.din("final_g", [D])
    B.setup_common()
    X = B.X
    XH = B.sb("XH", [128, KC, 3], F32)
    HH = B.sb("HH", [128, KC, 3], BF16)
    EF = B.sb("EF", [128, 2], F32)
    es = ExitStack()
    TM = [B.sb("TM%d" % i, [128, D], F32, es) for i in range(2)]
    P.dma("sp", lambda e: e.dma_start(out=EF[:], in_=ef), writes=["EF"])
    NTC = T // 128
    for n in range(NTC):
        tm = TM[n % 2]
        kt = "TM%d" % (n % 2)
        src = cin[n * 128:(n + 1) * 128, :] if n * 128 < NCX else xin[n * 128 - NCX:(n + 1) * 128 - NCX, :]
        P.dma("sp", lambda e, tm=tm, src=src: e.dma_start(out=tm[:], in_=src), writes=[kt])
        for k0 in range(0, KC, 4):
            nk = min(4, KC - k0)
            pi = (k0 // 4) % 3
            ps = B.psA[pi]
            kp = "psA%d" % pi
            def tr(e, tm=tm, k0=k0, nk=nk, ps=ps):
                r = None
                for i in range(nk):
                    r = e.transpose(ps[:, i * 128:(i + 1) * 128], tm[:, (k0 + i) * 128:(k0 + i + 1) * 128], B.ident[:])
                return r
            P.op("pe", tr, reads=[kt, "ident"], writes=[kp])
            P.op("act", lambda e, n=n, k0=k0, nk=nk, ps=ps: e.copy(out=X[:, k0:k0 + nk, n * 128:(n + 1) * 128], in_=ps[:, 0:nk * 128].rearrange("p (k t) -> p k t", k=nk)), reads=[kp], writes=["X"])
    with nc.allow_non_contiguous_dma(reason="3 halo rows"):
        k_ = B.fm_rows(lambda i: XH[:, :, i], xhalo, 3, "XH")
        P.op("dve", lambda e: e.tensor_copy(out=XH[:], in_=XH[:]), reads=k_, writes=["XH"])
    B.full_barrier()
    es.close()

    mods = {li: B.compute_mods(li, w_mod[li], b_mod[li], c_v, cctx_v, norm_g[li]) for li in layers}
    m0 = mods[li0]
    B.norm_mod(m0["GS_A"], m0["SH_A"], m0["keys"])
    es = ExitStack()
    sqh = B.sb("sqh", [128, KC, 3], F32, es)
    rh = B.sb("rh", [128, 3], F32, es)
    th = B.sb("th", [128, KC, 3], F32, es)
    P.op("dve", lambda e: e.tensor_tensor(out=sqh[:], in0=XH[:], in1=XH[:], op=ALU.mult), reads=["XH"], writes=["sqh"])
    def mmh(e):
        r = None
        for k in range(KC):
            r = e.matmul(B.psC[0][:, 0:3], lhsT=B.ones32[:], rhs=sqh[:, k, :], start=(k == 0), stop=(k == KC - 1))
        return r
    P.op("pe", mmh, reads=["sqh", "ones32"], writes=["psC0"])
    P.op("dve", lambda e: e.tensor_scalar(out=rh[:], in0=B.psC[0][:, 0:3], scalar1=1.0 / D, scalar2=EPS, op0=ALU.mult, op1=ALU.add), reads=["psC0"], writes=["rh"])
    P.op("act", lambda e: e.activation(out=rh[:], in_=rh[:], func=AF.Ln), reads=["rh"], writes=["rh"])
    P.op("act", lambda e: e.activation(out=rh[:], in_=rh[:], func=AF.Exp, scale=-0.5), reads=["rh"], writes=["rh"])
    for k in range(KC):
        P.op("dve", lambda e, k=k: e.scalar_tensor_tensor(out=th[:, k, :], in0=XH[:, k, :], scalar=m0["GS_A"][:, k, 0:1], in1=rh[:], op0=ALU.mult, op1=ALU.mult), reads=["XH", "rh"] + m0["keys"], writes=["th"])
        P.op("act", lambda e, k=k: e.activation(out=HH[:, k, :], in_=th[:, k, :], func=AF.Identity, bias=m0["SH_A"][:, k, 0:1], scale=1.0), reads=["th"] + m0["keys"], writes=["HH"])
    B.full_barrier()
    es.close()

    lp = B.lru_params(0, prm)
    CFIN = B.sb("CFIN", [128, 2, KC], F32)
    ctx_out = (li0 == 0)
    if stage in (1, 3):
        PSO = B.sb("PSO", [128, 2, 2, KC], F32)
        B.lru_pass(prm, lp, m0, NCX, T, False, 1, HH=HH, EF=EF, PSOUT=PSO)
        pso = B.dout("ps_out", [2, 2, D])
        with nc.allow_non_contiguous_dma(reason="tiny"):
            ok_ = []
            for a_ in range(2):
                for b_ in range(2):
                    P.dma("sp", lambda e, a_=a_, b_=b_: e.dma_start(out=pso[a_, b_].rearrange("(k p) -> p k", p=128), in_=PSO[:, a_, b_, :], allow_slow_non_contiguous=True), reads=["PSOUT"], writes=["o_ps%d%d" % (a_, b_)])
                    ok_.append("o_ps%d%d" % (a_, b_))
        P.wait_all("sp", ok_)
        return B.finish(), B

    B.lru_pass(prm, lp, m0, 0, NCX, True, 2, CFIN=CFIN, do_out=ctx_out)
    CAR = B.sb("CAR", [128, 2, KC], F32)
    PSC = B.sb("PSC", [128, 2, 7, 2, KC], F32)
    with nc.allow_non_contiguous_dma(reason="tiny"):
        for dr in range(2):
            k_ = B.fm_rows(lambda i, dr=dr: PSC[:, dr, i // 2, i % 2, :], pscar[dr].rearrange("s b d -> (s b) d"), 14, "PSC%d" % dr)
            P.op("dve", lambda e, dr=dr: e.tensor_copy(out=PSC[:, dr], in_=PSC[:, dr]), reads=k_, writes=["PSC%d" % dr])
    P.op("dve", lambda e: e.tensor_copy(out=CAR[:], in_=CFIN[:]), reads=["CFIN"], writes=["CARRY"])
    for dr in range(2):
        for st in range(7):
            P.op("dve", lambda e, dr=dr, st=st: e.tensor_tensor(out=CAR[:, dr, :], in0=CAR[:, dr, :], in1=PSC[:, dr, st, 0, :], op=ALU.mult), reads=["CARRY", "PSC%d" % dr], writes=["CARRY"])
            P.op("dve", lambda e, dr=dr, st=st: e.tensor_tensor(out=CAR[:, dr, :], in0=CAR[:, dr, :], in1=PSC[:, dr, st, 1, :], op=ALU.add), reads=["CARRY", "PSC%d" % dr], writes=["CARRY"])
    B.lru_pass(prm, lp, m0, NCX, T, False, 2, HH=HH, EF=EF, CARRY=CAR, do_out=True)
    B.norm_mod(m0["GS_B"], m0["SH_B"], m0["keys"])
    B.swiglu_into_x(prm["ffn_w1"], prm["ffn_w3"], prm["ffn_w2"], m0["GT_B"], m0["keys"], tag="ffn")
    m1 = mods[li0 + 1]
    sp_es = ExitStack()
    sp = B.sgu_params(prm, sp_es)
    B.norm_mod(m1["GS_A"], m1["SH_A"], m1["keys"])
    if li0 == 0:
        B.sgu_pass(prm, sp, m1, 0, NCX, True)
    B.sgu_pass(prm, sp, m1, NCX, T, False)
    B.full_barrier()
    sp_es.close()
    B.norm_mod(m1["GS_B"], m1["SH_B"], m1["keys"])
    GATE = B.moe_gates(prm["moe_router"], m1)
    GBC = B.sb("GBC", [128, T], F32)
    DG = B.sb("DG", [128, 4, 128], F32)
    for ex in range(cfg.NEXP):
        B.gate_bcast(GATE, ex, GBC, DG)
        B.swiglu_into_x(prm["moe_w1"][ex], prm["moe_w3"][ex], prm["moe_w2"][ex], m1["GT_B"], m1["keys"], gbc=GBC, gbc_key="GBC", tag="moe")
    es = ExitStack()
    OT = [B.sb("OT%d" % i, [128, D], F32, es) for i in range(2)]
    if stage == 4:
        FG = B.sb("FG", [128, KC], F32, es)
        with nc.allow_non_contiguous_dma(reason="tiny"):
            P.dma("sp", lambda e: e.dma_start(out=FG[:], in_=final_g.rearrange("(k p) -> p k", p=128), allow_slow_non_contiguous=True), writes=["FG"])
        sq = [B.sb("fsq%d" % i, [128, T], BF16, es) for i in range(2)]
        tiles = col_tiles(0, T)
        for k in range(KC):
            s_ = sq[k % 2]
            ks = "fsq%d" % (k % 2)
            P.op("act", lambda e, k=k, s_=s_: e.activation(out=s_[:], in_=X[:, k, :], func=AF.Square), reads=["X"], writes=[ks])
            def mm(e, k=k, s_=s_):
                r = None
                for i, (a, b) in enumerate(tiles):
                    r = e.matmul(B.psA[i][:, 0:b - a], lhsT=B.ones16[:], rhs=s_[:, a:b], start=(k == 0), stop=(k == KC - 1))
                return r
            P.op("pe", mm, reads=[ks, "ones16"], writes=["psA0", "psA1", "psA2"])
        for i, (a, b) in enumerate(tiles):
            P.op("dve", lambda e, i=i, a=a, b=b: e.tensor_scalar(out=B.RSTD[:, a:b], in0=B.psA[i][:, 0:b - a], scalar1=1.0 / D, scalar2=EPS, op0=ALU.mult, op1=ALU.add), reads=["psA%d" % i], writes=["RSTD"])
        P.op("act", lambda e: e.activation(out=B.RSTD[:], in_=B.RSTD[:], func=AF.Ln), reads=["RSTD"], writes=["RSTD"])
        P.op("act", lambda e: e.activation(out=B.RSTD[:], in_=B.RSTD[:], func=AF.Exp, scale=-0.5), reads=["RSTD"], writes=["RSTD"])
        for k in range(KC):
            P.op("dve", lambda e, k=k: e.scalar_tensor_tensor(out=X[:, k, NCX:T], in0=X[:, k, NCX:T], scalar=FG[:, k:k + 1], in1=B.RSTD[:, NCX:T], op0=ALU.mult, op1=ALU.mult), reads=["X", "RSTD", "FG"], writes=["X"])
    xout = B.dout("xout", [NL, D])
    cout = B.dout("cout", [NCX, D])
    okeys = []
    for n in range(NTC):
        ot = OT[n % 2]
        ko = "OT%d" % (n % 2)
        for k0 in range(0, KC, 4):
            nk = min(4, KC - k0)
            pi = (k0 // 4) % 3
            ps = B.psA[pi]
            kp = "psA%d" % pi
            def tr(e, n=n, k0=k0, nk=nk, ps=ps):
                r = None
                for i in range(nk):
                    r = e.transpose(ps[:, i * 128:(i + 1) * 128], X[:, k0 + i, n * 128:(n + 1) * 128], B.ident[:])
                return r
            P.op("pe", tr, reads=["X", "ident"], writes=[kp])
            P.op("act", lambda e, ot=ot, k0=k0, nk=nk, ps=ps: e.copy(out=ot[:, k0 * 128:(k0 + nk) * 128], in_=ps[:, 0:nk * 128]), reads=[kp], writes=[ko])
        dst = cout[n * 128:(n + 1) * 128, :] if n * 128 < NCX else xout[n * 128 - NCX:(n + 1) * 128 - NCX, :]
        kk = "o_x%d" % n
        P.dma("sp", lambda e, ot=ot, dst=dst: e.dma_start(out=dst, in_=ot[:]), reads=[ko], writes=[kk])
        okeys.append(kk)
    P.wait_all("sp", okeys)
    es.close()
    return B.finish(), B


_PROG_CACHE = {}


def _get_prog(cfg, stage):
    key = (cfg.D, cfg.NL, cfg.NCX, cfg.F, cfg.HEADS, cfg.SGROUPS, cfg.NEXP, stage)
    if key not in _PROG_CACHE:
        _PROG_CACHE[key] = build_stage(cfg, stage)
    return _PROG_CACHE[key]


def run_module(cfg, inp):
    NCO, NL, D = cfg.NCORES, cfg.NL, cfg.D
    f = lambda a: np.ascontiguousarray(a, dtype=np.float32)
    x = f(inp["x"][0])
    ctx = f(inp["ctx"][0])
    ident = np.eye(128, dtype=np.float32)
    zero_row = np.zeros((1, D), np.float32)

    def halos(xfull, c):
        lo = xfull[c * NL - 2:c * NL] if c > 0 else np.concatenate([zero_row, zero_row], 0)
        hi = xfull[(c + 1) * NL:(c + 1) * NL + 1] if c < NCO - 1 else zero_row
        return f(np.concatenate([lo, hi], 0))

    def efl(c):
        e = np.ones((128, 2), np.float32)
        if c == 0:
            e[:, 0] = 0.0
        if c == NCO - 1:
            e[:, 1] = 0.0
        return e

    def lru_inputs(j):
        d = {}
        for k in LRU_KEYS:
            a = inp[k][j]
            if k in ("lru_b_a", "lru_b_x"):
                a = a.reshape(2, D)
            d[k] = f(a)
        return d

    def base(c, xfull, cfull, lis):
        d = {"xin": f(xfull[c * NL:(c + 1) * NL]), "xhalo": halos(xfull, c), "cin": cfull, "ef": efl(c), "ident": ident,
             "c": f(inp["c"][0]), "c_ctx": f(inp["c_ctx"])}
        for li in lis:
            d["w_mod%d" % li] = f(inp["w_mod"][li])
            d["b_mod%d" % li] = f(inp["b_mod"][li])
            d["norm_g%d" % li] = f(inp["norm_g"][li])
        return d

    def carries(ps_all, c):
        out = np.zeros((2, 7, 2, D), np.float32)
        out[:, :, 0, :] = 1.0
        seq_f = list(range(0, c))
        seq_r = list(range(NCO - 1, c, -1))
        for i, k in enumerate(seq_f):
            out[0, i] = ps_all[k][0]
        for i, k in enumerate(seq_r):
            out[1, i] = ps_all[k][1]
        return out

    cores = list(range(NCO))
    for half in range(2):
        li0, j = 2 * half, half
        lw = lru_inputs(j)
        nc1, _ = _get_prog(cfg, 1 if half == 0 else 3)
        maps = []
        for c in cores:
            d = base(c, x, ctx, [li0])
            d.update(lw)
            maps.append(d)
        res = run_bass_kernel_spmd(nc1, maps, core_ids=cores)
        ps_all = [res.results[c]["ps_out"] for c in cores]
        nc2, _ = _get_prog(cfg, 2 if half == 0 else 4)
        maps = []
        for c in cores:
            d = base(c, x, ctx, [li0, li0 + 1])
            d.update(lw)
            d["carry_ps"] = carries(ps_all, c)
            d["ffn_w1"], d["ffn_w3"], d["ffn_w2"] = f(inp["ffn_w1"][j]), f(inp["ffn_w3"][j]), f(inp["ffn_w2"][j])
            for k in ("sgu_w_in", "sgu_ln_g", "sgu_ln_b", "sgu_w_s", "sgu_b_s", "sgu_w_out", "moe_router", "moe_w1", "moe_w3", "moe_w2"):
                d[k] = f(inp[k][j])
            if half == 1:
                d["final_g"] = f(inp["final_g"])
            maps.append(d)
        res = run_bass_kernel_spmd(nc2, maps, core_ids=cores)
        x = np.concatenate([res.results[c]["xout"] for c in cores], 0)
        ctx = f(res.results[0]["cout"])
    return x[None].astype(np.float32)


QUADS = [[0, 1, 2, 3], [4, 5, 6, 7]]
XPAIRS = [[0, 4], [1, 5], [2, 6], [3, 7]]


def exchange(B, src_ap, n, tag, rkey, stack=None):
    nc, P = B.nc, B.P
    b0 = nc.dram_tensor("xb0_" + tag, [128, n], F32)
    b1 = nc.dram_tensor("xb1_" + tag, [4 * 128, n], F32)
    b2 = nc.dram_tensor("xb2_" + tag, [8 * 128, n], F32)
    G = B.sb("XG_" + tag, [128, 8, n], F32, stack)
    P.dma("sp", lambda e: e.dma_start(out=b0.ap(), in_=src_ap), reads=[rkey], writes=["xb0_" + tag])
    P.collective(lambda e: e.collective_compute("AllGather", ALU.bypass, replica_groups=QUADS, ins=[b0.ap().opt()], outs=[b1.ap().opt()]),
                 reads=["xb0_" + tag], writes=["xb1_" + tag])
    P.collective(lambda e: e.collective_compute("AllGather", ALU.bypass, replica_groups=XPAIRS, ins=[b1.ap().opt()], outs=[b2.ap().opt()]),
                 reads=["xb1_" + tag], writes=["xb2_" + tag])
    P.dma("sp", lambda e: e.dma_start(out=G[:], in_=b2.ap().rearrange("(r p) n -> p r n", p=128)), reads=["xb2_" + tag], writes=["XG_" + tag])
    return G


def build_fused(cfg):
    B = Builder(cfg, 0)
    nc, P = B.nc, B.P
    D, KC, T, NL, NCX, F = cfg.D, cfg.KC, cfg.T, cfg.NL, cfg.NCX, cfg.F
    HD = D // cfg.HEADS
    xin = B.din("xin", [NL, D])
    xhalo = B.din("xhalo", [3, D])
    cin = B.din("cin", [NCX, D])
    ef = B.din("ef", [128, 2])
    selm = B.din("selmask", [128, 16])
    carm = B.din("carmask", [128, 16])
    c_v = B.din("c", [D])
    cctx_v = B.din("c_ctx", [D])
    w_mod = {li: B.din("w_mod%d" % li, [D, (6 * D) // 8]) for li in range(4)}
    b_mod = {li: B.din("b_mod%d" % li, [(6 * D) // 8]) for li in range(4)}
    norm_g = {li: B.din("norm_g%d" % li, [2, D]) for li in range(4)}
    final_g = B.din("final_g", [D])
    shapes = {"lru_w_in": [D, 2 * D], "lru_conv_w": [4, D], "lru_conv_b": [D], "lru_w_a": [2, cfg.HEADS, HD, HD], "lru_b_a": [2, D],
              "lru_w_x": [2, cfg.HEADS, HD, HD], "lru_b_x": [2, D], "lru_lambda": [2, D], "lru_w_out": [D, D],
              "ffn_w1": [D, F], "ffn_w3": [D, F], "ffn_w2": [F, D], "sgu_w_in": [D, 2 * D], "sgu_ln_g": [D], "sgu_ln_b": [D],
              "sgu_w_s": [cfg.SGROUPS, 128, 128], "sgu_b_s": [cfg.SGROUPS, 128], "sgu_w_out": [D, D], "moe_router": [D, cfg.NEXP],
              "moe_w1": [cfg.NEXP, D, F], "moe_w3": [cfg.NEXP, D, F], "moe_w2": [cfg.NEXP, F, D]}
    prms = []
    for j in range(2):
        prms.append({k: B.din("%s_%d" % (k, j), shp) for k, shp in shapes.items()})
    cx_w1 = B.din("moe_ctx_w1", [D, F])
    cx_w3 = B.din("moe_ctx_w3", [D, F])
    cx_w2 = B.din("moe_ctx_w2", [F, D])
    ohexp_d = B.din("ohexp", [128, cfg.NEXP])
    B.setup_common()
    X = B.X
    XH = B.sb("XH", [128, KC, 3], F32)
    HH = B.sb("HH", [128, KC, 3], BF16)
    EF = B.sb("EF", [128, 2], F32)
    SEL = B.sb("SEL", [128, 16], F32)
    CMK = B.sb("CMK", [128, 16], F32)
    CFIN = B.sb("CFIN", [128, 2, KC], F32)
    PSO = B.sb("PSO", [128, 2, 2, KC], F32)
    CAR = B.sb("CAR", [128, 2, KC], F32)
    T1 = B.sb("T1c", [128, KC], F32)
    HSRC = B.sb("HSRC", [128, KC, 3], F32)
    P.dma("sp", lambda e: e.dma_start(out=EF[:], in_=ef), writes=["EF"])
    P.dma("sp", lambda e: e.dma_start(out=SEL[:], in_=selm), writes=["SEL"])
    P.dma("sp", lambda e: e.dma_start(out=CMK[:], in_=carm), writes=["CMK"])
    OHE = B.sb("OHE", [128, cfg.NEXP], F32)
    P.dma("sp", lambda e: e.dma_start(out=OHE[:], in_=ohexp_d), writes=["OHE"])
    es = ExitStack()
    TM = [B.sb("TM%d" % i, [128, D], F32, es) for i in range(2)]
    NTC = T // 128
    for n in range(NTC):
        tm = TM[n % 2]
        kt = "TM%d" % (n % 2)
        src = cin[n * 128:(n + 1) * 128, :] if n * 128 < NCX else xin[n * 128 - NCX:(n + 1) * 128 - NCX, :]
        P.dma("sp", lambda e, tm=tm, src=src: e.dma_start(out=tm[:], in_=src), writes=[kt])
        for k0 in range(0, KC, 4):
            nk = min(4, KC - k0)
            pi = (k0 // 4) % 3
            ps = B.psA[pi]
            kp = "psA%d" % pi
            def tr(e, tm=tm, k0=k0, nk=nk, ps=ps):
                r = None
                for i in range(nk):
                    r = e.transpose(ps[:, i * 128:(i + 1) * 128], tm[:, (k0 + i) * 128:(k0 + i + 1) * 128], B.ident[:])
                return r
            P.op("pe", tr, reads=[kt, "ident"], writes=[kp])
            P.op("act", lambda e, n=n, k0=k0, nk=nk, ps=ps: e.copy(out=X[:, k0:k0 + nk, n * 128:(n + 1) * 128], in_=ps[:, 0:nk * 128].rearrange("p (k t) -> p k t", k=nk)), reads=[kp], writes=["X"])
    k_ = B.fm_rows(lambda i: XH[:, :, i], xhalo, 3, "XH")
    P.op("dve", lambda e: e.tensor_copy(out=XH[:], in_=XH[:]), reads=k_, writes=["XH"])
    B.full_barrier()
    es.close()

    mods = B.compute_mods_sharded(w_mod, b_mod, c_v, cctx_v, norm_g)

    def run_half(half):
        li0, j = 2 * half, half
        prm = prms[j]
        m0 = mods[li0]
        if half == 1:
            P.op("dve", lambda e: e.tensor_copy(out=HSRC[:, :, 0:1], in_=X[:, :, NCX:NCX + 1]), reads=["X"], writes=["HSRC"])
            P.op("dve", lambda e: e.tensor_copy(out=HSRC[:, :, 1:3], in_=X[:, :, T - 2:T]), reads=["X"], writes=["HSRC"])
            x_es = ExitStack()
            GH = exchange(B, HSRC[:].rearrange("p k t -> p (k t)"), KC * 3, "halo", "HSRC", x_es)
            GHv = GH[:].rearrange("p r (k t) -> p r k t", t=3)
            P.op("dve", lambda e: e.memset(XH[:], 0.0), reads=["XH"], writes=["XH"])
            for r in range(8):
                P.op("dve", lambda e, r=r: e.scalar_tensor_tensor(out=XH[:, :, 0:2], in0=GHv[:, r, :, 1:3], scalar=SEL[:, r:r + 1], in1=XH[:, :, 0:2], op0=ALU.mult, op1=ALU.add), reads=["XG_halo", "SEL", "XH"], writes=["XH"])
                P.op("dve", lambda e, r=r: e.scalar_tensor_tensor(out=XH[:, :, 2:3], in0=GHv[:, r, :, 0:1], scalar=SEL[:, 8 + r:9 + r], in1=XH[:, :, 2:3], op0=ALU.mult, op1=ALU.add), reads=["XG_halo", "SEL", "XH"], writes=["XH"])
            B.full_barrier()
            x_es.close()
        B.norm_mod(m0["GS_A"], m0["SH_A"], m0["keys"])
        es = ExitStack()
        sqh = B.sb(B.key("sqh"), [128, KC, 3], F32, es)
        rh = B.sb(B.key("rh"), [128, 3], F32, es)
        th = B.sb(B.key("th"), [128, KC, 3], F32, es)
        P.op("dve", lambda e: e.tensor_tensor(out=sqh[:], in0=XH[:], in1=XH[:], op=ALU.mult), reads=["XH"], writes=["sqh"])
        def mmh(e):
            r = None
            for k in range(KC):
                r = e.matmul(B.psC[0][:, 0:3], lhsT=B.ones32[:], rhs=sqh[:, k, :], start=(k == 0), stop=(k == KC - 1))
            return r
        P.op("pe", mmh, reads=["sqh", "ones32"], writes=["psC0"])
        P.op("dve", lambda e: e.tensor_scalar(out=rh[:], in0=B.psC[0][:, 0:3], scalar1=1.0 / D, scalar2=EPS, op0=ALU.mult, op1=ALU.add), reads=["psC0"], writes=["rh"])
        P.op("act", lambda e: e.activation(out=rh[:], in_=rh[:], func=AF.Ln), reads=["rh"], writes=["rh"])
        P.op("act", lambda e: e.activation(out=rh[:], in_=rh[:], func=AF.Exp, scale=-0.5), reads=["rh"], writes=["rh"])
        for k in range(KC):
            P.op("dve", lambda e, k=k: e.scalar_tensor_tensor(out=th[:, k, :], in0=XH[:, k, :], scalar=m0["GS_A"][:, k, 0:1], in1=rh[:], op0=ALU.mult, op1=ALU.mult), reads=["XH", "rh"] + m0["keys"], writes=["th"])
            P.op("act", lambda e, k=k: e.activation(out=HH[:, k, :], in_=th[:, k, :], func=AF.Identity, bias=m0["SH_A"][:, k, 0:1], scale=1.0), reads=["th"] + m0["keys"], writes=["HH"])
        B.full_barrier()
        es.close()

        lp = B.lru_params(j, prm)
        spill = nc.dram_tensor("lru_spill%d" % j, [KC * 4, 128, NL], F32).ap()
        B.lru_pass(prm, lp, m0, 0, NCX, True, 2, CFIN=CFIN, do_out=(half == 0))
        B.lru_pass(prm, lp, m0, NCX, T, False, 1, HH=HH, EF=EF, PSOUT=PSO, SPILL=spill)
        x_es2 = ExitStack()
        GPS = exchange(B, PSO[:].rearrange("p a b k -> p (a b k)"), 4 * KC, "ps%d" % half, "PSOUT", x_es2)
        GPv = GPS[:].rearrange("p r (a b k) -> p r a b k", a=2, b=2)
        gk = "XG_ps%d" % half
        P.op("dve", lambda e: e.tensor_copy(out=CAR[:], in_=CFIN[:]), reads=["CFIN", "CARRY"], writes=["CARRY"])
        for dr in range(2):
            order = list(range(8)) if dr == 0 else list(range(7, -1, -1))
            for r in order:
                P.op("dve", lambda e, dr=dr, r=r: e.tensor_tensor(out=T1[:], in0=CAR[:, dr, :], in1=GPv[:, r, dr, 0, :], op=ALU.mult), reads=["CARRY", gk], writes=["T1c"])
                P.op("dve", lambda e, dr=dr, r=r: e.tensor_tensor(out=T1[:], in0=T1[:], in1=GPv[:, r, dr, 1, :], op=ALU.add), reads=["T1c", gk], writes=["T1c"])
                P.op("dve", lambda e, dr=dr, r=r: e.tensor_tensor(out=T1[:], in0=T1[:], in1=CAR[:, dr, :], op=ALU.subtract), reads=["T1c", "CARRY"], writes=["T1c"])
                P.op("dve", lambda e, dr=dr, r=r: e.scalar_tensor_tensor(out=CAR[:, dr, :], in0=T1[:], scalar=CMK[:, dr * 8 + r:dr * 8 + r + 1], in1=CAR[:, dr, :], op0=ALU.mult, op1=ALU.add), reads=["T1c", "CARRY", "CMK"], writes=["CARRY"])
        B.full_barrier()
        x_es2.close()
        B.lru_pass(prm, lp, m0, NCX, T, False, 2, HH=HH, EF=EF, CARRY=CAR, do_out=True, SPILL=spill)
        B.norm_mod(m0["GS_B"], m0["SH_B"], m0["keys"])
        fsegs = None if half == 0 else [(NCX, T, 0)]
        B.swiglu_into_x(prm["ffn_w1"], prm["ffn_w3"], prm["ffn_w2"], m0["GT_B"], m0["keys"], tag="ffn", segs=fsegs)
        m1 = mods[li0 + 1]
        sp_es = ExitStack()
        sp = B.sgu_params(prm, sp_es)
        B.norm_mod(m1["GS_A"], m1["SH_A"], m1["keys"])
        if half == 0:
            B.sgu_pass(prm, sp, m1, 0, NCX, True)
        B.sgu_pass(prm, sp, m1, NCX, T, False)
        B.full_barrier()
        sp_es.close()
        B.norm_mod(m1["GS_B"], m1["SH_B"], m1["keys"])
        g_es = ExitStack()
        GATE = B.sb(B.key("GATE"), [128, T // 128, cfg.NEXP], F32, g_es)
        GBC = B.sb(B.key("GBC"), [128, T], F32, g_es)
        DG = B.sb(B.key("DG"), [128, 4, 128], F32, g_es)
        B.moe_gates(prm["moe_router"], m1, GATE=GATE)
        lat_segs = [(NCX, T, 0)]
        for ex in range(cfg.NEXP):
            B.gate_bcast(GATE, ex, GBC, DG)
            B.swiglu_into_x(prm["moe_w1"][ex], prm["moe_w3"][ex], prm["moe_w2"][ex], m1["GT_B"], m1["keys"], gbc=GBC, gbc_key="GBC", tag="moe", segs=lat_segs)
        if half == 0:
            NTC_ = T // 128
            GATEC = B.sb(B.key("GATEC"), [128, NTC_, 1], F32, g_es)
            DX = B.sb(B.key("DX"), [128, KC, NCX], F32, g_es)
            P.op("dve", lambda e: e.memset(DX[:], 0.0), writes=["DX"])
            P.op("dve", lambda e: e.tensor_scalar(out=GATEC[:, :, 0], in0=GATE[:, :, 0], scalar1=OHE[:, 0:1], scalar2=None, op0=ALU.mult), reads=["GATE", "OHE"], writes=["GATEC"])
            for ex in range(1, cfg.NEXP):
                P.op("dve", lambda e, ex=ex: e.scalar_tensor_tensor(out=GATEC[:, :, 0], in0=GATE[:, :, ex], scalar=OHE[:, ex:ex + 1], in1=GATEC[:, :, 0], op0=ALU.mult, op1=ALU.add), reads=["GATE", "OHE", "GATEC"], writes=["GATEC"])
            B.P.wait_all("dve", ["GATEC"])
            B.full_barrier()
            B.gate_bcast_keyed(GATEC, 0, GBC, DG, "GATEC")
            B.full_barrier()
            B.swiglu_into_x(cx_w1, cx_w3, cx_w2, m1["GT_B"], m1["keys"], gbc=GBC, gbc_key="GBC", tag="moe", segs=[(0, NCX, 1)], xdst=DX, use_pool=False)
            PIECE = 4 if KC % 4 == 0 else KC
            for p0 in range(0, KC, PIECE):
                p_es = ExitStack()
                GD = exchange(B, DX[:, p0:p0 + PIECE, :].rearrange("p k t -> p (k t)"), PIECE * NCX, "dx%d" % p0, "DX", p_es)
                GDv = GD[:].rearrange("p r (k t) -> p r k t", k=PIECE)
                for r in range(8):
                    P.op("dve", lambda e, r=r, p0=p0, GDv=GDv: e.tensor_tensor(out=X[:, p0:p0 + PIECE, 0:NCX], in0=X[:, p0:p0 + PIECE, 0:NCX], in1=GDv[:, r, :, :], op=ALU.add), reads=["XG_dx%d" % p0, "X"], writes=["X"])
                B.full_barrier()
                p_es.close()
        B.full_barrier()
        g_es.close()

    for half_ in range(2):
        run_half(half_)

    es = ExitStack()
    OT = [B.sb("OT%d" % i, [128, D], F32, es) for i in range(2)]
    FG = B.sb("FG", [128, KC], F32, es)
    P.dma("sp", lambda e: e.dma_start(out=FG[:], in_=final_g.rearrange("(k p) -> p k", p=128), allow_slow_non_contiguous=True), writes=["FG"])
    sq = [B.sb("fsq%d" % i, [128, T], BF16, es) for i in range(2)]
    tiles = col_tiles(0, T)
    for k in range(KC):
        s_ = sq[k % 2]
        ks = "fsq%d" % (k % 2)
        P.op("act", lambda e, k=k, s_=s_: e.activation(out=s_[:], in_=X[:, k, :], func=AF.Square), reads=["X"], writes=[ks])
        def mm(e, k=k, s_=s_):
            r = None
            for i, (a, b) in enumerate(tiles):
                r = e.matmul(B.psA[i][:, 0:b - a], lhsT=B.ones16[:], rhs=s_[:, a:b], start=(k == 0), stop=(k == KC - 1))
            return r
        P.op("pe", mm, reads=[ks, "ones16"], writes=["psA0", "psA1", "psA2"])
    for i, (a, b) in enumerate(tiles):
        P.op("dve", lambda e, i=i, a=a, b=b: e.tensor_scalar(out=B.RSTD[:, a:b], in0=B.psA[i][:, 0:b - a], scalar1=1.0 / D, scalar2=EPS, op0=ALU.mult, op1=ALU.add), reads=["psA%d" % i], writes=["RSTD"])
    P.op("act", lambda e: e.activation(out=B.RSTD[:], in_=B.RSTD[:], func=AF.Ln), reads=["RSTD"], writes=["RSTD"])
    P.op("act", lambda e: e.activation(out=B.RSTD[:], in_=B.RSTD[:], func=AF.Exp, scale=-0.5), reads=["RSTD"], writes=["RSTD"])
    for k in range(KC):
        P.op("dve", lambda e, k=k: e.scalar_tensor_tensor(out=X[:, k, NCX:T], in0=X[:, k, NCX:T], scalar=FG[:, k:k + 1], in1=B.RSTD[:, NCX:T], op0=ALU.mult, op1=ALU.mult), reads=["X", "RSTD", "FG"], writes=["X"])
    xout = B.dout("xout", [NL, D])
    okeys = []
    for n in range(NCX // 128, NTC):
        ot = OT[n % 2]
        ko = "OT%d" % (n % 2)
        for k0 in range(0, KC, 4):
            nk = min(4, KC - k0)
            pi = (k0 // 4) % 3
            ps = B.psA[pi]
            kp = "psA%d" % pi
            def tr(e, n=n, k0=k0, nk=nk, ps=ps):
                r = None
                for i in range(nk):
                    r = e.transpose(ps[:, i * 128:(i + 1) * 128], X[:, k0 + i, n * 128:(n + 1) * 128], B.ident[:])
                return r
            P.op("pe", tr, reads=["X", "ident"], writes=[kp])
            P.op("act", lambda e, ot=ot, k0=k0, nk=nk, ps=ps: e.copy(out=ot[:, k0 * 128:(k0 + nk) * 128], in_=ps[:, 0:nk * 128]), reads=[kp], writes=[ko])
        dst = xout[n * 128 - NCX:(n + 1) * 128 - NCX, :]
        kk = "o_x%d" % n
        P.dma("sp", lambda e, ot=ot, dst=dst: e.dma_start(out=dst, in_=ot[:]), reads=[ko], writes=[kk])
        okeys.append(kk)
    P.wait_all("sp", okeys)
    es.close()
    return B.finish(), B


def run_fused(cfg, inp):
    NCO, NL, D = cfg.NCORES, cfg.NL, cfg.D
    assert NCO == 8
    f = lambda a: np.ascontiguousarray(a, dtype=np.float32)
    x = f(inp["x"][0])
    ctx = f(inp["ctx"][0])
    ident = np.eye(128, dtype=np.float32)
    zero_row = np.zeros((1, D), np.float32)
    key = ("fused", cfg.D, cfg.NL, cfg.NCX, cfg.F, cfg.HEADS, cfg.SGROUPS, cfg.NEXP)
    if key not in _PROG_CACHE:
        _PROG_CACHE[key] = build_fused(cfg)
    nc, _ = _PROG_CACHE[key]
    shared = {"cin": ctx, "ident": ident, "c": f(inp["c"][0]), "c_ctx": f(inp["c_ctx"]), "final_g": f(inp["final_g"])}
    NS = (6 * D) // 8
    for li in range(4):
        shared["norm_g%d" % li] = f(inp["norm_g"][li])
    for j in range(2):
        for k in LRU_KEYS + ["ffn_w1", "ffn_w3", "ffn_w2", "sgu_w_in", "sgu_ln_g", "sgu_ln_b", "sgu_w_s", "sgu_b_s", "sgu_w_out", "moe_router", "moe_w1", "moe_w3", "moe_w2"]:
            a = inp[k][j]
            if k in ("lru_b_a", "lru_b_x"):
                a = a.reshape(2, D)
            shared["%s_%d" % (k, j)] = f(a)
    maps = []
    for c in range(NCO):
        d = dict(shared)
        d["xin"] = f(x[c * NL:(c + 1) * NL])
        for li in range(4):
            d["w_mod%d" % li] = f(inp["w_mod"][li][:, c * NS:(c + 1) * NS])
            d["b_mod%d" % li] = f(inp["b_mod"][li][c * NS:(c + 1) * NS])
        lo = x[c * NL - 2:c * NL] if c > 0 else np.concatenate([zero_row, zero_row], 0)
        hi = x[(c + 1) * NL:(c + 1) * NL + 1] if c < NCO - 1 else zero_row
        d["xhalo"] = f(np.concatenate([lo, hi], 0))
        e = np.ones((128, 2), np.float32)
        if c == 0:
            e[:, 0] = 0.0
        if c == NCO - 1:
            e[:, 1] = 0.0
        d["ef"] = e
        sel = np.zeros((128, 16), np.float32)
        if c > 0:
            sel[:, c - 1] = 1.0
        if c < NCO - 1:
            sel[:, 8 + c + 1] = 1.0
        d["selmask"] = sel
        cm = np.zeros((128, 16), np.float32)
        cm[:, 0:c] = 1.0
        cm[:, 8 + c + 1:16] = 1.0
        d["carmask"] = cm
        d["moe_ctx_w1"] = f(inp["moe_w1"][0][c])
        d["moe_ctx_w3"] = f(inp["moe_w3"][0][c])
        d["moe_ctx_w2"] = f(inp["moe_w2"][0][c])
        oh = np.zeros((128, cfg.NEXP), np.float32)
        oh[:, c] = 1.0
        d["ohexp"] = oh
        maps.append(d)
    res = run_bass_kernel_spmd(nc, maps, core_ids=list(range(NCO)))
    out = np.concatenate([res.results[c]["xout"] for c in range(NCO)], 0)
    return out[None].astype(np.float32)


def kernel(**inputs):
    cfg = Cfg()
    return run_fused(cfg, inputs)
```
